# Optimizing a Trainium2 kernel written in Bass

```python
import math
import jax, jax.numpy as jnp
from jax import lax
import numpy as np

D_MODEL = 1024
BATCH = 8
SEQ = 4096
DEPTH = 1

GRID_W = 64
CTX_LEN = 256
EPS = 1e-6

N_HEADS = 8
N_KV_HEADS = 2
HEAD_DIM = 64
AXIS_ROPE_DIM = HEAD_DIM // 2
ROPE_THETA = 10000.0
Q_BLOCK = 128

HY_DIM = D_MODEL // 2
HY_ORDER = 2
HY_SHORT = 3
HY_EMB_BANDS = 16
HY_EMB_DIM = 1 + 2 * HY_EMB_BANDS
HY_FFN = 64
HY_FAST_DECAY = 0.3
HY_SLOW_DECAY = 1.5
HY_TARGET = 1e-2

Q_DIM = N_HEADS * HEAD_DIM
KV_DIM = N_KV_HEADS * HEAD_DIM
HY_COLS = (HY_ORDER + 1) * HY_DIM
GATE_COLS = 2 * D_MODEL
IN_COLS = Q_DIM + 2 * KV_DIM + HY_COLS + GATE_COLS

PEER_HEADS = 8
PEER_N_KEYS = 128
PEER_N_EXPERTS = PEER_N_KEYS * PEER_N_KEYS
PEER_DK = 256
PEER_TOPK = 16
PEER_CHUNK = 128

kernel_name = "hybrid_hyena_gqa_peer_dit_block"


def rmsnorm(x, g):
    xf = x.astype(jnp.float32)
    y = xf * lax.rsqrt(jnp.mean(xf * xf, axis=-1, keepdims=True) + EPS)
    return (y * g.astype(jnp.float32)).astype(x.dtype)


def modulate(h, shift, scale):
    return h * (1.0 + scale) + shift


def axial_rope(rows):
    t = jnp.arange(rows * GRID_W)
    row = (t // GRID_W).astype(jnp.float32)
    col = (t % GRID_W).astype(jnp.float32)
    inv = ROPE_THETA ** (-jnp.arange(0, AXIS_ROPE_DIM, 2, dtype=jnp.float32) / AXIS_ROPE_DIM)
    ar = row[:, None] * inv
    ac = col[:, None] * inv
    ang = jnp.concatenate([ar, ar, ac, ac], axis=-1)
    return jnp.cos(ang), jnp.sin(ang)


def apply_rope(x, cos, sin):
    xf = x.astype(jnp.float32)
    half = AXIS_ROPE_DIM // 2

    def rot(a):
        return jnp.concatenate([-a[..., half:], a[..., :half]], axis=-1)

    xr = jnp.concatenate([rot(xf[..., :AXIS_ROPE_DIM]), rot(xf[..., AXIS_ROPE_DIM:])], axis=-1)
    return (xf * cos[:, None] + xr * sin[:, None]).astype(x.dtype)


def gqa_blocks(q, k, v):
    B, L, _, _ = q.shape
    G = N_HEADS // N_KV_HEADS
    nb = L // Q_BLOCK
    qb = jnp.moveaxis(q.reshape(B, nb, Q_BLOCK, N_KV_HEADS, G, HEAD_DIM), 1, 0)
    scale = HEAD_DIM ** -0.5

    def one_block(qblk):
        s = jnp.einsum('bqkgd,btkd->bkgqt', qblk, k) * scale
        p = jax.nn.softmax(s.astype(jnp.float32), axis=-1).astype(v.dtype)
        return jnp.einsum('bkgqt,btkd->bqkgd', p, v)

    o = lax.map(one_block, qb)
    return jnp.moveaxis(o, 0, 1).reshape(B, L, Q_DIM)


def short_conv(z, w, b):
    zp = jnp.pad(z, ((0, 0), (1, 1), (0, 0)))
    return zp[:, :-2] * w[0] + zp[:, 1:-1] * w[1] + zp[:, 2:] * w[2] + b


def implicit_filters(L, w1, b1, w2, b2, w3, b3, w4, freq):
    t01 = jnp.linspace(0.0, 1.0, L, dtype=jnp.float32)[:, None]
    w = 2.0 * math.pi * jnp.arange(L, dtype=jnp.float32)[:, None] / L
    f = jnp.linspace(1e-4, HY_EMB_BANDS - 1, HY_EMB_BANDS, dtype=jnp.float32)[None]
    z = jnp.concatenate([t01, jnp.cos(f * w), -jnp.sin(f * w)], axis=-1)
    act = lambda a: jnp.sin(freq * a)
    hmid = act(z @ w1 + b1)
    hmid = act(hmid @ w2 + b2)
    hmid = act(hmid @ w3 + b3)
    h = (hmid @ w4).astype(jnp.float32).reshape(L, HY_ORDER, 2, HY_DIM)
    max_decay = math.log(HY_TARGET) / HY_FAST_DECAY
    min_decay = math.log(HY_TARGET) / HY_SLOW_DECAY
    deltas = jnp.abs(jnp.linspace(min_decay, max_decay, HY_DIM, dtype=jnp.float32))
    h = h * jnp.exp(-t01[:, :, None, None] * deltas)
    fwd = h[:, :, 0]
    bwd = h[:, :, 1]
    k = jnp.concatenate([fwd, jnp.zeros_like(fwd[:1]), bwd[:0:-1]], axis=0)
    k = k / (jnp.sum(jnp.abs(k), axis=0, keepdims=True) + EPS)
    return jnp.fft.rfft(k, axis=0)


def fftconv(u, kf, d):
    L = u.shape[1]
    uf = u.astype(jnp.float32)
    U = jnp.fft.rfft(uf, n=2 * L, axis=1)
    y = jnp.fft.irfft(U * kf[None], n=2 * L, axis=1)[:, :L]
    return y + uf * d.astype(jnp.float32)


def hyena(z_hy, conv_w, conv_b, kf, skip):
    zs = short_conv(z_hy, conv_w, conv_b)
    v, x1, x2 = jnp.split(zs, 3, axis=-1)
    y = x1.astype(jnp.float32) * fftconv(v, kf[:, 0], skip[0])
    y = x2.astype(jnp.float32) * fftconv(y, kf[:, 1], skip[1])
    return y.astype(z_hy.dtype)


def mix(h, p, rope, kv_ext):
    B, L, _ = h.shape
    z = h @ p["w_in"]
    q, k, v, z_hy, gates = jnp.split(
        z, [Q_DIM, Q_DIM + KV_DIM, Q_DIM + 2 * KV_DIM, Q_DIM + 2 * KV_DIM + HY_COLS], axis=-1)
    q = rmsnorm(q.reshape(B, L, N_HEADS, HEAD_DIM), p["q_norm_g"])
    k = rmsnorm(k.reshape(B, L, N_KV_HEADS, HEAD_DIM), p["k_norm_g"])
    v = v.reshape(B, L, N_KV_HEADS, HEAD_DIM)
    if rope is not None:
        q = apply_rope(q, rope[0], rope[1])
        k = apply_rope(k, rope[0], rope[1])
    k_self, v_self = k, v
    if kv_ext is not None:
        k = jnp.concatenate([k, kv_ext[0]], axis=1)
        v = jnp.concatenate([v, kv_ext[1]], axis=1)
    attn = gqa_blocks(q, k, v)
    kf = implicit_filters(L, p["hf_w1"], p["hf_b1"], p["hf_w2"], p["hf_b2"],
                          p["hf_w3"], p["hf_b3"], p["hf_w4"], p["hf_freq"])
    hy = hyena(z_hy, p["hy_conv_w"], p["hy_conv_b"], kf, p["hy_skip"])
    g_attn, g_hy = jnp.split(gates, 2, axis=-1)
    merged = (jax.nn.sigmoid(g_attn) * (attn @ p["w_attn_out"])
              + jax.nn.sigmoid(g_hy) * (hy @ p["w_hy_out"]))
    return merged @ p["w_out"], k_self, v_self


def context_kv(hc, p):
    B, L, _ = hc.shape
    z = hc @ p["w_in"][:, Q_DIM:Q_DIM + 2 * KV_DIM]
    k, v = jnp.split(z, 2, axis=-1)
    k = rmsnorm(k.reshape(B, L, N_KV_HEADS, HEAD_DIM), p["k_norm_g"])
    return k, v.reshape(B, L, N_KV_HEADS, HEAD_DIM)


def peer(h, wq, keys1, keys2, u_tab, v_tab):
    B, L, D = h.shape
    xt = h.reshape(B * L // PEER_CHUNK, PEER_CHUNK, D)
    half = PEER_DK // 2

    def chunk(xc):
        q = (xc @ wq).reshape(PEER_CHUNK, PEER_HEADS, 2, half)
        s1 = jnp.einsum('chd,hnd->chn', q[:, :, 0], keys1).astype(jnp.float32)
        s2 = jnp.einsum('chd,hnd->chn', q[:, :, 1], keys2).astype(jnp.float32)
        v1, i1 = lax.top_k(s1, PEER_TOPK)
        v2, i2 = lax.top_k(s2, PEER_TOPK)
        cand = (v1[..., :, None] + v2[..., None, :]).reshape(PEER_CHUNK, PEER_HEADS, PEER_TOPK * PEER_TOPK)
        cidx = (i1[..., :, None] * PEER_N_KEYS + i2[..., None, :]).reshape(PEER_CHUNK, PEER_HEADS, PEER_TOPK * PEER_TOPK)
        best, pos = lax.top_k(cand, PEER_TOPK)
        eidx = jnp.take_along_axis(cidx, pos, axis=-1)
        g = jax.nn.softmax(best, axis=-1)
        a = jnp.einsum('chkd,cd->chk', u_tab[eidx], xc)
        wgt = (jax.nn.gelu(a.astype(jnp.float32)) * g).astype(xc.dtype)
        return jnp.einsum('chk,chkd->cd', wgt, v_tab[eidx])

    return lax.map(chunk, xt).reshape(B, L, D)


def setup_inputs(seed: int = 0) -> dict:
    key = jax.random.key(seed)
    ks = iter(jax.random.split(key, 40))

    def nrm(shape, scale):
        return jax.random.normal(next(ks), shape, jnp.float32) * scale

    L = DEPTH
    return {
        "x": nrm((BATCH, SEQ, D_MODEL), 1.0),
        "c": nrm((BATCH, D_MODEL), 1.0),
        "ctx": nrm((BATCH, CTX_LEN, D_MODEL), 1.0),
        "c_ctx": nrm((D_MODEL,), 1.0),
        "ada_w": nrm((L, D_MODEL, 6 * D_MODEL), D_MODEL ** -0.5),
        "ada_b": nrm((L, 6 * D_MODEL), 0.02),
        "norm_mix_g": 1.0 + nrm((L, D_MODEL), 0.02),
        "norm_ffn_g": 1.0 + nrm((L, D_MODEL), 0.02),
        "w_in": nrm((L, D_MODEL, IN_COLS), D_MODEL ** -0.5),
        "q_norm_g": 1.0 + nrm((L, HEAD_DIM), 0.02),
        "k_norm_g": 1.0 + nrm((L, HEAD_DIM), 0.02),
        "hy_conv_w": nrm((L, HY_SHORT, HY_COLS), HY_SHORT ** -0.5),
        "hy_conv_b": nrm((L, HY_COLS), 0.02),
        "hf_w1": nrm((L, HY_EMB_DIM, HY_FFN), HY_EMB_DIM ** -0.5),
        "hf_b1": nrm((L, HY_FFN), 0.02),
        "hf_w2": nrm((L, HY_FFN, HY_FFN), HY_FFN ** -0.5),
        "hf_b2": nrm((L, HY_FFN), 0.02),
        "hf_w3": nrm((L, HY_FFN, HY_FFN), HY_FFN ** -0.5),
        "hf_b3": nrm((L, HY_FFN), 0.02),
        "hf_w4": nrm((L, HY_FFN, HY_ORDER * 2 * HY_DIM), HY_FFN ** -0.5),
        "hf_freq": 1.0 + nrm((L, HY_FFN), 0.02),
        "hy_skip": nrm((L, HY_ORDER, HY_DIM), 0.5),
        "w_attn_out": nrm((L, Q_DIM, D_MODEL), Q_DIM ** -0.5),
        "w_hy_out": nrm((L, HY_DIM, D_MODEL), HY_DIM ** -0.5),
        "w_out": nrm((L, D_MODEL, D_MODEL), D_MODEL ** -0.5),
        "peer_wq": nrm((L, D_MODEL, PEER_HEADS * PEER_DK), D_MODEL ** -0.5),
        "peer_keys1": nrm((L, PEER_HEADS, PEER_N_KEYS, PEER_DK // 2), (PEER_DK // 2) ** -0.5),
        "peer_keys2": nrm((L, PEER_HEADS, PEER_N_KEYS, PEER_DK // 2), (PEER_DK // 2) ** -0.5),
        "peer_u": nrm((L, PEER_N_EXPERTS, D_MODEL), D_MODEL ** -0.5),
        "peer_v": nrm((L, PEER_N_EXPERTS, D_MODEL), PEER_HEADS ** -0.5),
        "final_norm_g": 1.0 + nrm((D_MODEL,), 0.02),
    }


def reference(x, c, ctx, c_ctx, ada_w, ada_b, norm_mix_g, norm_ffn_g, w_in, q_norm_g, k_norm_g,
              hy_conv_w, hy_conv_b, hf_w1, hf_b1, hf_w2, hf_b2, hf_w3, hf_b3, hf_w4, hf_freq, hy_skip,
              w_attn_out, w_hy_out, w_out, peer_wq, peer_keys1, peer_keys2, peer_u, peer_v, final_norm_g):
    ROWS = x.shape[1] // GRID_W
    rope = axial_rope(ROWS)
    sc = jax.nn.silu(c)
    scc = jax.nn.silu(c_ctx)
    for i in range(DEPTH):
        p = dict(w_in=w_in[i], q_norm_g=q_norm_g[i], k_norm_g=k_norm_g[i],
                 hy_conv_w=hy_conv_w[i], hy_conv_b=hy_conv_b[i],
                 hf_w1=hf_w1[i], hf_b1=hf_b1[i], hf_w2=hf_w2[i], hf_b2=hf_b2[i],
                 hf_w3=hf_w3[i], hf_b3=hf_b3[i], hf_w4=hf_w4[i], hf_freq=hf_freq[i],
                 hy_skip=hy_skip[i], w_attn_out=w_attn_out[i], w_hy_out=w_hy_out[i], w_out=w_out[i])
        mod = (sc @ ada_w[i] + ada_b[i])[:, None, :]
        mod_c = scc @ ada_w[i] + ada_b[i]
        sh1, sc1, g1, sh2, sc2, g2 = jnp.split(mod, 6, axis=-1)
        csh1, csc1, cg1, csh2, csc2, cg2 = jnp.split(mod_c, 6, axis=-1)

        h = modulate(rmsnorm(x, norm_mix_g[i]), sh1, sc1)
        hc = modulate(rmsnorm(ctx, norm_mix_g[i]), csh1, csc1)
        if i < DEPTH - 1:
            out_c, kc, vc = mix(hc, p, None, None)
        else:
            kc, vc = context_kv(hc, p)
        out, _, _ = mix(h, p, rope, (kc, vc))
        x = x + g1 * out
        x = x + g2 * peer(modulate(rmsnorm(x, norm_ffn_g[i]), sh2, sc2),
                          peer_wq[i], peer_keys1[i], peer_keys2[i], peer_u[i], peer_v[i])
        if i < DEPTH - 1:
            ctx = ctx + cg1 * out_c
            ctx = ctx + cg2 * peer(modulate(rmsnorm(ctx, norm_ffn_g[i]), csh2, csc2),
                                   peer_wq[i], peer_keys1[i], peer_keys2[i], peer_u[i], peer_v[i])
    return rmsnorm(x, final_norm_g)
```

```python
import numpy as np
import ml_dtypes
import concourse.bass as bass
import concourse.mybir as mybir
from concourse.bass_utils import run_bass_kernel_spmd

F32 = mybir.dt.float32
BF16 = mybir.dt.bfloat16
I32 = mybir.dt.int32
U32 = mybir.dt.uint32
U8 = mybir.dt.uint8
ALU = mybir.AluOpType
AF = mybir.ActivationFunctionType
AX = mybir.AxisListType

D = 1024
L = 4096
NT = 32
CTX = 256
EPS = 1e-6
NKEY = L + CTX
NKC = NKEY // 128
IN_COLS = 4352
HYC = 512


ATTACH_WAITS = True


class KB:
    def __init__(self, nc, n_dma_slots=32):
        self.nc = nc
        self.eng = {"pe": nc.tensor, "act": nc.scalar, "dve": nc.vector,
                    "pool": nc.gpsimd, "sp": nc.sync}
        self.sem, self.cnt, self.semobj = {}, {}, {}
        for n in self.eng:
            self.sem[n] = nc.alloc_semaphore("s_" + n)
            self.cnt[n] = 0
            self.semobj["s_" + n] = self.sem[n]
        self.slots = []
        for i in range(n_dma_slots):
            s = nc.alloc_semaphore("d_%d" % i)
            self.slots.append([s, 0])
            self.semobj["d_%d" % i] = s
        self.slot_rr = 0
        self.known = {n: {} for n in self.eng}
        self.lastw, self.reads = {}, {}
        self.ninstr = 0

    def _need(self, e, tick, pend=None):
        if tick is None:
            return
        sn, val = tick
        if self.known[e].get(sn, 0) >= val:
            return
        self.known[e][sn] = val
        if pend is not None:
            pend[sn] = max(pend.get(sn, 0), val)
            return
        self.eng[e].wait_ge(self.semobj[sn], val)
        self.ninstr += 1

    def _pre(self, e, reads, writes, pend=None):
        for k in reads:
            self._need(e, self.lastw.get(k), pend)
        for k in writes:
            self._need(e, self.lastw.get(k), pend)
            for t in self.reads.get(k, ()):
                self._need(e, t, pend)

    def _flush(self, e, pend, keep_last):
        items = list(pend.items())
        last = None
        if keep_last and items:
            last = items.pop()
        for sn, val in items:
            self.eng[e].wait_ge(self.semobj[sn], val)
            self.ninstr += 1
        return last

    def _post(self, tick, reads, writes):
        for k in reads:
            self.reads.setdefault(k, []).append(tick)
        for k in writes:
            self.lastw[k] = tick
            self.reads[k] = []

    def op(self, e, fn, reads=(), writes=()):
        psr = [r for r in reads if isinstance(r, str) and r.startswith("ps")]
        if psr:
            reads = [r for r in reads if r not in psr]
            writes = list(writes) + psr
        pend = {}
        self._pre(e, reads, writes, pend)
        single = ATTACH_WAITS and getattr(fn, "__name__", "") == "<lambda>"
        last = self._flush(e, pend, single)
        ins = fn(self.eng[e])
        if last is not None:
            ins._wait_ge(self.semobj[last[0]], last[1])
        self.cnt[e] += 1
        ins.then_inc(self.sem[e], 1)
        self.ninstr += 1
        tick = ("s_" + e, self.cnt[e])
        self._post(tick, reads, writes)
        return tick

    def dma(self, e, out, in_, reads=(), writes=(), fn=None, **kw):
        pend = {}
        self._pre(e, reads, writes, pend)
        si = self.slot_rr
        self.slot_rr = (self.slot_rr + 1) % len(self.slots)
        slot = self.slots[si]
        sn = "d_%d" % si
        self._need(e, (sn, slot[1]) if slot[1] else None, pend)
        last = self._flush(e, pend, ATTACH_WAITS)
        if fn is None:
            ins = self.eng[e].dma_start(out=out, in_=in_, **kw)
        else:
            ins = fn(self.eng[e])
        if last is not None:
            ins._wait_ge(self.semobj[last[0]], last[1])
        slot[1] += 16
        ins.then_inc(slot[0], 16)
        self.ninstr += 1
        tick = (sn, slot[1])
        self._post(tick, reads, writes)
        return tick

    def barrier(self):
        for e in self.eng:
            for i, s in enumerate(self.slots):
                if s[1]:
                    self._need(e, ("d_%d" % i, s[1]))
            for n in self.eng:
                if n != e and self.cnt[n]:
                    self._need(e, ("s_" + n, self.cnt[n]))
        self.lastw, self.reads = {}, {}


class Arena:
    def __init__(self, nc, nbytes):
        self.big = nc.alloc_sbuf_tensor("arena", [128, nbytes], U8)
        self.nbytes = nbytes
        self.off = 0

    def reset(self, off=0):
        self.off = off

    def alloc(self, shape, dtype, parts=128):
        isz = 4 if dtype in (F32, I32, U32) else 2
        n = int(np.prod(shape[1:]))
        size = (n * isz + 63) // 64 * 64
        assert self.off + size <= self.nbytes, ("arena overflow", self.off, size, self.nbytes)
        ap = self.big[0:shape[0], self.off:self.off + n * isz].bitcast(dtype)
        self.off += size
        if len(shape) > 2:
            names = " ".join("d%d" % i for i in range(1, len(shape)))
            kw = {"d%d" % i: shape[i] for i in range(1, len(shape))}
            ap = ap.rearrange("p (%s) -> p %s" % (names, names), **kw)
        return ap


def bcast(ap, shape):
    return ap.to_broadcast(list(shape))


def build_program(debug=(), stop=None, skip=()):
    nc = bass.Bass("TRN2", target_bir_lowering=False)
    IN = {}

    def inp(name, shape, dt=F32):
        IN[name] = nc.dram_tensor(name, list(shape), dt, kind="ExternalInput").ap()
        return IN[name]

    x_d = inp("x", [L, D])
    ctx_d = inp("ctx", [CTX, D])
    cT_d = inp("cT", [128, 8])
    cctxT_d = inp("cctxT", [128, 8])
    adaw_d = inp("ada_w", [D, 6 * D])
    adabT_d = inp("ada_bT", [128, 48])
    gmixT_d = inp("gmixT", [128, 8])
    gffnT_d = inp("gffnT", [128, 8])
    win_d = inp("w_in", [D, IN_COLS])
    cos_d = inp("rope_cos", [128, NT * 64])
    sin_d = inp("rope_sin", [128, NT * 64])
    gq_d = inp("gq", [1, 64])
    gk_d = inp("gk", [1, 64])
    gqsw_d = inp("gqsw", [1, 64])
    gksw_d = inp("gksw", [1, 64])
    wao_d = inp("w_attn_out", [512, D])
    wout_d = inp("w_out", [D, D])
    gfin_d = inp("final_norm_g", [1, D])
    why_d = inp("w_hy_out", [HYC, D]); wq_d = inp("peer_wq", [D, 2048]); keysT_d = inp("keysT", [128, 2048])
    pu_d = inp("peer_u", [16384, D]); pv_d = inp("peer_v", [16384, D])
    W1_d = inp("W1", [128, 512]); KC_d = inp("KC", [128, 128]); KS_d = inp("KS", [128, 128]); NKS_d = inp("NKS", [128, 128])
    R1_d = inp("R1", [128, 256]); R2_d = inp("R2", [128, 256]); C2_d = inp("C2", [128, 256]); NS2_d = inp("NS2", [128, 256])
    TW1_d = inp("TW1", [128, 768]); TW2_d = inp("TW2", [128, 768]); skipT_d = inp("skipT", [128, 256])
    zT_d = inp("zT", [33, L]); t01_d = inp("t01", [1, L]); ndelT_d = inp("ndelT", [128, 4])
    hfw1_d = inp("hf_w1", [33, 64]); hfw2_d = inp("hf_w2", [64, 64]); hfw3_d = inp("hf_w3", [64, 64]); hfw4_d = inp("hf_w4", [64, 2048])
    hfb_d = inp("hf_b", [64, 4]); hcw_d = inp("hy_conv_w", [3, 3 * HYC]); hcb_d = inp("hy_conv_b", [1, 3 * HYC])

    out_d = nc.dram_tensor("out", [L, D], F32, kind="ExternalOutput").ap()
    DBG = {}

    def dbg_out(name, shape, dt=F32):
        DBG[name] = nc.dram_tensor("dbg_" + name, list(shape), dt, kind="ExternalOutput").ap()
        return DBG[name]

    zhy_d = nc.dram_tensor("zhy_s", [L + 2, 3 * HYC], F32).ap()
    gates_d = nc.dram_tensor("gates_s", [L, 2 * D], BF16).ap()
    attnT_d = nc.dram_tensor("attnT_s", [8, 64, L], BF16).ap()
    kf_d = nc.dram_tensor("kf_s", [2, 128, 128, 768], F32).ap()
    uvb_d = nc.dram_tensor("uvb_s", [16384, 2 * D], BF16).ap()

    k = KB(nc)
    A = Arena(nc, 206 * 1024)
    PS = [nc.alloc_psum_tensor("ps%d" % i, [128, 512], F32) for i in range(8)]

    ident = A.alloc([128, 128], F32)
    ti = A.alloc([128, 128], I32)
    k.op("pool", lambda e: e.iota(ti, pattern=[[1, 128]], base=0, channel_multiplier=-1), writes=["ti"])
    k.op("dve", lambda e: e.tensor_scalar(ident, ti, 0, None, ALU.is_equal), reads=["ti"], writes=["ident"])
    modT = A.alloc([128, 48, 2], F32)
    A1 = A.alloc([128, 8], F32)
    Ac1 = A.alloc([128, 8], F32)
    A2 = A.alloc([128, 8], F32)
    gmixT = A.alloc([128, 8], F32)
    gffnT = A.alloc([128, 8], F32)
    negmb = A.alloc([128, 1], F32)
    epsc = A.alloc([128, 1], F32)
    k.op("dve", lambda e: e.memset(epsc, EPS), writes=["epsc"])
    PERSIST = A.off

    craw = A.alloc([128, 2, 8], F32)
    sc2 = A.alloc([128, 8, 2], F32)
    adabT = A.alloc([128, 48], F32)
    k.dma("sp", craw[:, 0, :], cT_d, writes=["craw0"])
    k.dma("sp", craw[:, 1, :], cctxT_d, writes=["craw1"])
    k.dma("sp", adabT, adabT_d, writes=["adabT"])
    k.dma("sp", gmixT, gmixT_d, writes=["gmixT"])
    k.dma("sp", gffnT, gffnT_d, writes=["gffnT"])
    k.op("act", lambda e: e.activation(sc2[:, :, 0], craw[:, 0, :], AF.Silu), reads=["craw0"], writes=["sc2a"])
    k.op("act", lambda e: e.activation(sc2[:, :, 1], craw[:, 1, :], AF.Silu), reads=["craw1"], writes=["sc2b"])
    awt = [A.alloc([128, 8, 1024], F32) for _ in range(2)]
    adaw_v = adaw_d.rearrange("(k p) c -> p k c", p=128)
    for blk in range(6):
        b = blk % 2
        for kk in range(8):
            k.dma("sp" if kk % 2 == 0 else "pool", awt[b][:, kk, :], adaw_d[kk * 128:(kk + 1) * 128, blk * 1024:(blk + 1) * 1024],
                  writes=[("awt", b, kk)])

        def mm(e, blk=blk, b=b):
            ins = None
            for n in range(8):
                cn = blk * 8 + n
                for kk in range(8):
                    ins = e.matmul(PS[0][:, 2 * cn:2 * cn + 2], awt[b][:, kk, n * 128:(n + 1) * 128], sc2[:, kk, :],
                                   start=(kk == 0), stop=(kk == 7))
            return ins
        k.op("pe", mm, reads=[("awt", b, kk) for kk in range(8)] + ["sc2a", "sc2b"], writes=["ps0"])
    k.op("dve", lambda e: e.tensor_tensor(modT, PS[0][:, 0:96].rearrange("p (c t) -> p c t", t=2),
                                          bcast(adabT.unsqueeze(2), [128, 48, 2]), ALU.add),
         reads=["ps0", "adabT"], writes=["modT"])
    k.op("dve", lambda e: e.scalar_tensor_tensor(A1, modT[:, 8:16, 0], 1.0, gmixT, ALU.add, ALU.mult),
         reads=["modT", "gmixT"], writes=["A1"])
    k.op("dve", lambda e: e.scalar_tensor_tensor(Ac1, modT[:, 8:16, 1], 1.0, gmixT, ALU.add, ALU.mult),
         reads=["modT", "gmixT"], writes=["Ac1"])
    k.op("dve", lambda e: e.scalar_tensor_tensor(A2, modT[:, 32:40, 0], 1.0, gffnT, ALU.add, ALU.mult),
         reads=["modT", "gffnT"], writes=["A2"])
    if "modT" in debug:
        k.dma("sp", dbg_out("modT", [128, 96]), modT.rearrange("p c t -> p (c t)"), reads=["modT"])
    k.barrier()
    A.reset(PERSIST)

    if stop == 'p0':
        return nc, IN, DBG, k
    winb = A.alloc([128, 8, IN_COLS], BF16)
    QT = A.alloc([128, 4, L], BF16)
    KT = A.alloc([128, NKEY], BF16)
    VX = A.alloc([128, NKC, 2, 65], BF16)
    cosq = A.alloc([128, NT, 64], F32)
    sinq = A.alloc([128, NT, 64], F32)
    cosk = A.alloc([128, NT, 64], F32)
    sink = A.alloc([128, NT, 64], F32)
    gtab = A.alloc([128, 4, 64], F32)
    P3 = A.off
    for j, gd in enumerate((gq_d, gk_d, gqsw_d, gksw_d)):
        k.dma("sp", gtab[:, j, :], bcast(gd, [128, 64]), writes=[("gtab", j)])
    HW = IN_COLS // 4
    wst = [A.alloc([128, HW], F32) for _ in range(4)]
    for kk in range(8):
        for hh in range(4):
            k.dma("sp" if hh % 2 == 0 else "pool", wst[hh], win_d[kk * 128:(kk + 1) * 128, hh * HW:(hh + 1) * HW], writes=[("wst", hh)])
            if hh % 2 == 0:
                k.op("act", lambda e, kk=kk, hh=hh: e.copy(winb[:, kk, hh * HW:(hh + 1) * HW], wst[hh]), reads=[("wst", hh)], writes=[("winb", kk, hh)])
            else:
                k.op("dve", lambda e, kk=kk, hh=hh: e.tensor_copy(winb[:, kk, hh * HW:(hh + 1) * HW], wst[hh]), reads=[("wst", hh)], writes=[("winb", kk, hh)])
    for q4 in range(4):
        k.dma("sp", cosk[:, q4 * 8:(q4 + 1) * 8, :].rearrange("p a b -> p (a b)"), cos_d[:, q4 * 512:(q4 + 1) * 512], writes=[("cosk", q4)])
        k.dma("pool", sink[:, q4 * 8:(q4 + 1) * 8, :].rearrange("p a b -> p (a b)"), sin_d[:, q4 * 512:(q4 + 1) * 512], writes=[("sink", q4)])
    k.op("dve", lambda e: e.tensor_tensor(cosq, cosk, bcast(gtab[:, 0:1, :], [128, NT, 64]), ALU.mult),
         reads=[("cosk", q) for q in range(4)] + [("gtab", 0)], writes=["cosq"])
    k.op("dve", lambda e: e.tensor_tensor(sinq, sink, bcast(gtab[:, 2:3, :], [128, NT, 64]), ALU.mult),
         reads=[("sink", q) for q in range(4)] + [("gtab", 2)], writes=["sinq"])
    k.op("dve", lambda e: e.tensor_tensor(cosk, cosk, bcast(gtab[:, 1:2, :], [128, NT, 64]), ALU.mult),
         reads=["cosq", ("gtab", 1)], writes=["cosk"])
    k.op("dve", lambda e: e.tensor_tensor(sink, sink, bcast(gtab[:, 3:4, :], [128, NT, 64]), ALU.mult),
         reads=["sinq", ("gtab", 3)], writes=["sink"])
    mqk = A.alloc([128, 2], F32)
    k.op("dve", lambda e: e.tensor_reduce(mqk[:, 0:1], gtab[:, 0, :], AX.X, ALU.max, apply_absolute_value=True),
         reads=[("gtab", 0)], writes=["mqk0"])
    k.op("dve", lambda e: e.tensor_reduce(mqk[:, 1:2], gtab[:, 1, :], AX.X, ALU.max, apply_absolute_value=True),
         reads=[("gtab", 1)], writes=["mqk1"])
    k.op("dve", lambda e: e.scalar_tensor_tensor(negmb, mqk[:, 0:1], -8.0, mqk[:, 1:2], ALU.mult, ALU.mult),
         reads=["mqk0", "mqk1"], writes=["negmb"])
    k.op("pool", lambda e: e.memset(VX[:, :, :, 64:65], 1.0), writes=["vx1"])
    zrow = A.alloc([128, 3 * HYC], F32)
    k.op("pool", lambda e: e.memset(zrow, 0.0), writes=["zrow"])
    k.dma("pool", zhy_d[0:1, :], zrow[0:1, :], reads=["zrow"])
    k.dma("pool", zhy_d[L + 1:L + 2, :], zrow[0:1, :], reads=["zrow"])

    k.barrier()
    if stop == 'p3a':
        return nc, IN, DBG, k
    A.reset(P3)
    xb = [A.alloc([128, D], F32) for _ in range(2)]
    xn = [A.alloc([128, D], F32) for _ in range(2)]
    hT = [A.alloc([128, 8, 128], BF16) for _ in range(2)]
    st = [A.alloc([128, 24], F32) for _ in range(2)]
    sq = A.alloc([128, 640], F32)
    t1 = A.alloc([128, 640], F32)
    t2 = A.alloc([128, 640], F32)
    qrp = A.alloc([128, 4, 2, 64], F32)
    kr = A.alloc([128, 128], F32)
    zh = [A.alloc([128, 3 * HYC], F32)] * 2
    gs = [A.alloc([128, 2 * D], BF16)] * 2
    x_v = x_d.rearrange("(p i) d -> i p d", i=NT)
    zhy_v = zhy_d[1:L + 1, :].rearrange("(p i) c -> i p c", i=NT)
    gates_v = gates_d.rearrange("(p i) c -> i p c", i=NT)
    zb = 0

    for it in range(NT + 2):
        if stop == 'p3b' and it == 1:
            k.barrier()
            return nc, IN, DBG, k
        b = it % 2
        isx = it < NT
        src = x_v[it] if isx else ctx_d[(it - NT) * 128:(it - NT + 1) * 128, :]
        k.dma("sp", xb[b], src, writes=[("xb", b)])
        s = st[b]
        k.op("act", lambda e, b=b, s=s: e.activation(xn[b], xb[b], AF.Square, accum_out=s[:, 0:1]),
             reads=[("xb", b)], writes=[("xn", b), ("st", b)])
        k.op("dve", lambda e, s=s: e.tensor_scalar(s[:, 1:2], s[:, 0:1], 1.0 / D, EPS, ALU.mult, ALU.add),
             reads=[("st", b)], writes=[("st", b)])
        k.op("act", lambda e, s=s: e.activation(s[:, 2:3], s[:, 1:2], AF.Sqrt), reads=[("st", b)], writes=[("st", b)])
        k.op("dve", lambda e, s=s: e.reciprocal(s[:, 3:4], s[:, 2:3]), reads=[("st", b)], writes=[("st", b)])
        k.op("act", lambda e, b=b, s=s: e.activation(xn[b], xb[b], AF.Identity, scale=s[:, 3:4]),
             reads=[("xb", b), ("st", b)], writes=[("xn", b)])
        for half in range(2):
            def tr(e, b=b, half=half):
                ins = None
                for j in range(4):
                    jj = half * 4 + j
                    ins = e.transpose(PS[half][:, j * 128:(j + 1) * 128], xn[b][:, jj * 128:(jj + 1) * 128], ident)
                return ins
            k.op("pe", tr, reads=[("xn", b), "ident"], writes=["ps%d" % half])
        Asc = A1 if isx else Ac1
        bcol = 0 if isx else 1
        for jj in range(8):
            half, j = jj // 4, jj % 4
            if jj % 2 == 0:
                k.op("act", lambda e, b=b, jj=jj, half=half, j=j, Asc=Asc, bcol=bcol: e.activation(
                    hT[b][:, jj, :], PS[half][:, j * 128:(j + 1) * 128], AF.Identity,
                    scale=Asc[:, jj:jj + 1], bias=modT[:, jj, bcol:bcol + 1]),
                    reads=["ps%d" % half, "A1", "Ac1", "modT"], writes=[("hT", b, jj)])
            else:
                k.op("dve", lambda e, b=b, jj=jj, half=half, j=j, Asc=Asc, bcol=bcol: e.tensor_scalar(
                    hT[b][:, jj, :], PS[half][:, j * 128:(j + 1) * 128],
                    Asc[:, jj:jj + 1], modT[:, jj, bcol:bcol + 1], ALU.mult, ALU.add),
                    reads=["ps%d" % half, "A1", "Ac1", "modT"], writes=[("hT", b, jj)])
        hkeys = [("hT", b, jj) for jj in range(8)]
        wkeys = []

        def zmm(c0, n, bank):
            def f(e):
                ins = None
                for kk in range(8):
                    ins = e.matmul(PS[bank][:, 0:n], hT[b][:, kk, :], winb[:, kk, c0:c0 + n], start=(kk == 0), stop=(kk == 7))
                return ins
            k.op("pe", f, reads=hkeys + wkeys, writes=["ps%d" % bank])

        bank = 2 + zb % 4; zb += 1
        zmm(512, 256, bank)
        kp = PS[bank][:, 0:128]
        k.op("act", lambda e, kp=kp: e.activation(sq[:, 512:640], kp, AF.Square), reads=["ps%d" % bank], writes=["sqk"])
        k.op("dve", lambda e, s=s: e.tensor_reduce(s[:, 4:6], sq[:, 512:640].rearrange("p (h d) -> p h d", d=64), AX.X, ALU.add),
             reads=["sqk"], writes=[("st", b)])
        k.op("dve", lambda e, s=s: e.tensor_scalar(s[:, 4:6], s[:, 4:6], 1.0 / 64, EPS, ALU.mult, ALU.add),
             reads=[("st", b)], writes=[("st", b)])
        k.op("act", lambda e, s=s: e.activation(s[:, 4:6], s[:, 4:6], AF.Sqrt), reads=[("st", b)], writes=[("st", b)])
        k.op("dve", lambda e, s=s: e.reciprocal(s[:, 6:8], s[:, 4:6]), reads=[("st", b)], writes=[("st", b)])
        kp3 = kp.rearrange("p (h d) -> p h d", d=64)
        t1k = t1[:, 512:640].rearrange("p (h d) -> p h d", d=64)
        t2k = t2[:, 512:640].rearrange("p (h d) -> p h d", d=64)
        if isx:
            k.op("dve", lambda e: e.tensor_tensor(t1k, kp3, bcast(cosk[:, it:it + 1, :], [128, 2, 64]), ALU.mult),
                 reads=["ps%d" % bank, "cosk"], writes=["t1k"])
            for a in range(2):
                for f in range(2):
                    o0 = a * 32 + f * 16
                    i0 = a * 32 + (1 - f) * 16
                    k.op("dve", lambda e, o0=o0, i0=i0: e.tensor_tensor(
                        t2k[:, :, o0:o0 + 16], kp3[:, :, i0:i0 + 16],
                        bcast(sink[:, it:it + 1, o0:o0 + 16], [128, 2, 16]), ALU.mult),
                        reads=["ps%d" % bank, "sink"], writes=[("t2k", a, f)])
            k.op("dve", lambda e: e.tensor_tensor(t1k, t1k, t2k, ALU.add),
                 reads=["t1k"] + [("t2k", a, f) for a in range(2) for f in range(2)], writes=["t1k"])
        else:
            k.op("dve", lambda e: e.tensor_tensor(t1k, kp3, bcast(gtab[:, 1:2, :], [128, 2, 64]), ALU.mult),
                 reads=["ps%d" % bank, ("gtab", 1)], writes=["t1k"])
        k.op("dve", lambda e, s=s: e.tensor_tensor(kr.rearrange("p (h d) -> p h d", d=64), t1k,
                                                   bcast(s[:, 6:8].unsqueeze(2), [128, 2, 64]), ALU.mult),
             reads=["t1k", ("st", b)], writes=["kr"])
        k.op("act", lambda e, bank=bank: e.copy(VX[:, it, :, 0:64], PS[bank][:, 128:256].rearrange("p (g d) -> p g d", d=64)),
             reads=["ps%d" % bank], writes=[("vx", it)])
        k.op("pe", lambda e: e.transpose(PS[7][:, 0:128], kr, ident), reads=["kr", "ident"], writes=["ps7"])
        k.op("act", lambda e: e.copy(KT[:, it * 128:(it + 1) * 128], PS[7][:, 0:128]), reads=["ps7"], writes=[("kt", it)])
        if not isx:
            continue
        bank = 2 + zb % 4; zb += 1
        zmm(0, 512, bank)
        qp = PS[bank][:, 0:512]
        k.op("act", lambda e, qp=qp: e.activation(sq[:, 0:512], qp, AF.Square), reads=["ps%d" % bank], writes=["sqq"])
        k.op("dve", lambda e, s=s: e.tensor_reduce(s[:, 8:16], sq[:, 0:512].rearrange("p (h d) -> p h d", d=64), AX.X, ALU.add),
             reads=["sqq"], writes=[("st", b)])
        k.op("dve", lambda e, s=s: e.tensor_scalar(s[:, 8:16], s[:, 8:16], 1.0 / 64, EPS, ALU.mult, ALU.add),
             reads=[("st", b)], writes=[("st", b)])
        k.op("act", lambda e, s=s: e.activation(s[:, 8:16], s[:, 8:16], AF.Sqrt), reads=[("st", b)], writes=[("st", b)])
        k.op("dve", lambda e, s=s: e.reciprocal(s[:, 16:24], s[:, 8:16]), reads=[("st", b)], writes=[("st", b)])
        qp3 = qp.rearrange("p (h d) -> p h d", d=64)
        t1q = t1[:, 0:512].rearrange("p (h d) -> p h d", d=64)
        t2q = t2[:, 0:512].rearrange("p (h d) -> p h d", d=64)
        k.op("dve", lambda e: e.tensor_tensor(t1q, qp3, bcast(cosq[:, it:it + 1, :], [128, 8, 64]), ALU.mult),
             reads=["ps%d" % bank, "cosq"], writes=["t1q"])
        for a in range(2):
            for f in range(2):
                o0 = a * 32 + f * 16
                i0 = a * 32 + (1 - f) * 16
                k.op("dve", lambda e, o0=o0, i0=i0: e.tensor_tensor(
                    t2q[:, :, o0:o0 + 16], qp3[:, :, i0:i0 + 16],
                    bcast(sinq[:, it:it + 1, o0:o0 + 16], [128, 8, 16]), ALU.mult),
                    reads=["ps%d" % bank, "sinq"], writes=[("t2q", a, f)])
        k.op("dve", lambda e: e.tensor_tensor(t1q, t1q, t2q, ALU.add),
             reads=["t1q"] + [("t2q", a, f) for a in range(2) for f in range(2)], writes=["t1q"])
        k.op("dve", lambda e, s=s: e.tensor_tensor(
            qrp.rearrange("p a s d -> p s a d"), t1[:, 0:512].rearrange("p (s a d) -> p s a d", s=2, a=4),
            bcast(s[:, 16:24].rearrange("p (s a) -> p s a", s=2).unsqueeze(3), [128, 2, 4, 64]), ALU.mult),
            reads=["t1q", ("st", b)], writes=["qrp"])

        def trq(e):
            ins = None
            for a in range(4):
                ins = e.transpose(PS[6][:, a * 128:(a + 1) * 128], qrp[:, a, :, :].rearrange("p s d -> p (s d)"), ident)
            return ins
        k.op("pe", trq, reads=["qrp", "ident"], writes=["ps6"])
        k.op("act", lambda e: e.copy(QT[:, :, it * 128:(it + 1) * 128], PS[6][:, :].rearrange("p (a t) -> p a t", a=4)),
             reads=["ps6"], writes=[("qt", it)])
        for c in range(3):
            bank = 2 + zb % 4; zb += 1
            zmm(768 + c * 512, 512, bank)
            k.op("act", lambda e, c=c, bank=bank: e.copy(zh[b][:, c * 512:(c + 1) * 512], PS[bank][:, :]),
                 reads=["ps%d" % bank], writes=[("zh", c)])
        for c in range(3):
            if "scr" not in skip:
                k.dma("pool", zhy_v[it][:, c * 512:(c + 1) * 512], zh[b][:, c * 512:(c + 1) * 512], reads=[("zh", c)])
        for c in range(4):
            bank = 2 + zb % 4; zb += 1
            zmm(2304 + c * 512, 512, bank)
            k.op("act", lambda e, c=c, bank=bank: e.activation(gs[b][:, c * 512:(c + 1) * 512], PS[bank][:, :], AF.Sigmoid),
                 reads=["ps%d" % bank], writes=[("gs", c)])
        if "scr" not in skip:
            k.dma("pool", gates_v[it], gs[b], reads=[("gs", c) for c in range(4)])

    if "QT" in debug:
        k.barrier()
        A.reset(P3)
        qf = A.alloc([128, 4, 512], F32)
        k.op("dve", lambda e: e.tensor_copy(qf, QT[:, :, 0:512]), reads=[("qt", i) for i in range(4)], writes=["qf"])
        dq = dbg_out("QT", [128, 2048])
        for a in range(4):
            k.dma("sp", dq[:, a * 512:(a + 1) * 512], qf[:, a, :], reads=["qf"])
        kf_ = A.alloc([128, NKEY], F32)
        k.op("dve", lambda e: e.tensor_copy(kf_, KT), reads=[("kt", i) for i in range(NKC)], writes=["kf_"])
        dk = dbg_out("KT", [128, NKEY])
        for a in range(NKC // 2):
            k.dma("sp", dk[:, a * 256:(a + 1) * 256], kf_[:, a * 256:(a + 1) * 256], reads=["kf_"])
    k.barrier()
    A.reset(P3)

    if stop == 'p3':
        return nc, IN, DBG, k
    pT = [A.alloc([128, 512], BF16) for _ in range(3)]
    osb = [A.alloc([128, 512], F32) for _ in range(2)]
    rec = A.alloc([128, 512], F32)
    aT = [A.alloc([128, 512], BF16) for _ in range(2)]
    ones = A.alloc([128, 64], BF16)
    onesf = A.alloc([128, 64], F32)
    k.op("dve", lambda e: e.memset(onesf, 1.0), writes=["onesf"])
    cst = [A.alloc([128, D], F32) for _ in range(4)]
    cbf = [A.alloc([128, D], BF16) for _ in range(4)]
    cvn = [0]

    def conv_chunk():
        cn = cvn[0]; cvn[0] += 1
        if cn >= 256:
            return
        tb, rch, b4 = cn // 128, cn % 128, cn % 4
        src_d = pu_d if tb == 0 else pv_d
        k.dma("sp", cst[b4], src_d[rch * 128:(rch + 1) * 128, :], writes=[("cst", b4)])
        k.op("dve", lambda e: e.tensor_copy(cbf[b4], cst[b4]), reads=[("cst", b4)], writes=[("cbf", b4)])
        k.dma("sp", uvb_d[rch * 128:(rch + 1) * 128, tb * D:(tb + 1) * D], cbf[b4], reads=[("cbf", b4)], writes=["uvb"])
    step = 0
    for h in range(8):
        g, a = h // 4, h % 4
        pr = slice(64 * g, 64 * g + 64)
        for qc in range(8):
            ob = (h * 8 + qc) % 2
            obank = 4 + ob
            sbs = {}

            def st_s(kc):
                nonlocal step
                if kc % 8 == 0:
                    conv_chunk()
                sb = step % 3
                step += 1
                sbs[kc] = sb
                k.op("pe", lambda e: e.matmul(
                    PS[sb][:, :], KT[pr, kc * 128:(kc + 1) * 128], QT[pr, a, qc * 512:(qc + 1) * 512],
                    start=True, stop=True), writes=["ps%d" % sb])
                k.op("act", lambda e: e.activation(
                    pT[sb], PS[sb][:, :], AF.Exp, scale=0.125, bias=negmb),
                    reads=["ps%d" % sb], writes=[("pT", sb)])

            def st_pv(kc):
                sb = sbs[kc]
                wr = ["ps%d" % obank] if kc in (0, NKC - 1) else []
                k.op("pe", lambda e: e.matmul(
                    PS[obank][0:65, :], VX[:, kc, g, :], pT[sb], start=(kc == 0), stop=(kc == NKC - 1)),
                    reads=[("pT", sb)], writes=wr)
            st_s(0)
            for kc in range(1, NKC):
                st_s(kc)
                st_pv(kc - 1)
            st_pv(NKC - 1)
            k.op("act", lambda e, ob=ob, obank=obank: e.copy(osb[ob][0:65, :], PS[obank][0:65, :]),
                 reads=["ps%d" % obank], writes=[("osb", ob)])
            k.op("pe", lambda e, ob=ob: e.matmul(PS[6][0:64, :], onesf[64:65, 0:64], osb[ob][64:65, :], start=True, stop=True),
                 reads=[("osb", ob), "onesf"], writes=["ps6"])
            k.op("dve", lambda e: e.reciprocal(rec[0:64, :], PS[6][0:64, :]), reads=["ps6"], writes=["rec"])
            k.op("dve", lambda e, ob=ob: e.tensor_tensor(aT[ob][0:64, :], osb[ob][0:64, :], rec[0:64, :], ALU.mult),
                 reads=["rec", ("osb", ob)], writes=[("aT", ob)])
            k.dma("pool", attnT_d[h, :, qc * 512:(qc + 1) * 512], aT[ob][0:64, :], reads=[("aT", ob)], writes=["attnT_d"])
    if "attn" in debug:
        af = A.alloc([128, 512], BF16)
        for h in range(8):
            k.dma("sp", af[0:64, :], attnT_d[h, :, 0:512], reads=["attnT_d"], writes=["af"])
            k.dma("sp", dbg_out("attnT%d" % h, [64, 512], BF16), af[0:64, :], reads=["af"])
    k.barrier()
    A.reset(P3)

    if stop == 'attn':
        return nc, IN, DBG, k
    A.reset(PERSIST)
    hy_sb = A.alloc([128, NT, HYC], BF16)
    PERSIST2 = A.off
    NF = 8192.0
    TWO_PI = 6.283185307179586

    def load_tab(dram, shape, dt, pieces=1):
        stg_ = A.alloc(shape, F32)
        tab = A.alloc(shape, dt) if dt != F32 else stg_
        flat = (lambda ap: ap if len(shape) == 2 else ap.rearrange("p a b -> p (a b)") if len(shape) == 3 else ap.rearrange("p a b c -> p (a b c)"))
        n = int(np.prod(shape[1:]))
        step = n // pieces
        for q in range(pieces):
            k.dma("sp", flat(stg_)[0:shape[0], q * step:(q + 1) * step], dram[:, q * step:(q + 1) * step], writes=[("tabstg", id(stg_), q)])
        if dt != F32:
            k.op("dve", lambda e: e.tensor_copy(flat(tab), flat(stg_)), reads=[("tabstg", id(stg_), q) for q in range(pieces)], writes=[("tab", id(tab))])
        return tab
    W1 = load_tab(W1_d, [128, 512], BF16)
    KC = load_tab(KC_d, [128, 128], BF16)
    KS = load_tab(KS_d, [128, 128], BF16)
    NKS = load_tab(NKS_d, [128, 128], BF16)
    R1 = load_tab(R1_d, [128, 256], BF16)
    R2 = load_tab(R2_d, [128, 256], BF16)
    C2 = load_tab(C2_d, [128, 2, 128], BF16)
    NS2 = load_tab(NS2_d, [128, 2, 128], BF16)
    TW1 = load_tab(TW1_d, [128, 768], F32)
    TW2 = load_tab(TW2_d, [128, 2, 3, 128], F32)
    skipN = load_tab(skipT_d, [128, 2, 128], F32)
    k.op("dve", lambda e: e.tensor_scalar(skipN, skipN, 1.0 / NF, None, ALU.mult),
         reads=[("tabstg", id(skipN), 0)], writes=["skipN"])
    k.barrier()
    PERSIST_T = A.off
    def fft_A(u_flat, rkeys, sl):
        k.op("pe", lambda e: e.matmul(PS[sl][:, :], u_flat, W1, start=True, stop=True), reads=rkeys, writes=["ps%d" % sl])
        t1 = ft1[sl]; t2 = ft2[sl]; ap_ = fap[sl]
        k.op("dve", lambda e: e.tensor_tensor(t1.rearrange("p (r f) -> p r f", r=2), PS[sl][:, :].rearrange("p (r f) -> p r f", r=2),
                                              bcast(TW1[:, 0:256].unsqueeze(1), [128, 2, 256]), ALU.mult),
             reads=["ps%d" % sl], writes=[("ft1", sl)])
        k.op("dve", lambda e: e.tensor_tensor(t2[:, 0:256], PS[sl][:, 256:512], TW1[:, 256:512], ALU.mult),
             reads=["ps%d" % sl], writes=[("ft2a", sl)])
        k.op("dve", lambda e: e.tensor_tensor(t2[:, 256:512], PS[sl][:, 0:256], TW1[:, 512:768], ALU.mult),
             reads=["ps%d" % sl], writes=[("ft2b", sl)])
        k.op("pool", lambda e: e.tensor_tensor(ap_, t1, t2, ALU.add),
             reads=[("ft1", sl), ("ft2a", sl), ("ft2b", sl)], writes=[("fap", sl)])

    def fft_B(sl, bb):
        ap_ = fap[sl]

        def f2(e):
            e.matmul(PS[bb][:, 0:256], KC, ap_[:, 0:256], start=True, stop=False)
            e.matmul(PS[bb][:, 0:256], KS, ap_[:, 256:512], start=False, stop=True)
            e.matmul(PS[bb][:, 256:512], KC, ap_[:, 256:512], start=True, stop=False)
            return e.matmul(PS[bb][:, 256:512], NKS, ap_[:, 0:256], start=False, stop=True)
        k.op("pe", f2, reads=[("fap", sl)], writes=["ps%d" % bb])
        return bb

    ft1 = [A.alloc([128, 512], F32) for _ in range(4)]
    ft2 = [A.alloc([128, 512], F32) for _ in range(4)]
    fap = [A.alloc([128, 512], BF16) for _ in range(4)]
    FWORK = A.off
    hA = A.alloc([128, L], F32)
    zT = A.alloc([128, L], F32)
    hB = A.alloc([128, L], F32)
    w123 = A.alloc([128, 3, 64], F32)
    bfr = A.alloc([128, 8], F32)
    for q in range(8):
        k.dma("sp", zT[0:33, q * 512:(q + 1) * 512], zT_d[:, q * 512:(q + 1) * 512], writes=[("zT", q)])
    k.dma("sp", w123[0:33, 0, :], hfw1_d, writes=["w1"])
    k.dma("sp", w123[0:64, 1, :], hfw2_d, writes=["w2"])
    k.dma("sp", w123[0:64, 2, :], hfw3_d, writes=["w3"])
    k.dma("sp", bfr[0:64, 0:4], hfb_d, writes=["bfr"])
    k.op("dve", lambda e: e.tensor_scalar(bfr[0:64, 4:5], bfr[0:64, 3:4], 1.0 / TWO_PI, None, ALU.mult), reads=["bfr"], writes=["bfr2"])
    ri_ = A.alloc([128, 512], I32)
    rr_ = A.alloc([128, 512], F32)
    srcs = [zT, hA, hB, hA]
    Ks_ = [33, 64, 64]
    for layer in range(3):
        src_, dst_ = srcs[layer], srcs[layer + 1]
        K_ = Ks_[layer]
        for q in range(8):
            bk = q % 2
            k.op("pe", lambda e: e.matmul(PS[bk][0:64, :], w123[0:K_, layer, :], src_[0:K_, q * 512:(q + 1) * 512], start=True, stop=True),
                 reads=[("zT", q), "w1", "w2", "w3", ("hm", layer, q)], writes=["ps%d" % bk])
            k.op("dve", lambda e: e.tensor_scalar(rr_[0:64, :], PS[bk][0:64, :], bfr[0:64, layer:layer + 1], bfr[0:64, 4:5], ALU.add, ALU.mult),
                 reads=["ps%d" % bk, "bfr", "bfr2"], writes=["rr"])
            k.op("dve", lambda e: e.tensor_copy(ri_[0:64, :], rr_[0:64, :]), reads=["rr"], writes=["ri"])
            k.op("dve", lambda e: e.tensor_tensor(rr_[0:64, :], rr_[0:64, :], ri_[0:64, :], ALU.subtract), reads=["rr", "ri"], writes=["rr"])
            k.op("act", lambda e: e.activation(dst_[0:64, q * 512:(q + 1) * 512], rr_[0:64, :], AF.Sin, scale=TWO_PI * 0.999999),
                 reads=["rr"], writes=[("hm", layer + 1, q)])
    k.barrier()
    A.reset(FWORK)
    hm3 = A.alloc([128, L], F32)
    w4s = A.alloc([128, 2048], F32)
    for q in range(4):
        k.dma("pool", w4s[0:64, q * 512:(q + 1) * 512], hfw4_d[:, q * 512:(q + 1) * 512], writes=[("w4", q)])
    t01bc = A.alloc([128, L], F32)
    for q in range(8):
        k.dma("sp", t01bc[:, q * 512:(q + 1) * 512], bcast(t01_d[:, q * 512:(q + 1) * 512], [128, 512]), writes=[("t01", q)])
    ndel = A.alloc([128, 4], F32)
    k.dma("sp", ndel, ndelT_d, writes=["ndel"])
    decay = A.alloc([128, L], F32)
    hraw = [A.alloc([128, L], F32) for _ in range(2)]
    junkb = A.alloc([128, L // 2], BF16)
    hf = [A.alloc([128, 32, 32, 4], BF16) for _ in range(2)]
    ssum = A.alloc([128, 8], F32)
    ffs = A.alloc([128, 512], F32)
    ktmp = A.alloc([128, 512], F32)
    kf3 = [A.alloc([128, 768], F32) for _ in range(2)]
    kfn = 0
    for cc in range(4):
        for q in range(8):
            k.op("act", lambda e, q=q: e.activation(decay[:, q * 512:(q + 1) * 512], t01bc[:, q * 512:(q + 1) * 512], AF.Exp, scale=ndel[:, cc:cc + 1]),
                 reads=[("t01", q), "ndel"], writes=[("decay", q)])
        for o in range(2):
            for dr in range(2):
                col0 = o * 1024 + dr * 512 + cc * 128
                for q in range(8):
                    bk = 4 + q % 2
                    k.op("pe", lambda e, q=q: e.matmul(PS[bk][:, :], w4s[0:64, col0:col0 + 128], hm3[0:64, q * 512:(q + 1) * 512], start=True, stop=True),
                         reads=[("w4", col0 // 512)], writes=["ps%d" % bk])
                    k.op("dve", lambda e, q=q: e.tensor_tensor(hraw[dr][:, q * 512:(q + 1) * 512], PS[bk][:, :], decay[:, q * 512:(q + 1) * 512], ALU.mult),
                         reads=["ps%d" % bk, ("decay", q)], writes=[("hraw", dr, q)])
                if dr == 1:
                    k.op("dve", lambda e: e.memset(hraw[1][:, 0:1], 0.0), reads=[("hraw", 1, 0)], writes=[("hraw", 1, 0)])
                for hh_ in range(2):
                    k.op("act", lambda e, dr=dr, hh_=hh_: e.activation(junkb, hraw[dr][:, hh_ * 2048:(hh_ + 1) * 2048], AF.Abs,
                                                                    accum_out=ssum[:, 4 + 2 * dr + hh_:5 + 2 * dr + hh_]),
                         reads=[("hraw", dr, q) for q in range(8)], writes=["junkb", ("ssum", dr, hh_)])
            k.op("dve", lambda e: e.tensor_reduce(ssum[:, 0:1], ssum[:, 4:8], AX.X, ALU.add),
                 reads=[("ssum", d_, h_) for d_ in range(2) for h_ in range(2)], writes=["ssum0"])
            k.op("dve", lambda e: e.tensor_scalar(ssum[:, 2:3], ssum[:, 0:1], EPS, None, ALU.add), reads=["ssum0"], writes=["ssum2"])
            k.op("dve", lambda e: e.reciprocal(ssum[:, 3:4], ssum[:, 2:3]), reads=["ssum2"], writes=["ssum3"])
            for dr in range(2):
                for q in range(8):
                    k.op("act", lambda e, dr=dr, q=q: e.activation(hraw[dr][:, q * 512:(q + 1) * 512], hraw[dr][:, q * 512:(q + 1) * 512], AF.Identity, scale=ssum[:, 3:4]),
                         reads=["ssum3", "junkb", ("hraw", dr, q)], writes=[("hraw", dr, q)])
                hv = hraw[dr].rearrange("p (a i) -> p a i", i=32)
                for i4 in range(8):
                    bk = 6 + i4 % 2

                    def trf(e, i4=i4, bk=bk, hv=hv):
                        ins = None
                        for j in range(4):
                            ins = e.transpose(PS[bk][:, j * 128:(j + 1) * 128], hv[:, :, i4 * 4 + j], ident)
                        return ins
                    k.op("pe", trf, reads=[("hraw", dr, q) for q in range(8)] + ["ident"], writes=["ps%d" % bk])
                    k.op("act", lambda e, i4=i4, bk=bk, dr=dr: e.copy(
                        hf[dr][:, :, i4 * 4:(i4 + 1) * 4, :].rearrange("p g i c -> p i g c"),
                        PS[bk][:, :].rearrange("p (i g c) -> p i g c", i=4, g=32)),
                        reads=["ps%d" % bk], writes=[("hf", dr, i4)])
            hkeys0 = [("hf", 0, i4) for i4 in range(8)]
            hkeys1 = [("hf", 1, i4) for i4 in range(8)]

            def stA(g):
                sl = 2 * (g % 2)
                fft_A(hf[0][:, g, :, :].rearrange("p i c -> p (i c)"), hkeys0, sl)
                fft_A(hf[1][:, g, :, :].rearrange("p i c -> p (i c)"), hkeys1, sl + 1)

            def stB(g):
                nonlocal kfn
                G = cc * 32 + g
                sl = 2 * (g % 2)
                bf_ = fft_B(sl, 4 + sl)
                k.op("act", lambda e: e.copy(ffs, PS[bf_][:, :]), reads=["ps%d" % bf_], writes=["ffs"])
                bb_ = fft_B(sl + 1, 5 + sl)
                kk_ = kf3[kfn % 2]; kfn += 1
                k.op("dve", lambda e: e.tensor_tensor(ktmp[:, 0:256], PS[bb_][:, 0:256], ffs[:, 0:256], ALU.add),
                     reads=["ps%d" % bb_, "ffs"], writes=["ktmpa"])
                k.op("dve", lambda e: e.tensor_tensor(ktmp[:, 256:512], ffs[:, 256:512], PS[bb_][:, 256:512], ALU.subtract),
                     reads=["ps%d" % bb_, "ffs"], writes=["ktmpb"])
                k.op("dve", lambda e: e.tensor_scalar(kk_[:, 0:256], ktmp[:, 0:256], 1.0 / NF, skipN[:, o, G:G + 1], ALU.mult, ALU.add),
                     reads=["ktmpa", "skipN"], writes=[("kf3a", kfn % 2)])
                k.op("act", lambda e: e.activation(kk_[:, 256:512], ktmp[:, 256:512], AF.Identity, scale=-1.0 / NF),
                     reads=["ktmpb"], writes=[("kf3b", kfn % 2)])
                k.op("act", lambda e: e.activation(kk_[:, 512:768], ktmp[:, 256:512], AF.Identity, scale=1.0 / NF),
                     reads=["ktmpb"], writes=[("kf3c", kfn % 2)])
                k.dma("sp", kf_d[o, G, :, :], kk_, reads=[("kf3a", kfn % 2), ("kf3b", kfn % 2), ("kf3c", kfn % 2)], writes=[("kfd", o, G)])
            stA(0)
            for g in range(1, 32):
                stA(g)
                stB(g - 1)
            stB(31)
    if "kf" in debug:
        k.barrier()
        for o in range(2):
            for G in (0, 77):
                dk_ = dbg_out("kf_%d_%d" % (o, G), [128, 768])
                k.dma("sp", kf3[0], kf_d[o, G, :, :], writes=["kkdbg"])
                k.dma("sp", dk_, kf3[0], reads=["kkdbg"], writes=["kkdbg2"])
                k.barrier()
    k.barrier()
    if stop == 'filt':
        return nc, IN, DBG, k
    A.reset(PERSIST_T)
    ft1 = [A.alloc([128, 512], F32) for _ in range(4)]
    ft2 = [A.alloc([128, 512], F32) for _ in range(4)]
    fap = [A.alloc([128, 512], BF16) for _ in range(4)]

    cw = A.alloc([128, 3, 3 * HYC], F32)
    cb = A.alloc([128, 3 * HYC], F32)
    for j in range(3):
        for hh in range(3):
            k.dma("sp", cw[:, j, hh * 512:(hh + 1) * 512], bcast(hcw_d[j:j + 1, hh * 512:(hh + 1) * 512], [128, 512]), writes=[("cw", j, hh)])
    for hh in range(3):
        k.dma("sp", cb[:, hh * 512:(hh + 1) * 512], bcast(hcb_d[:, hh * 512:(hh + 1) * 512], [128, 512]), writes=[("cb", hh)])
    k.barrier()
    CH = 64
    Zb = [A.alloc([128, 34, CH], F32) for _ in range(3)]
    zt_ = A.alloc([128, 32, CH], F32)
    zu_ = A.alloc([128, 32, CH], F32)
    xg = [A.alloc([128, 32, CH], F32) for _ in range(2)]
    u1 = A.alloc([128, 16, 32, 4], BF16)
    u2 = A.alloc([128, 16, 32, 4], BF16)
    kfb = [A.alloc([128, 768], F32) for _ in range(3)]
    pt1 = [A.alloc([128, 512], F32) for _ in range(2)]
    pt2 = [A.alloc([128, 512], F32) for _ in range(2)]
    ysb = [A.alloc([128, 512], BF16) for _ in range(2)]
    it1 = [A.alloc([128, 2, 2, 128], F32) for _ in range(2)]
    it2 = [A.alloc([128, 2, 2, 128], F32) for _ in range(2)]
    Bbuf = [A.alloc([128, 2, 2, 4, 128], BF16) for _ in range(2)]
    zmain = zhy_d[0:L, :].rearrange("(p j) c -> p j c", j=32)
    zext = zhy_d[2:L + 2, :].rearrange("(p j) c -> p j c", j=32)
    cnt = [0]

    def conv(u_in, ukey, o, G0, gate, gkey, writer):
        base = cnt[0]
        cnt[0] += 16

        def stA(g):
            n = base + g
            k.dma("sp", kfb[n % 3], kf_d[o, G0 + g, :, :], reads=[("kfd", o, G0 + g)], writes=[("kfb", n % 3)])
            fft_A(u_in[:, g, :, :].rearrange("p i c -> p (i c)"), [ukey], n % 2)

        def stB(g):
            n = base + g
            kfbuf = kfb[n % 3]
            bu = fft_B(n % 2, 2 + n % 2)
            a1 = pt1[n % 2]; a2 = pt2[n % 2]; ys = ysb[n % 2]
            k.op("dve", lambda e: e.tensor_tensor(a1.rearrange("p (r f) -> p r f", r=2), PS[bu][:, :].rearrange("p (r f) -> p r f", r=2),
                                                  bcast(kfbuf[:, 0:256].unsqueeze(1), [128, 2, 256]), ALU.mult),
                 reads=["ps%d" % bu, ("kfb", n % 3)], writes=[("pt1", n % 2)])
            k.op("dve", lambda e: e.tensor_tensor(a2[:, 0:256], PS[bu][:, 256:512], kfbuf[:, 256:512], ALU.mult),
                 reads=["ps%d" % bu, ("kfb", n % 3)], writes=[("pt2a", n % 2)])
            k.op("dve", lambda e: e.tensor_tensor(a2[:, 256:512], PS[bu][:, 0:256], kfbuf[:, 512:768], ALU.mult),
                 reads=["ps%d" % bu, ("kfb", n % 3)], writes=[("pt2b", n % 2)])
            k.op("pool", lambda e: e.tensor_tensor(ys, a1, a2, ALU.add),
                 reads=[("pt1", n % 2), ("pt2a", n % 2), ("pt2b", n % 2)], writes=[("ysb", n % 2)])

        def stC(g):
            n = base + g
            ys = ysb[n % 2]
            bi = 4 + n % 2

            def i1(e):
                ins = None
                for hh in range(2):
                    e.matmul(PS[bi][:, hh * 256:(hh + 1) * 256], ys[:, hh * 128:(hh + 1) * 128], R1, start=True, stop=False)
                    ins = e.matmul(PS[bi][:, hh * 256:(hh + 1) * 256], ys[:, 256 + hh * 128:256 + (hh + 1) * 128], R2, start=False, stop=True)
                return ins
            k.op("pe", i1, reads=[("ysb", n % 2)], writes=["ps%d" % bi])
            b1 = it1[n % 2]; b2 = it2[n % 2]
            Bv = PS[bi][:, :].rearrange("p (h r x) -> p h r x", h=2, r=2)
            k.op("dve", lambda e: e.tensor_tensor(b1, Bv, bcast(TW2[:, :, 0:1, :], [128, 2, 2, 128]), ALU.mult),
                 reads=["ps%d" % bi], writes=[("it1", n % 2)])
            k.op("dve", lambda e: e.tensor_tensor(b2[:, :, 0, :], Bv[:, :, 1, :], TW2[:, :, 1, :], ALU.mult),
                 reads=["ps%d" % bi], writes=[("it2a", n % 2)])
            k.op("dve", lambda e: e.tensor_tensor(b2[:, :, 1, :], Bv[:, :, 0, :], TW2[:, :, 2, :], ALU.mult),
                 reads=["ps%d" % bi], writes=[("it2b", n % 2)])
            q4, slot = (n // 4), n % 4
            Bb = Bbuf[q4 % 2]
            k.op("pool", lambda e: e.tensor_tensor(Bb[:, :, :, slot, :], b1, b2, ALU.add),
                 reads=[("it1", n % 2), ("it2a", n % 2), ("it2b", n % 2)], writes=[("Bbuf", q4 % 2, slot)])
            if slot == 3:
                bo = 6 + q4 % 2

                def i2(e):
                    e.matmul(PS[bo][:, :], C2[:, 0, :], Bb[:, 0, 0, :, :].rearrange("p s x -> p (s x)"), start=True, stop=False)
                    e.matmul(PS[bo][:, :], NS2[:, 0, :], Bb[:, 0, 1, :, :].rearrange("p s x -> p (s x)"), start=False, stop=False)
                    e.matmul(PS[bo][:, :], C2[:, 1, :], Bb[:, 1, 0, :, :].rearrange("p s x -> p (s x)"), start=False, stop=False)
                    return e.matmul(PS[bo][:, :], NS2[:, 1, :], Bb[:, 1, 1, :, :].rearrange("p s x -> p (s x)"), start=False, stop=True)
                k.op("pe", i2, reads=[("Bbuf", q4 % 2, s_) for s_ in range(4)], writes=["ps%d" % bo])
                gq = g // 4
                yv = PS[bo][:, :].rearrange("p (s i c) -> p s i c", s=4, i=32)
                gv = gate[:, :, gq * 16:(gq + 1) * 16].rearrange("p i (s c) -> p s i c", s=4)
                writer(gq, yv, gv, "ps%d" % bo, gkey)
        for st in range(18):
            if st < 16:
                stA(st)
            if 1 <= st <= 16:
                stB(st - 1)
            if 2 <= st <= 17:
                stC(st - 2)

    for chn in range(HYC // CH):
        ch0 = chn * CH
        G0 = chn * 16
        for kind in range(3):
            col0 = kind * HYC + ch0
            k.dma("sp", Zb[kind][:, 0:16, :], zmain[:, 0:16, col0:col0 + CH], writes=[("Zb", kind, 0)])
            k.dma("sp", Zb[kind][:, 16:32, :], zmain[:, 16:32, col0:col0 + CH], writes=[("Zb", kind, 1)])
            k.dma("sp", Zb[kind][:, 32:34, :], zext[:, 30:32, col0:col0 + CH], writes=[("Zb", kind, 2)])
            zk = [("Zb", kind, j) for j in range(3)]
            wv = lambda j: bcast(cw[:, j:j + 1, col0:col0 + CH], [128, 32, CH])
            k.op("dve", lambda e: e.tensor_tensor(zt_, Zb[kind][:, 0:32, :], wv(0), ALU.mult), reads=zk, writes=["zt"])
            k.op("pool", lambda e: e.tensor_tensor(zu_, Zb[kind][:, 1:33, :], wv(1), ALU.mult), reads=zk, writes=["zu"])
            k.op("dve", lambda e: e.tensor_tensor(zt_, zt_, zu_, ALU.add), reads=["zt", "zu"], writes=["zt"])
            k.op("pool", lambda e: e.tensor_tensor(zu_, Zb[kind][:, 2:34, :], wv(2), ALU.mult), reads=zk + ["zt"], writes=["zu"])
            k.op("dve", lambda e: e.tensor_tensor(zt_, zt_, zu_, ALU.add), reads=["zt", "zu"], writes=["zt"])
            bv = bcast(cb[:, col0:col0 + CH].unsqueeze(1), [128, 32, CH])
            if kind == 0:
                k.op("dve", lambda e: e.tensor_tensor(u1.rearrange("p g i c -> p i g c"), zt_.rearrange("p i (g c) -> p i g c", c=4),
                                                      bv.rearrange("p i (g c) -> p i g c", c=4) if False else bcast(cb[:, col0:col0 + CH].rearrange("p (g c) -> p g c", c=4).unsqueeze(1), [128, 32, 16, 4]), ALU.add),
                     reads=["zt"], writes=["u1"])
            else:
                k.op("dve", lambda e: e.tensor_tensor(xg[kind - 1], zt_, bv, ALU.add), reads=["zt"], writes=[("xg", kind - 1)])

        def w1_(gq, yv, gv, pkey, gkey):
            k.op("dve", lambda e: e.tensor_tensor(u2[:, gq * 4:(gq + 1) * 4, :, :], yv, gv, ALU.mult), reads=[pkey, gkey], writes=["u2"])

        def w2_(gq, yv, gv, pkey, gkey):
            ov = hy_sb[:, :, ch0 + gq * 16:ch0 + (gq + 1) * 16].rearrange("p i (s c) -> p s i c", s=4)
            k.op("dve", lambda e: e.tensor_tensor(ov, yv, gv, ALU.mult), reads=[pkey, gkey], writes=[("hy", chn, gq)])
        conv(u1, "u1", 0, G0, xg[0], ("xg", 0), w1_)
        conv(u2, "u2", 1, G0, xg[1], ("xg", 1), w2_)
    if "hy" in debug:
        k.barrier()
        hyf = A.alloc([128, 4, HYC], F32) if False else pt1[0]
        for i_ in range(4):
            k.op("dve", lambda e, i_=i_: e.tensor_copy(hyf, hy_sb[:, i_, :]), writes=["hyf"])
            k.dma("sp", dbg_out("hy%d" % i_, [128, HYC]), hyf, reads=["hyf"], writes=["hyfd"])
            k.barrier()
    k.barrier()
    if stop == 'hy':
        return nc, IN, DBG, k
    A.reset(PERSIST2)
    wao = A.alloc([128, 4, D], BF16)
    wout = A.alloc([128, 8, D], BF16)
    why = A.alloc([128, 4, D], BF16)
    wqb = A.alloc([128, 8, 2048], BF16)
    keysT = A.alloc([128, 16, 128], BF16)
    g1bc = A.alloc([128, D], F32)
    g2bc = A.alloc([128, D], F32)
    A2bc = A.alloc([128, D], BF16)
    B2bc = A.alloc([128, D], BF16)
    gfin = A.alloc([128, D], F32)
    iota16 = A.alloc([128, 16], F32)
    PB = A.off
    rep = A.alloc([128, 128], F32)
    stg = [A.alloc([128, D], F32) for _ in range(2)]
    io_ = A.alloc([128, 16], I32)
    k.op("pool", lambda e: e.iota(io_, pattern=[[1, 16]], base=0, channel_multiplier=0), writes=["io_"])
    k.op("dve", lambda e: e.tensor_copy(iota16, io_), reads=["io_"], writes=["iota16"])
    k.dma("sp", gfin, bcast(gfin_d, [128, D]), writes=["gfin"])

    def load_w(dst_fn, src_fn, n):
        for j in range(n):
            b = j % 2
            k.dma("sp" if b == 0 else "pool", stg[b], src_fn(j), writes=[("stg", b)])
            if b == 0:
                k.op("act", lambda e, j=j: e.copy(dst_fn(j), stg[0]), reads=[("stg", 0)], writes=[("wload", id(dst_fn), j)])
            else:
                k.op("dve", lambda e, j=j: e.tensor_copy(dst_fn(j), stg[1]), reads=[("stg", 1)], writes=[("wload", id(dst_fn), j)])
    load_w(lambda j: wao[:, j, :], lambda j: wao_d[j * 128:(j + 1) * 128, :], 4)
    load_w(lambda j: wout[:, j, :], lambda j: wout_d[j * 128:(j + 1) * 128, :], 8)
    load_w(lambda j: why[:, j, :], lambda j: why_d[j * 128:(j + 1) * 128, :], 4)
    load_w(lambda j: wqb[:, j // 2, (j % 2) * 1024:(j % 2 + 1) * 1024], lambda j: wq_d[(j // 2) * 128:(j // 2 + 1) * 128, (j % 2) * 1024:(j % 2 + 1) * 1024], 16)
    load_w(lambda j: keysT[:, j * 8:(j + 1) * 8, :].rearrange("p a n -> p (a n)"), lambda j: keysT_d[:, j * 1024:(j + 1) * 1024], 2)
    bcl = [(g1bc, lambda j: modT[:, 16 + j, 0:1]), (g2bc, lambda j: modT[:, 40 + j, 0:1]),
           (A2bc, lambda j: A2[:, j:j + 1]), (B2bc, lambda j: modT[:, 24 + j, 0:1])]
    for bi_, (dst, colf) in enumerate(bcl):
        for j in range(8):
            k.op("dve", lambda e, j=j, colf=colf: e.tensor_copy(rep, bcast(colf(j), [128, 128])), reads=["modT", "A2"], writes=["rep"])
            k.op("pe", lambda e, j=j: e.matmul(PS[j // 4][:, (j % 4) * 128:(j % 4 + 1) * 128], rep, ident, start=True, stop=True),
                 reads=["rep", "ident"], writes=["ps%d" % (j // 4)])
        for hf in range(2):
            k.op("act", lambda e, hf=hf, dst=dst: e.copy(dst[:, hf * 512:(hf + 1) * 512], PS[hf][:, :]), reads=["ps%d" % hf], writes=[("bc", bi_, hf)])
    k.barrier()
    A.reset(PB)
    aTt = A.alloc([128, 4, 128], BF16)
    gsb = A.alloc([128, 2 * D], BF16)
    xs = A.alloc([128, D], F32)
    mg = A.alloc([128, D], F32)
    mT = A.alloc([128, 8, 128], BF16)
    x1 = A.alloc([128, D], F32)
    ob = A.alloc([128, D], F32)
    hyf = A.alloc([128, HYC], F32)
    hyT = A.alloc([128, 4, 128], BF16)
    sB = A.alloc([128, 16], F32)
    qTs = A.alloc([128, 16, 128], BF16)
    big8 = A.alloc([128, 2048], F32)
    mx = A.alloc([128, 16, 16], F32)
    mi = A.alloc([128, 16, 16], U32)
    mif = A.alloc([128, 16, 16], F32)
    wk4 = A.alloc([128, 4, 256], F32)
    best = A.alloc([128, 8, 16], F32)
    pos = A.alloc([128, 8, 16], U32)
    pia = A.alloc([128, 8, 16], U32)
    pib = A.alloc([128, 8, 16], U32)
    paf = A.alloc([128, 8, 16], F32)
    pbf = A.alloc([128, 8, 16], F32)
    isel = A.alloc([128, 2, 128], F32)
    eidf = A.alloc([128, 128], F32)
    eidx = A.alloc([128, 128], I32)
    gw = A.alloc([128, 8, 16], F32)
    sm8 = A.alloc([128, 16], F32)
    av = A.alloc([128, 128], F32)
    wgt = A.alloc([128, 128], F32)
    junkp = ob
    acc = xs
    gl = A.alloc([128, 128], F32)
    dg = [A.alloc([128, 128], BF16) for _ in range(4)]
    identb = A.alloc([128, 128], BF16)
    k.op("dve", lambda e: e.tensor_copy(identb, ident), reads=["ident"], writes=["identb"])
    NR = 8
    ring = [A.alloc([128, 2 * D], BF16) for _ in range(NR)]
    out_v = out_d.rearrange("(p i) d -> i p d", i=NT)
    attn_ev = attnT_d.rearrange("(j two) d t -> two d j t", two=2)
    rn = [0]
    sc3 = big8.rearrange("p (a n) -> p a n", a=16)
    cand = big8.rearrange("p (h a b) -> p h a b", h=8, a=16)
    mxv = mx.rearrange("p (h t) k -> p h t k", t=2)
    mifv = mif.rearrange("p (h t) k -> p h t k", t=2)
    npe = NT if "nopeer" not in skip else 0
    GW = 512 if "halfrow" in skip else D
    for it in range(NT):
        k.dma("sp", xs, x_v[it], writes=["xs"])
        for two in range(2):
            k.dma("pool", aTt[64 * two:64 * two + 64, :, :], attn_ev[two][:, :, it * 128:(it + 1) * 128], writes=[("aTt", two)])
        k.dma("sp", gsb, gates_v[it], writes=["gsb"])
        k.op("act", lambda e: e.copy(hyf, hy_sb[:, it, :]), writes=["hyf"])

        def trh(e):
            ins = None
            for j in range(4):
                ins = e.transpose(PS[6][:, j * 128:(j + 1) * 128], hyf[:, j * 128:(j + 1) * 128], ident)
            return ins
        k.op("pe", trh, reads=["hyf", "ident"], writes=["ps6"])
        k.op("act", lambda e: e.copy(hyT, PS[6][:, :].rearrange("p (j t) -> p j t", j=4)), reads=["ps6"], writes=["hyT"])
        for hf in range(2):
            def pa(e, hf=hf):
                ins = None
                for j in range(4):
                    ins = e.matmul(PS[hf][:, :], aTt[:, j, :], wao[:, j, hf * 512:(hf + 1) * 512], start=(j == 0), stop=(j == 3))
                return ins
            k.op("pe", pa, reads=[("aTt", 0), ("aTt", 1)], writes=["ps%d" % hf])
            k.op("dve", lambda e, hf=hf: e.tensor_tensor(mg[:, hf * 512:(hf + 1) * 512], PS[hf][:, :], gsb[:, hf * 512:(hf + 1) * 512], ALU.mult),
                 reads=["ps%d" % hf, "gsb"], writes=[("mg", hf)])

            def ph(e, hf=hf):
                ins = None
                for j in range(4):
                    ins = e.matmul(PS[6 + hf][:, :], hyT[:, j, :], why[:, j, hf * 512:(hf + 1) * 512], start=(j == 0), stop=(j == 3))
                return ins
            k.op("pe", ph, reads=["hyT"], writes=["ps%d" % (6 + hf)])
            k.op("dve", lambda e, hf=hf: e.tensor_tensor(ob[:, hf * 512:(hf + 1) * 512], PS[6 + hf][:, :], gsb[:, D + hf * 512:D + (hf + 1) * 512], ALU.mult),
                 reads=["ps%d" % (6 + hf), "gsb"], writes=[("ob", hf)])
            k.op("pool", lambda e, hf=hf: e.tensor_tensor(mg[:, hf * 512:(hf + 1) * 512], mg[:, hf * 512:(hf + 1) * 512], ob[:, hf * 512:(hf + 1) * 512], ALU.add),
                 reads=[("mg", hf), ("ob", hf)], writes=[("mg", hf)])
        for hf in range(2):
            def trm(e, hf=hf):
                ins = None
                for j in range(4):
                    jj = hf * 4 + j
                    ins = e.transpose(PS[2 + hf][:, j * 128:(j + 1) * 128], mg[:, jj * 128:(jj + 1) * 128], ident)
                return ins
            k.op("pe", trm, reads=[("mg", 0), ("mg", 1)], writes=["ps%d" % (2 + hf)])
            k.op("act", lambda e, hf=hf: e.copy(mT[:, hf * 4:(hf + 1) * 4, :], PS[2 + hf][:, :].rearrange("p (j t) -> p j t", j=4)),
                 reads=["ps%d" % (2 + hf)], writes=[("mT", hf)])
        for hf in range(2):
            def mo(e, hf=hf):
                ins = None
                for kk in range(8):
                    ins = e.matmul(PS[4 + hf][:, :], mT[:, kk, :], wout[:, kk, hf * 512:(hf + 1) * 512], start=(kk == 0), stop=(kk == 7))
                return ins
            k.op("pe", mo, reads=[("mT", 0), ("mT", 1)], writes=["ps%d" % (4 + hf)])
            k.op("dve", lambda e, hf=hf: e.tensor_tensor(x1[:, hf * 512:(hf + 1) * 512], PS[4 + hf][:, :], g1bc[:, hf * 512:(hf + 1) * 512], ALU.mult),
                 reads=["ps%d" % (4 + hf)], writes=[("x1", hf)])
        k.op("pool", lambda e: e.tensor_tensor(x1, x1, xs, ALU.add), reads=[("x1", 0), ("x1", 1), "xs"], writes=["x1s"])
        if "x1" in debug and it < 4:
            k.dma("sp", dbg_out("x1_%d" % it, [128, D]), x1, reads=["x1s"])
        s = sB
        if it < npe:
            k.op("act", lambda e: e.activation(mg, x1, AF.Square, accum_out=s[:, 4:5]), reads=["x1s", ("mg", 0), ("mg", 1)], writes=["mgx", "sB2"])
            k.op("dve", lambda e: e.tensor_scalar(s[:, 5:6], s[:, 4:5], 1.0 / D, EPS, ALU.mult, ALU.add), reads=["sB2"], writes=["sB2"])
            k.op("act", lambda e: e.activation(s[:, 6:7], s[:, 5:6], AF.Sqrt), reads=["sB2"], writes=["sB2"])
            k.op("dve", lambda e: e.reciprocal(s[:, 7:8], s[:, 6:7]), reads=["sB2"], writes=["sB2"])
            k.op("act", lambda e: e.activation(mg, x1, AF.Identity, scale=s[:, 7:8]), reads=["x1s", "sB2", "mgx"], writes=["mgx"])
            for hf in range(2):
                def tr2(e, hf=hf):
                    ins = None
                    for j in range(4):
                        jj = hf * 4 + j
                        ins = e.transpose(PS[hf][:, j * 128:(j + 1) * 128], mg[:, jj * 128:(jj + 1) * 128], ident)
                    return ins
                k.op("pe", tr2, reads=["mgx"], writes=["ps%d" % hf])
            for jj in range(8):
                hf, j = jj // 4, jj % 4
                if jj % 2 == 0:
                    k.op("act", lambda e, jj=jj, hf=hf, j=j: e.activation(mT[:, jj, :], PS[hf][:, j * 128:(j + 1) * 128], AF.Identity,
                                                                       scale=A2[:, jj:jj + 1], bias=modT[:, 24 + jj, 0:1]),
                         reads=["ps%d" % hf, ("mT", 0), ("mT", 1)], writes=[("h2T", jj)])
                else:
                    k.op("dve", lambda e, jj=jj, hf=hf, j=j: e.tensor_scalar(mT[:, jj, :], PS[hf][:, j * 128:(j + 1) * 128],
                                                                         A2[:, jj:jj + 1], modT[:, 24 + jj, 0:1], ALU.mult, ALU.add),
                         reads=["ps%d" % hf, ("mT", 0), ("mT", 1)], writes=[("h2T", jj)])
            k.op("dve", lambda e: e.tensor_tensor(mg, mg, A2bc, ALU.mult), reads=["mgx"], writes=["mgx"])
            k.op("pool", lambda e: e.tensor_tensor(mg, mg, B2bc, ALU.add), reads=["mgx"], writes=["h2"])
            h2keys = [("h2T", jj) for jj in range(8)]
            for qb in range(4):
                def qmm(e, qb=qb):
                    ins = None
                    for q4 in range(4):
                        hh = qb * 4 + q4
                        for kk in range(8):
                            ins = e.matmul(PS[2 + qb][:, q4 * 128:(q4 + 1) * 128], wqb[:, kk, hh * 128:(hh + 1) * 128], mT[:, kk, :],
                                           start=(kk == 0), stop=(kk == 7))
                    return ins
                k.op("pe", qmm, reads=h2keys, writes=["ps%d" % (2 + qb)])
                k.op("act", lambda e, qb=qb: e.copy(qTs[:, qb * 4:(qb + 1) * 4, :], PS[2 + qb][:, :].rearrange("p (a t) -> p a t", a=4)),
                     reads=["ps%d" % (2 + qb)], writes=[("qTs", qb)])
            sbanks = [6, 7, 0, 1]
            for qb in range(4):
                bk = sbanks[qb]

                def smm(e, qb=qb, bk=bk):
                    ins = None
                    for q4 in range(4):
                        hh = qb * 4 + q4
                        ins = e.matmul(PS[bk][:, q4 * 128:(q4 + 1) * 128], qTs[:, hh, :], keysT[:, hh, :], start=True, stop=True)
                    return ins
                k.op("pe", smm, reads=[("qTs", qb)], writes=["ps%d" % bk])
                if qb % 2 == 0:
                    k.op("act", lambda e, qb=qb, bk=bk: e.copy(big8[:, qb * 512:(qb + 1) * 512], PS[bk][:, :]), reads=["ps%d" % bk, "big8"], writes=[("sc", qb)])
                else:
                    k.op("dve", lambda e, qb=qb, bk=bk: e.tensor_copy(big8[:, qb * 512:(qb + 1) * 512], PS[bk][:, :]), reads=["ps%d" % bk, "big8"], writes=[("sc", qb)])
            for q4 in range(4):
                hhs = [4 * q4 + u for u in range(4)]
                sk_ = [("sc", q4)]
                for hh in hhs:
                    k.op("dve", lambda e, hh=hh: e.max(mx[:, hh, 0:8], sc3[:, hh, :]), reads=sk_, writes=[("mx", hh)])
                for hh in hhs:
                    k.op("dve", lambda e, hh=hh: e.max_index(mi[:, hh, 0:8], mx[:, hh, 0:8], sc3[:, hh, :]), reads=sk_ + [("mx", hh)], writes=[("mi", hh)])
                for u, hh in enumerate(hhs):
                    k.op("dve", lambda e, hh=hh, u=u: e.match_replace(wk4[:, u, 0:128], mx[:, hh, 0:8], sc3[:, hh, :], -1e30), reads=sk_ + [("mx", hh)], writes=[("wk", u)])
                for u, hh in enumerate(hhs):
                    k.op("dve", lambda e, hh=hh, u=u: e.max(mx[:, hh, 8:16], wk4[:, u, 0:128]), reads=[("wk", u)], writes=[("mx", hh)])
                for u, hh in enumerate(hhs):
                    k.op("dve", lambda e, hh=hh, u=u: e.max_index(mi[:, hh, 8:16], mx[:, hh, 8:16], wk4[:, u, 0:128]), reads=[("wk", u), ("mx", hh)], writes=[("mi", hh)])
            mxk = [("mx", hh) for hh in range(16)]
            mik = [("mi", hh) for hh in range(16)]
            k.op("dve", lambda e: e.tensor_copy(mif, mi), reads=mik, writes=["mif"])
            k.op("dve", lambda e: e.tensor_tensor(cand, bcast(mxv[:, :, 0, :].unsqueeze(3), [128, 8, 16, 16]),
                                                  bcast(mxv[:, :, 1, :].unsqueeze(2), [128, 8, 16, 16]), ALU.add),
                 reads=mxk + [("sc", q) for q in range(4)], writes=["big8"])
            candf = big8.rearrange("p (h x) -> p h x", h=8)
            for q4 in range(2):
                hs = [4 * q4 + u for u in range(4)]
                for h in hs:
                    k.op("dve", lambda e, h=h: e.max(best[:, h, 0:8], candf[:, h, :]), reads=["big8"], writes=[("best", h)])
                for h in hs:
                    k.op("dve", lambda e, h=h: e.max_index(pos[:, h, 0:8], best[:, h, 0:8], candf[:, h, :]), reads=["big8", ("best", h)], writes=[("pos", h)])
                for u, h in enumerate(hs):
                    k.op("dve", lambda e, h=h, u=u: e.match_replace(wk4[:, u, :], best[:, h, 0:8], candf[:, h, :], -1e30), reads=["big8", ("best", h)], writes=[("wk", u)])
                for u, h in enumerate(hs):
                    k.op("dve", lambda e, h=h, u=u: e.max(best[:, h, 8:16], wk4[:, u, :]), reads=[("wk", u)], writes=[("best", h)])
                for u, h in enumerate(hs):
                    k.op("dve", lambda e, h=h, u=u: e.max_index(pos[:, h, 8:16], best[:, h, 8:16], wk4[:, u, :]), reads=[("wk", u), ("best", h)], writes=[("pos", h)])
            bk_ = [("best", h) for h in range(8)]
            pk_ = [("pos", h) for h in range(8)]
            k.op("dve", lambda e: e.tensor_scalar(pia, pos, 4, None, ALU.arith_shift_right), reads=pk_, writes=["pia"])
            k.op("dve", lambda e: e.tensor_scalar(pib, pos, 15, None, ALU.bitwise_and), reads=pk_, writes=["pib"])
            k.op("dve", lambda e: e.tensor_copy(paf, pia), reads=["pia"], writes=["paf"])
            k.op("dve", lambda e: e.tensor_copy(pbf, pib), reads=["pib"], writes=["pbf"])
            oh = big8.rearrange("p (h a b) -> p h a b", h=8, a=16)
            for t_, pf_ in enumerate((paf, pbf)):
                k.op("dve", lambda e, pf_=pf_: e.tensor_tensor(oh, bcast(pf_.unsqueeze(3), [128, 8, 16, 16]),
                                                               bcast(iota16.unsqueeze(1).unsqueeze(1), [128, 8, 16, 16]), ALU.is_equal),
                     reads=["paf", "pbf", "iota16", "big8"] + bk_ + pk_, writes=["big8"])
                k.op("dve", lambda e, t_=t_: e.tensor_tensor(oh, oh, bcast(mifv[:, :, t_, :].unsqueeze(2), [128, 8, 16, 16]), ALU.mult),
                     reads=["big8", "mif"], writes=["big8"])
                k.op("dve", lambda e, t_=t_: e.tensor_reduce(isel[:, t_, :].rearrange("p (h a) -> p h a", h=8), oh, AX.X, ALU.add),
                     reads=["big8"], writes=[("isel", t_)])
            k.op("dve", lambda e: e.scalar_tensor_tensor(eidf, isel[:, 0, :], 128.0, isel[:, 1, :], ALU.mult, ALU.add),
                 reads=[("isel", 0), ("isel", 1)], writes=["eidf"])
            k.op("dve", lambda e: e.tensor_copy(eidx, eidf), reads=["eidf"], writes=["eidx"])
            k.op("dve", lambda e: e.tensor_tensor(gw, best, bcast(best[:, :, 0:1], [128, 8, 16]), ALU.subtract), reads=bk_, writes=["gw"])
            k.op("act", lambda e: e.activation(gw, gw, AF.Exp), reads=["gw"], writes=["gw"])
            k.op("dve", lambda e: e.tensor_reduce(sm8[:, 0:8], gw, AX.X, ALU.add), reads=["gw"], writes=["sm8"])
            k.op("dve", lambda e: e.reciprocal(sm8[:, 8:16], sm8[:, 0:8]), reads=["sm8"], writes=["sm8"])
            k.op("dve", lambda e: e.tensor_tensor(gw, gw, bcast(sm8[:, 8:16].unsqueeze(2), [128, 8, 16]), ALU.mult), reads=["gw", "sm8"], writes=["gw"])
            k.op("dve", lambda e: e.memset(av, 0.0), writes=["av"])
            gwf = gw.rearrange("p h k -> p (h k)")
            slots = {}
            for j in range(129):
                if j < 128:
                    r_ = rn[0] % NR; rn[0] += 1
                    slots[j] = r_
                    k.dma("pool", None, None, reads=["eidx"], writes=[("ring", r_)],
                          fn=lambda e, j=j, r_=r_: e.indirect_dma_start(out=ring[r_], out_offset=None, in_=uvb_d[:, :],
                                                                        in_offset=bass.IndirectOffsetOnAxis(ap=eidx[:, j:j + 1], axis=0)))
                    k.op("dve", lambda e, j=j, r_=r_: e.scalar_tensor_tensor(junkp, ring[r_][:, 0:D], 1.0, mg, ALU.mult, ALU.mult, accum_out=av[:, j:j + 1]),
                         reads=[("ring", r_), "h2", "av"], writes=[("av", j)])
                    k.op("act", lambda e, j=j: e.activation(gl[:, j:j + 1], av[:, j:j + 1], AF.Gelu), reads=[("av", j)], writes=[("gl", j)])
                if j >= 1:
                    jj = j - 1
                    r_ = slots[jj]
                    db = jj % 4
                    k.op("dve", lambda e, jj=jj, db=db: e.tensor_scalar(dg[db], identb, gl[:, jj:jj + 1], gwf[:, jj:jj + 1], ALU.mult, ALU.mult),
                         reads=[("gl", jj), "gw", "identb"], writes=[("dg", db)])

                    def vmm(e, jj=jj, db=db, r_=r_):
                        e.matmul(PS[2][:, :], dg[db], ring[r_][:, D:D + 512], start=(jj == 0), stop=(jj == 127))
                        return e.matmul(PS[3][:, :], dg[db], ring[r_][:, D + 512:2 * D], start=(jj == 0), stop=(jj == 127))
                    k.op("pe", vmm, reads=[("dg", db), ("ring", r_)], writes=(["ps2", "ps3"] if jj in (0, 127) else []))
            for hf in range(2):
                k.op("act", lambda e, hf=hf: e.copy(acc[:, hf * 512:(hf + 1) * 512], PS[2 + hf][:, :]), reads=["ps%d" % (2 + hf), "x1s"], writes=["xs"])
            if "peer" in debug and it < 2:
                k.dma("sp", dbg_out("peer_%d" % it, [128, D]), acc, reads=["xs"])
                k.dma("sp", dbg_out("h2_%d" % it, [128, D]), mg, reads=["h2"])
                k.dma("sp", dbg_out("eid_%d" % it, [128, 128]), eidf, reads=["eidf"])
            k.op("dve", lambda e: e.tensor_tensor(acc, acc, g2bc, ALU.mult), reads=["xs"], writes=["xs"])
            k.op("pool", lambda e: e.tensor_tensor(x1, x1, acc, ALU.add), reads=["xs", "x1s"], writes=["x1s"])
        k.op("act", lambda e: e.activation(ob, x1, AF.Square, accum_out=s[:, 0:1]), reads=["x1s", ("ob", 0), ("ob", 1)], writes=["obx", "sB"])
        k.op("dve", lambda e: e.tensor_scalar(s[:, 1:2], s[:, 0:1], 1.0 / D, EPS, ALU.mult, ALU.add), reads=["sB"], writes=["sB"])
        k.op("act", lambda e: e.activation(s[:, 2:3], s[:, 1:2], AF.Sqrt), reads=["sB"], writes=["sB"])
        k.op("dve", lambda e: e.reciprocal(s[:, 3:4], s[:, 2:3]), reads=["sB"], writes=["sB"])
        k.op("dve", lambda e: e.scalar_tensor_tensor(ob, x1, s[:, 3:4], gfin, ALU.mult, ALU.mult),
             reads=["x1s", "sB", "gfin", "obx"], writes=["obx"])
        k.dma("sp", out_v[it], ob, reads=["obx"], writes=["out"])
    k.barrier()
    return nc, IN, DBG, k


def rope_tables():
    p = np.arange(128)[:, None]
    i = np.arange(NT)[None, :]
    t = 32 * p + i
    row = (t // 64).astype(np.float32)
    col = (t % 64).astype(np.float32)
    inv = (10000.0 ** (-np.arange(0, 32, 2, dtype=np.float32) / 32)).astype(np.float32)
    ar = row[..., None] * inv
    ac = col[..., None] * inv
    ang = np.concatenate([ar, ar, ac, ac], axis=-1)
    cos = np.cos(ang).astype(np.float32)
    sin = np.sin(ang).astype(np.float32)
    sgn = np.ones(64, np.float32)
    sgn[0:16] = -1; sgn[32:48] = -1
    return cos.reshape(128, NT * 64), (sin * sgn).reshape(128, NT * 64)


def swap_halves(g):
    g = g.reshape(2, 2, 16)
    return np.ascontiguousarray(g[:, ::-1, :]).reshape(1, 64)


def fft_plan_tables():
    p = np.arange(128, dtype=np.float64)[:, None]
    f1 = np.arange(256, dtype=np.float64)[None, :]
    a = 2 * np.pi * p * f1 / 256
    W1 = np.concatenate([np.cos(a), -np.sin(a)], axis=1)
    P = np.arange(128)
    s2 = (P // 4).astype(np.float64)
    c4 = P % 4
    th = 2 * np.pi * s2[:, None] * s2[None, :] / 32
    dl = (c4[:, None] == c4[None, :]).astype(np.float64)
    KC = np.cos(th) * dl
    KS = np.sin(th) * dl
    R1 = np.concatenate([KC, KS], axis=1)
    R2 = np.concatenate([-KS, KC], axis=1)
    hh = np.arange(2, dtype=np.float64)[None, :, None]
    s1 = np.arange(128, dtype=np.float64)[None, None, :]
    a2 = 2 * np.pi * (128 * hh + p[:, :, None]) * s1 / 256
    C2 = np.cos(a2).reshape(128, 256)
    NS2 = (-np.sin(a2)).reshape(128, 256)
    ph = 2 * np.pi * s2[:, None] * f1 / 8192
    TW1 = np.concatenate([np.cos(ph), np.sin(ph), -np.sin(ph)], axis=1)
    f1b = (128 * np.arange(2, dtype=np.float64)[None, :, None] + p[:, :, None])
    ph2 = 2 * np.pi * s2[None, None, :] * f1b / 8192
    TW2 = np.stack([np.cos(ph2), -np.sin(ph2), np.sin(ph2)], axis=2).reshape(128, 768)
    f = lambda x: np.ascontiguousarray(x.astype(np.float32))
    return {"W1": f(W1), "KC": f(KC), "KS": f(KS), "NKS": f(-KS), "R1": f(R1), "R2": f(R2),
            "C2": f(C2), "NS2": f(NS2), "TW1": f(TW1), "TW2": f(TW2)}


def filter_consts():
    t01 = np.linspace(0.0, 1.0, L, dtype=np.float32)[:, None]
    w = (np.float32(2.0 * np.pi) * np.arange(L, dtype=np.float32)[:, None] / np.float32(L)).astype(np.float32)
    fb = np.linspace(1e-4, 15, 16, dtype=np.float32)[None]
    z = np.concatenate([t01, np.cos(fb * w), -np.sin(fb * w)], axis=-1).astype(np.float32)
    max_decay = np.log(1e-2) / 0.3
    min_decay = np.log(1e-2) / 1.5
    deltas = np.abs(np.linspace(min_decay, max_decay, HYC, dtype=np.float32))
    return {"zT": np.ascontiguousarray(z.T), "t01": np.ascontiguousarray(t01.T),
            "ndelT": np.ascontiguousarray((-deltas).reshape(4, 128).T.astype(np.float32))}


def make_in_maps(inputs):
    f = lambda a: np.ascontiguousarray(np.asarray(a, dtype=np.float32))
    cos, sin = rope_tables()
    shared = {
        "cctxT": f(inputs["c_ctx"].reshape(8, 128).T),
        "ada_w": f(inputs["ada_w"][0]),
        "ada_bT": f(inputs["ada_b"][0].reshape(48, 128).T),
        "gmixT": f(inputs["norm_mix_g"][0].reshape(8, 128).T),
        "gffnT": f(inputs["norm_ffn_g"][0].reshape(8, 128).T),
        "w_in": f(inputs["w_in"][0]),
        "rope_cos": cos, "rope_sin": sin,
        "gq": f(inputs["q_norm_g"][0].reshape(1, 64)),
        "gk": f(inputs["k_norm_g"][0].reshape(1, 64)),
        "gqsw": f(swap_halves(np.asarray(inputs["q_norm_g"][0]))),
        "gksw": f(swap_halves(np.asarray(inputs["k_norm_g"][0]))),
        "w_attn_out": f(inputs["w_attn_out"][0]),
        "w_out": f(inputs["w_out"][0]),
        "final_norm_g": f(inputs["final_norm_g"].reshape(1, D)),
        "w_hy_out": f(inputs["w_hy_out"][0]), "peer_wq": f(inputs["peer_wq"][0]),
        "keysT": f(np.stack([np.asarray(inputs["peer_keys1"][0]), np.asarray(inputs["peer_keys2"][0])], axis=1).transpose(3, 0, 1, 2).reshape(128, 2048)),
        "peer_u": f(inputs["peer_u"][0]), "peer_v": f(inputs["peer_v"][0]),
        "hf_w1": f(inputs["hf_w1"][0]), "hf_w2": f(inputs["hf_w2"][0]), "hf_w3": f(inputs["hf_w3"][0]), "hf_w4": f(inputs["hf_w4"][0]),
        "hf_b": f(np.stack([np.asarray(inputs["hf_b1"][0]), np.asarray(inputs["hf_b2"][0]), np.asarray(inputs["hf_b3"][0]), np.asarray(inputs["hf_freq"][0])], axis=1)),
        "hy_conv_w": f(inputs["hy_conv_w"][0]), "hy_conv_b": f(np.asarray(inputs["hy_conv_b"][0]).reshape(1, 3 * HYC)),
        "skipT": f(np.asarray(inputs["hy_skip"][0]).reshape(2, 128, 4)[:, :, np.arange(128) % 4].transpose(2, 0, 1).reshape(128, 256)),
    }
    shared.update(fft_plan_tables())
    shared.update(filter_consts())
    maps = []
    for b in range(8):
        m = dict(shared)
        m["x"] = f(inputs["x"][b])
        m["ctx"] = f(inputs["ctx"][b])
        m["cT"] = f(inputs["c"][b].reshape(8, 128).T)
        maps.append(m)
    return maps


def kernel(**inputs):
    nc, IN, DBG, k = build_program()
    maps = make_in_maps(inputs)
    maps = [{n: m[n] for n in IN} for m in maps]
    res = run_bass_kernel_spmd(nc, maps, core_ids=list(range(8)))
    out = np.stack([np.asarray(r["out"], dtype=np.float32) for r in res.results], axis=0)
    return out
```

```python
import numpy as np
import ml_dtypes
import concourse.bass as bass
import concourse.mybir as mybir
from concourse.bass_utils import run_bass_kernel_spmd

F32 = mybir.dt.float32
BF16 = mybir.dt.bfloat16
I32 = mybir.dt.int32
U32 = mybir.dt.uint32
U8 = mybir.dt.uint8
ALU = mybir.AluOpType
AF = mybir.ActivationFunctionType
AX = mybir.AxisListType

D = 1024
L = 4096
NT = 32
CTX = 256
EPS = 1e-6
NKEY = L + CTX
NKC = NKEY // 128
IN_COLS = 4352
HYC = 512


ATTACH_WAITS = True


class KB:
    def __init__(self, nc, n_dma_slots=32):
        self.nc = nc
        self.eng = {"pe": nc.tensor, "act": nc.scalar, "dve": nc.vector,
                    "pool": nc.gpsimd, "sp": nc.sync}
        self.sem, self.cnt, self.semobj = {}, {}, {}
        for n in self.eng:
            self.sem[n] = nc.alloc_semaphore("s_" + n)
            self.cnt[n] = 0
            self.semobj["s_" + n] = self.sem[n]
        self.slots = []
        for i in range(n_dma_slots):
            s = nc.alloc_semaphore("d_%d" % i)
            self.slots.append([s, 0])
            self.semobj["d_%d" % i] = s
        self.slot_rr = 0
        self.known = {n: {} for n in self.eng}
        self.lastw, self.reads = {}, {}
        self.ninstr = 0

    def _need(self, e, tick, pend=None):
        if tick is None:
            return
        sn, val = tick
        if self.known[e].get(sn, 0) >= val:
            return
        self.known[e][sn] = val
        if pend is not None:
            pend[sn] = max(pend.get(sn, 0), val)
            return
        self.eng[e].wait_ge(self.semobj[sn], val)
        self.ninstr += 1

    def _pre(self, e, reads, writes, pend=None):
        for k in reads:
            self._need(e, self.lastw.get(k), pend)
        for k in writes:
            self._need(e, self.lastw.get(k), pend)
            for t in self.reads.get(k, ()):
                self._need(e, t, pend)

    def _flush(self, e, pend, keep_last):
        items = list(pend.items())
        last = None
        if keep_last and items:
            last = items.pop()
        for sn, val in items:
            self.eng[e].wait_ge(self.semobj[sn], val)
            self.ninstr += 1
        return last

    def _post(self, tick, reads, writes):
        for k in reads:
            self.reads.setdefault(k, []).append(tick)
        for k in writes:
            self.lastw[k] = tick
            self.reads[k] = []

    def op(self, e, fn, reads=(), writes=()):
        psr = [r for r in reads if isinstance(r, str) and r.startswith("ps")]
        if psr:
            reads = [r for r in reads if r not in psr]
            writes = list(writes) + psr
        pend = {}
        self._pre(e, reads, writes, pend)
        single = ATTACH_WAITS and getattr(fn, "__name__", "") == "<lambda>"
        last = self._flush(e, pend, single)
        ins = fn(self.eng[e])
        if last is not None:
            ins._wait_ge(self.semobj[last[0]], last[1])
        self.cnt[e] += 1
        ins.then_inc(self.sem[e], 1)
        self.ninstr += 1
        tick = ("s_" + e, self.cnt[e])
        self._post(tick, reads, writes)
        return tick

    def dma(self, e, out, in_, reads=(), writes=(), fn=None, **kw):
        pend = {}
        self._pre(e, reads, writes, pend)
        si = self.slot_rr
        self.slot_rr = (self.slot_rr + 1) % len(self.slots)
        slot = self.slots[si]
        sn = "d_%d" % si
        self._need(e, (sn, slot[1]) if slot[1] else None, pend)
        last = self._flush(e, pend, ATTACH_WAITS)
        if fn is None:
            ins = self.eng[e].dma_start(out=out, in_=in_, **kw)
        else:
            ins = fn(self.eng[e])
        if last is not None:
            ins._wait_ge(self.semobj[last[0]], last[1])
        slot[1] += 16
        ins.then_inc(slot[0], 16)
        self.ninstr += 1
        tick = (sn, slot[1])
        self._post(tick, reads, writes)
        return tick

    def barrier(self):
        for e in self.eng:
            for i, s in enumerate(self.slots):
                if s[1]:
                    self._need(e, ("d_%d" % i, s[1]))
            for n in self.eng:
                if n != e and self.cnt[n]:
                    self._need(e, ("s_" + n, self.cnt[n]))
        self.lastw, self.reads = {}, {}


class Arena:
    def __init__(self, nc, nbytes):
        self.big = nc.alloc_sbuf_tensor("arena", [128, nbytes], U8)
        self.nbytes = nbytes
        self.off = 0

    def reset(self, off=0):
        self.off = off

    def alloc(self, shape, dtype, parts=128):
        isz = 4 if dtype in (F32, I32, U32) else 2
        n = int(np.prod(shape[1:]))
        size = (n * isz + 63) // 64 * 64
        assert self.off + size <= self.nbytes, ("arena overflow", self.off, size, self.nbytes)
        ap = self.big[0:shape[0], self.off:self.off + n * isz].bitcast(dtype)
        self.off += size
        if len(shape) > 2:
            names = " ".join("d%d" % i for i in range(1, len(shape)))
            kw = {"d%d" % i: shape[i] for i in range(1, len(shape))}
            ap = ap.rearrange("p (%s) -> p %s" % (names, names), **kw)
        return ap


def bcast(ap, shape):
    return ap.to_broadcast(list(shape))


def build_program(debug=(), stop=None, skip=()):
    nc = bass.Bass("TRN2", target_bir_lowering=False)
    IN = {}

    def inp(name, shape, dt=F32):
        IN[name] = nc.dram_tensor(name, list(shape), dt, kind="ExternalInput").ap()
        return IN[name]

    x_d = inp("x", [L, D])
    ctx_d = inp("ctx", [CTX, D])
    cT_d = inp("cT", [128, 8])
    cctxT_d = inp("cctxT", [128, 8])
    adaw_d = inp("ada_w", [D, 6 * D])
    adabT_d = inp("ada_bT", [128, 48])
    gmixT_d = inp("gmixT", [128, 8])
    gffnT_d = inp("gffnT", [128, 8])
    win_d = inp("w_in", [D, IN_COLS])
    cos_d = inp("rope_cos", [128, NT * 64])
    sin_d = inp("rope_sin", [128, NT * 64])
    gq_d = inp("gq", [1, 64])
    gk_d = inp("gk", [1, 64])
    gqsw_d = inp("gqsw", [1, 64])
    gksw_d = inp("gksw", [1, 64])
    wao_d = inp("w_attn_out", [512, D])
    wout_d = inp("w_out", [D, D])
    gfin_d = inp("final_norm_g", [1, D])
    why_d = inp("w_hy_out", [HYC, D]); wq_d = inp("peer_wq", [D, 2048]); keysT_d = inp("keysT", [128, 2048])
    pu_d = inp("peer_u", [16384, D]); pv_d = inp("peer_v", [16384, D])
    W1_d = inp("W1", [128, 512]); KC_d = inp("KC", [128, 128]); KS_d = inp("KS", [128, 128]); NKS_d = inp("NKS", [128, 128])
    R1_d = inp("R1", [128, 256]); R2_d = inp("R2", [128, 256]); C2_d = inp("C2", [128, 256]); NS2_d = inp("NS2", [128, 256])
    TW1_d = inp("TW1", [128, 768]); TW2_d = inp("TW2", [128, 768]); skipT_d = inp("skipT", [128, 256])
    zT_d = inp("zT", [33, L]); t01_d = inp("t01", [1, L]); ndelT_d = inp("ndelT", [128, 4])
    hfw1_d = inp("hf_w1", [33, 64]); hfw2_d = inp("hf_w2", [64, 64]); hfw3_d = inp("hf_w3", [64, 64]); hfw4_d = inp("hf_w4", [64, 2048])
    hfb_d = inp("hf_b", [64, 4]); hcw_d = inp("hy_conv_w", [3, 3 * HYC]); hcb_d = inp("hy_conv_b", [1, 3 * HYC])

    out_d = nc.dram_tensor("out", [L, D], F32, kind="ExternalOutput").ap()
    DBG = {}

    def dbg_out(name, shape, dt=F32):
        DBG[name] = nc.dram_tensor("dbg_" + name, list(shape), dt, kind="ExternalOutput").ap()
        return DBG[name]

    zhy_d = nc.dram_tensor("zhy_s", [L + 2, 3 * HYC], F32).ap()
    gates_d = nc.dram_tensor("gates_s", [L, 2 * D], BF16).ap()
    attnT_d = nc.dram_tensor("attnT_s", [8, 64, L], BF16).ap()
    kf_d = nc.dram_tensor("kf_s", [2, 128, 128, 768], F32).ap()
    uvb_d = nc.dram_tensor("uvb_s", [16384, 2 * D], BF16).ap()

    k = KB(nc)
    A = Arena(nc, 206 * 1024)
    PS = [nc.alloc_psum_tensor("ps%d" % i, [128, 512], F32) for i in range(8)]

    ident = A.alloc([128, 128], F32)
    ti = A.alloc([128, 128], I32)
    k.op("pool", lambda e: e.iota(ti, pattern=[[1, 128]], base=0, channel_multiplier=-1), writes=["ti"])
    k.op("dve", lambda e: e.tensor_scalar(ident, ti, 0, None, ALU.is_equal), reads=["ti"], writes=["ident"])
    modT = A.alloc([128, 48, 2], F32)
    A1 = A.alloc([128, 8], F32)
    Ac1 = A.alloc([128, 8], F32)
    A2 = A.alloc([128, 8], F32)
    gmixT = A.alloc([128, 8], F32)
    gffnT = A.alloc([128, 8], F32)
    negmb = A.alloc([128, 1], F32)
    epsc = A.alloc([128, 1], F32)
    k.op("dve", lambda e: e.memset(epsc, EPS), writes=["epsc"])
    PERSIST = A.off

    craw = A.alloc([128, 2, 8], F32)
    sc2 = A.alloc([128, 8, 2], F32)
    adabT = A.alloc([128, 48], F32)
    k.dma("sp", craw[:, 0, :], cT_d, writes=["craw0"])
    k.dma("sp", craw[:, 1, :], cctxT_d, writes=["craw1"])
    k.dma("sp", adabT, adabT_d, writes=["adabT"])
    k.dma("sp", gmixT, gmixT_d, writes=["gmixT"])
    k.dma("sp", gffnT, gffnT_d, writes=["gffnT"])
    k.op("act", lambda e: e.activation(sc2[:, :, 0], craw[:, 0, :], AF.Silu), reads=["craw0"], writes=["sc2a"])
    k.op("act", lambda e: e.activation(sc2[:, :, 1], craw[:, 1, :], AF.Silu), reads=["craw1"], writes=["sc2b"])
    awt = [A.alloc([128, 8, 1024], F32) for _ in range(2)]
    adaw_v = adaw_d.rearrange("(k p) c -> p k c", p=128)
    for blk in range(6):
        b = blk % 2
        for kk in range(8):
            k.dma("sp" if kk % 2 == 0 else "pool", awt[b][:, kk, :], adaw_d[kk * 128:(kk + 1) * 128, blk * 1024:(blk + 1) * 1024],
                  writes=[("awt", b, kk)])

        def mm(e, blk=blk, b=b):
            ins = None
            for n in range(8):
                cn = blk * 8 + n
                for kk in range(8):
                    ins = e.matmul(PS[0][:, 2 * cn:2 * cn + 2], awt[b][:, kk, n * 128:(n + 1) * 128], sc2[:, kk, :],
                                   start=(kk == 0), stop=(kk == 7))
            return ins
        k.op("pe", mm, reads=[("awt", b, kk) for kk in range(8)] + ["sc2a", "sc2b"], writes=["ps0"])
    k.op("dve", lambda e: e.tensor_tensor(modT, PS[0][:, 0:96].rearrange("p (c t) -> p c t", t=2),
                                          bcast(adabT.unsqueeze(2), [128, 48, 2]), ALU.add),
         reads=["ps0", "adabT"], writes=["modT"])
    k.op("dve", lambda e: e.scalar_tensor_tensor(A1, modT[:, 8:16, 0], 1.0, gmixT, ALU.add, ALU.mult),
         reads=["modT", "gmixT"], writes=["A1"])
    k.op("dve", lambda e: e.scalar_tensor_tensor(Ac1, modT[:, 8:16, 1], 1.0, gmixT, ALU.add, ALU.mult),
         reads=["modT", "gmixT"], writes=["Ac1"])
    k.op("dve", lambda e: e.scalar_tensor_tensor(A2, modT[:, 32:40, 0], 1.0, gffnT, ALU.add, ALU.mult),
         reads=["modT", "gffnT"], writes=["A2"])
    if "modT" in debug:
        k.dma("sp", dbg_out("modT", [128, 96]), modT.rearrange("p c t -> p (c t)"), reads=["modT"])
    k.barrier()
    A.reset(PERSIST)

    if stop == 'p0':
        return nc, IN, DBG, k
    winb = A.alloc([128, 8, IN_COLS], BF16)
    QT = A.alloc([128, 4, L], BF16)
    KT = A.alloc([128, NKEY], BF16)
    VX = A.alloc([128, NKC, 2, 65], BF16)
    cosq = A.alloc([128, NT, 64], F32)
    sinq = A.alloc([128, NT, 64], F32)
    cosk = A.alloc([128, NT, 64], F32)
    sink = A.alloc([128, NT, 64], F32)
    gtab = A.alloc([128, 4, 64], F32)
    P3 = A.off
    for j, gd in enumerate((gq_d, gk_d, gqsw_d, gksw_d)):
        k.dma("sp", gtab[:, j, :], bcast(gd, [128, 64]), writes=[("gtab", j)])
    HW = IN_COLS // 4
    wst = [A.alloc([128, HW], F32) for _ in range(4)]
    for kk in range(8):
        for hh in range(4):
            k.dma("sp" if hh % 2 == 0 else "pool", wst[hh], win_d[kk * 128:(kk + 1) * 128, hh * HW:(hh + 1) * HW], writes=[("wst", hh)])
            if hh % 2 == 0:
                k.op("act", lambda e, kk=kk, hh=hh: e.copy(winb[:, kk, hh * HW:(hh + 1) * HW], wst[hh]), reads=[("wst", hh)], writes=[("winb", kk, hh)])
            else:
                k.op("dve", lambda e, kk=kk, hh=hh: e.tensor_copy(winb[:, kk, hh * HW:(hh + 1) * HW], wst[hh]), reads=[("wst", hh)], writes=[("winb", kk, hh)])
    for q4 in range(4):
        k.dma("sp", cosk[:, q4 * 8:(q4 + 1) * 8, :].rearrange("p a b -> p (a b)"), cos_d[:, q4 * 512:(q4 + 1) * 512], writes=[("cosk", q4)])
        k.dma("pool", sink[:, q4 * 8:(q4 + 1) * 8, :].rearrange("p a b -> p (a b)"), sin_d[:, q4 * 512:(q4 + 1) * 512], writes=[("sink", q4)])
    k.op("dve", lambda e: e.tensor_tensor(cosq, cosk, bcast(gtab[:, 0:1, :], [128, NT, 64]), ALU.mult),
         reads=[("cosk", q) for q in range(4)] + [("gtab", 0)], writes=["cosq"])
    k.op("dve", lambda e: e.tensor_tensor(sinq, sink, bcast(gtab[:, 2:3, :], [128, NT, 64]), ALU.mult),
         reads=[("sink", q) for q in range(4)] + [("gtab", 2)], writes=["sinq"])
    k.op("dve", lambda e: e.tensor_tensor(cosk, cosk, bcast(gtab[:, 1:2, :], [128, NT, 64]), ALU.mult),
         reads=["cosq", ("gtab", 1)], writes=["cosk"])
    k.op("dve", lambda e: e.tensor_tensor(sink, sink, bcast(gtab[:, 3:4, :], [128, NT, 64]), ALU.mult),
         reads=["sinq", ("gtab", 3)], writes=["sink"])
    mqk = A.alloc([128, 2], F32)
    k.op("dve", lambda e: e.tensor_reduce(mqk[:, 0:1], gtab[:, 0, :], AX.X, ALU.max, apply_absolute_value=True),
         reads=[("gtab", 0)], writes=["mqk0"])
    k.op("dve", lambda e: e.tensor_reduce(mqk[:, 1:2], gtab[:, 1, :], AX.X, ALU.max, apply_absolute_value=True),
         reads=[("gtab", 1)], writes=["mqk1"])
    k.op("dve", lambda e: e.scalar_tensor_tensor(negmb, mqk[:, 0:1], -8.0, mqk[:, 1:2], ALU.mult, ALU.mult),
         reads=["mqk0", "mqk1"], writes=["negmb"])
    k.op("pool", lambda e: e.memset(VX[:, :, :, 64:65], 1.0), writes=["vx1"])
    zrow = A.alloc([128, 3 * HYC], F32)
    k.op("pool", lambda e: e.memset(zrow, 0.0), writes=["zrow"])
    k.dma("pool", zhy_d[0:1, :], zrow[0:1, :], reads=["zrow"])
    k.dma("pool", zhy_d[L + 1:L + 2, :], zrow[0:1, :], reads=["zrow"])

    k.barrier()
    if stop == 'p3a':
        return nc, IN, DBG, k
    A.reset(P3)
    xb = [A.alloc([128, D], F32) for _ in range(2)]
    xn = [A.alloc([128, D], F32) for _ in range(2)]
    hT = [A.alloc([128, 8, 128], BF16) for _ in range(2)]
    st = [A.alloc([128, 24], F32) for _ in range(2)]
    sq = A.alloc([128, 640], F32)
    t1 = A.alloc([128, 640], F32)
    t2 = A.alloc([128, 640], F32)
    qrp = A.alloc([128, 4, 2, 64], F32)
    kr = A.alloc([128, 128], F32)
    zh = [A.alloc([128, 3 * HYC], F32)] * 2
    gs = [A.alloc([128, 2 * D], BF16)] * 2
    x_v = x_d.rearrange("(p i) d -> i p d", i=NT)
    zhy_v = zhy_d[1:L + 1, :].rearrange("(p i) c -> i p c", i=NT)
    gates_v = gates_d.rearrange("(p i) c -> i p c", i=NT)
    zb = 0

    for it in range(NT + 2):
        if stop == 'p3b' and it == 1:
            k.barrier()
            return nc, IN, DBG, k
        b = it % 2
        isx = it < NT
        src = x_v[it] if isx else ctx_d[(it - NT) * 128:(it - NT + 1) * 128, :]
        k.dma("sp", xb[b], src, writes=[("xb", b)])
        s = st[b]
        k.op("act", lambda e, b=b, s=s: e.activation(xn[b], xb[b], AF.Square, accum_out=s[:, 0:1]),
             reads=[("xb", b)], writes=[("xn", b), ("st", b)])
        k.op("dve", lambda e, s=s: e.tensor_scalar(s[:, 1:2], s[:, 0:1], 1.0 / D, EPS, ALU.mult, ALU.add),
             reads=[("st", b)], writes=[("st", b)])
        k.op("act", lambda e, s=s: e.activation(s[:, 2:3], s[:, 1:2], AF.Sqrt), reads=[("st", b)], writes=[("st", b)])
        k.op("dve", lambda e, s=s: e.reciprocal(s[:, 3:4], s[:, 2:3]), reads=[("st", b)], writes=[("st", b)])
        k.op("act", lambda e, b=b, s=s: e.activation(xn[b], xb[b], AF.Identity, scale=s[:, 3:4]),
             reads=[("xb", b), ("st", b)], writes=[("xn", b)])
        for half in range(2):
            def tr(e, b=b, half=half):
                ins = None
                for j in range(4):
                    jj = half * 4 + j
                    ins = e.transpose(PS[half][:, j * 128:(j + 1) * 128], xn[b][:, jj * 128:(jj + 1) * 128], ident)
                return ins
            k.op("pe", tr, reads=[("xn", b), "ident"], writes=["ps%d" % half])
        Asc = A1 if isx else Ac1
        bcol = 0 if isx else 1
        for jj in range(8):
            half, j = jj // 4, jj % 4
            if jj % 2 == 0:
                k.op("act", lambda e, b=b, jj=jj, half=half, j=j, Asc=Asc, bcol=bcol: e.activation(
                    hT[b][:, jj, :], PS[half][:, j * 128:(j + 1) * 128], AF.Identity,
                    scale=Asc[:, jj:jj + 1], bias=modT[:, jj, bcol:bcol + 1]),
                    reads=["ps%d" % half, "A1", "Ac1", "modT"], writes=[("hT", b, jj)])
            else:
                k.op("dve", lambda e, b=b, jj=jj, half=half, j=j, Asc=Asc, bcol=bcol: e.tensor_scalar(
                    hT[b][:, jj, :], PS[half][:, j * 128:(j + 1) * 128],
                    Asc[:, jj:jj + 1], modT[:, jj, bcol:bcol + 1], ALU.mult, ALU.add),
                    reads=["ps%d" % half, "A1", "Ac1", "modT"], writes=[("hT", b, jj)])
        hkeys = [("hT", b, jj) for jj in range(8)]
        wkeys = []

        def zmm(c0, n, bank):
            def f(e):
                ins = None
                for kk in range(8):
                    ins = e.matmul(PS[bank][:, 0:n], hT[b][:, kk, :], winb[:, kk, c0:c0 + n], start=(kk == 0), stop=(kk == 7))
                return ins
            k.op("pe", f, reads=hkeys + wkeys, writes=["ps%d" % bank])

        bank = 2 + zb % 4; zb += 1
        zmm(512, 256, bank)
        kp = PS[bank][:, 0:128]
        k.op("act", lambda e, kp=kp: e.activation(sq[:, 512:640], kp, AF.Square), reads=["ps%d" % bank], writes=["sqk"])
        k.op("dve", lambda e, s=s: e.tensor_reduce(s[:, 4:6], sq[:, 512:640].rearrange("p (h d) -> p h d", d=64), AX.X, ALU.add),
             reads=["sqk"], writes=[("st", b)])
        k.op("dve", lambda e, s=s: e.tensor_scalar(s[:, 4:6], s[:, 4:6], 1.0 / 64, EPS, ALU.mult, ALU.add),
             reads=[("st", b)], writes=[("st", b)])
        k.op("act", lambda e, s=s: e.activation(s[:, 4:6], s[:, 4:6], AF.Sqrt), reads=[("st", b)], writes=[("st", b)])
        k.op("dve", lambda e, s=s: e.reciprocal(s[:, 6:8], s[:, 4:6]), reads=[("st", b)], writes=[("st", b)])
        kp3 = kp.rearrange("p (h d) -> p h d", d=64)
        t1k = t1[:, 512:640].rearrange("p (h d) -> p h d", d=64)
        t2k = t2[:, 512:640].rearrange("p (h d) -> p h d", d=64)
        if isx:
            k.op("dve", lambda e: e.tensor_tensor(t1k, kp3, bcast(cosk[:, it:it + 1, :], [128, 2, 64]), ALU.mult),
                 reads=["ps%d" % bank, "cosk"], writes=["t1k"])
            for a in range(2):
                for f in range(2):
                    o0 = a * 32 + f * 16
                    i0 = a * 32 + (1 - f) * 16
                    k.op("dve", lambda e, o0=o0, i0=i0: e.tensor_tensor(
                        t2k[:, :, o0:o0 + 16], kp3[:, :, i0:i0 + 16],
                        bcast(sink[:, it:it + 1, o0:o0 + 16], [128, 2, 16]), ALU.mult),
                        reads=["ps%d" % bank, "sink"], writes=[("t2k", a, f)])
            k.op("dve", lambda e: e.tensor_tensor(t1k, t1k, t2k, ALU.add),
                 reads=["t1k"] + [("t2k", a, f) for a in range(2) for f in range(2)], writes=["t1k"])
        else:
            k.op("dve", lambda e: e.tensor_tensor(t1k, kp3, bcast(gtab[:, 1:2, :], [128, 2, 64]), ALU.mult),
                 reads=["ps%d" % bank, ("gtab", 1)], writes=["t1k"])
        k.op("dve", lambda e, s=s: e.tensor_tensor(kr.rearrange("p (h d) -> p h d", d=64), t1k,
                                                   bcast(s[:, 6:8].unsqueeze(2), [128, 2, 64]), ALU.mult),
             reads=["t1k", ("st", b)], writes=["kr"])
        k.op("act", lambda e, bank=bank: e.copy(VX[:, it, :, 0:64], PS[bank][:, 128:256].rearrange("p (g d) -> p g d", d=64)),
             reads=["ps%d" % bank], writes=[("vx", it)])
        k.op("pe", lambda e: e.transpose(PS[7][:, 0:128], kr, ident), reads=["kr", "ident"], writes=["ps7"])
        k.op("act", lambda e: e.copy(KT[:, it * 128:(it + 1) * 128], PS[7][:, 0:128]), reads=["ps7"], writes=[("kt", it)])
        if not isx:
            continue
        bank = 2 + zb % 4; zb += 1
        zmm(0, 512, bank)
        qp = PS[bank][:, 0:512]
        k.op("act", lambda e, qp=qp: e.activation(sq[:, 0:512], qp, AF.Square), reads=["ps%d" % bank], writes=["sqq"])
        k.op("dve", lambda e, s=s: e.tensor_reduce(s[:, 8:16], sq[:, 0:512].rearrange("p (h d) -> p h d", d=64), AX.X, ALU.add),
             reads=["sqq"], writes=[("st", b)])
        k.op("dve", lambda e, s=s: e.tensor_scalar(s[:, 8:16], s[:, 8:16], 1.0 / 64, EPS, ALU.mult, ALU.add),
             reads=[("st", b)], writes=[("st", b)])
        k.op("act", lambda e, s=s: e.activation(s[:, 8:16], s[:, 8:16], AF.Sqrt), reads=[("st", b)], writes=[("st", b)])
        k.op("dve", lambda e, s=s: e.reciprocal(s[:, 16:24], s[:, 8:16]), reads=[("st", b)], writes=[("st", b)])
        qp3 = qp.rearrange("p (h d) -> p h d", d=64)
        t1q = t1[:, 0:512].rearrange("p (h d) -> p h d", d=64)
        t2q = t2[:, 0:512].rearrange("p (h d) -> p h d", d=64)
        k.op("dve", lambda e: e.tensor_tensor(t1q, qp3, bcast(cosq[:, it:it + 1, :], [128, 8, 64]), ALU.mult),
             reads=["ps%d" % bank, "cosq"], writes=["t1q"])
        for a in range(2):
            for f in range(2):
                o0 = a * 32 + f * 16
                i0 = a * 32 + (1 - f) * 16
                k.op("dve", lambda e, o0=o0, i0=i0: e.tensor_tensor(
                    t2q[:, :, o0:o0 + 16], qp3[:, :, i0:i0 + 16],
                    bcast(sinq[:, it:it + 1, o0:o0 + 16], [128, 8, 16]), ALU.mult),
                    reads=["ps%d" % bank, "sinq"], writes=[("t2q", a, f)])
        k.op("dve", lambda e: e.tensor_tensor(t1q, t1q, t2q, ALU.add),
             reads=["t1q"] + [("t2q", a, f) for a in range(2) for f in range(2)], writes=["t1q"])
        k.op("dve", lambda e, s=s: e.tensor_tensor(
            qrp.rearrange("p a s d -> p s a d"), t1[:, 0:512].rearrange("p (s a d) -> p s a d", s=2, a=4),
            bcast(s[:, 16:24].rearrange("p (s a) -> p s a", s=2).unsqueeze(3), [128, 2, 4, 64]), ALU.mult),
            reads=["t1q", ("st", b)], writes=["qrp"])

        def trq(e):
            ins = None
            for a in range(4):
                ins = e.transpose(PS[6][:, a * 128:(a + 1) * 128], qrp[:, a, :, :].rearrange("p s d -> p (s d)"), ident)
            return ins
        k.op("pe", trq, reads=["qrp", "ident"], writes=["ps6"])
        k.op("act", lambda e: e.copy(QT[:, :, it * 128:(it + 1) * 128], PS[6][:, :].rearrange("p (a t) -> p a t", a=4)),
             reads=["ps6"], writes=[("qt", it)])
        for c in range(3):
            bank = 2 + zb % 4; zb += 1
            zmm(768 + c * 512, 512, bank)
            k.op("act", lambda e, c=c, bank=bank: e.copy(zh[b][:, c * 512:(c + 1) * 512], PS[bank][:, :]),
                 reads=["ps%d" % bank], writes=[("zh", c)])
        for c in range(3):
            if "scr" not in skip:
                k.dma("pool", zhy_v[it][:, c * 512:(c + 1) * 512], zh[b][:, c * 512:(c + 1) * 512], reads=[("zh", c)])
        for c in range(4):
            bank = 2 + zb % 4; zb += 1
            zmm(2304 + c * 512, 512, bank)
            k.op("act", lambda e, c=c, bank=bank: e.activation(gs[b][:, c * 512:(c + 1) * 512], PS[bank][:, :], AF.Sigmoid),
                 reads=["ps%d" % bank], writes=[("gs", c)])
        if "scr" not in skip:
            k.dma("pool", gates_v[it], gs[b], reads=[("gs", c) for c in range(4)])

    if "QT" in debug:
        k.barrier()
        A.reset(P3)
        qf = A.alloc([128, 4, 512], F32)
        k.op("dve", lambda e: e.tensor_copy(qf, QT[:, :, 0:512]), reads=[("qt", i) for i in range(4)], writes=["qf"])
        dq = dbg_out("QT", [128, 2048])
        for a in range(4):
            k.dma("sp", dq[:, a * 512:(a + 1) * 512], qf[:, a, :], reads=["qf"])
        kf_ = A.alloc([128, NKEY], F32)
        k.op("dve", lambda e: e.tensor_copy(kf_, KT), reads=[("kt", i) for i in range(NKC)], writes=["kf_"])
        dk = dbg_out("KT", [128, NKEY])
        for a in range(NKC // 2):
            k.dma("sp", dk[:, a * 256:(a + 1) * 256], kf_[:, a * 256:(a + 1) * 256], reads=["kf_"])
    k.barrier()
    A.reset(P3)

    if stop == 'p3':
        return nc, IN, DBG, k
    pT = [A.alloc([128, 512], BF16) for _ in range(3)]
    osb = [A.alloc([128, 512], F32) for _ in range(2)]
    rec = A.alloc([128, 512], F32)
    aT = [A.alloc([128, 512], BF16) for _ in range(2)]
    ones = A.alloc([128, 64], BF16)
    onesf = A.alloc([128, 64], F32)
    k.op("dve", lambda e: e.memset(onesf, 1.0), writes=["onesf"])
    cst = [A.alloc([128, D], F32) for _ in range(4)]
    cbf = [A.alloc([128, D], BF16) for _ in range(4)]
    cvn = [0]

    def conv_chunk():
        cn = cvn[0]; cvn[0] += 1
        if cn >= 256:
            return
        tb, rch, b4 = cn // 128, cn % 128, cn % 4
        src_d = pu_d if tb == 0 else pv_d
        k.dma("sp", cst[b4], src_d[rch * 128:(rch + 1) * 128, :], writes=[("cst", b4)])
        k.op("dve", lambda e: e.tensor_copy(cbf[b4], cst[b4]), reads=[("cst", b4)], writes=[("cbf", b4)])
        k.dma("sp", uvb_d[rch * 128:(rch + 1) * 128, tb * D:(tb + 1) * D], cbf[b4], reads=[("cbf", b4)], writes=["uvb"])
    step = 0
    for h in range(8):
        g, a = h // 4, h % 4
        pr = slice(64 * g, 64 * g + 64)
        for qc in range(8):
            ob = (h * 8 + qc) % 2
            obank = 4 + ob
            sbs = {}

            def st_s(kc):
                nonlocal step
                if kc % 8 == 0:
                    conv_chunk()
                sb = step % 3
                step += 1
                sbs[kc] = sb
                k.op("pe", lambda e: e.matmul(
                    PS[sb][:, :], KT[pr, kc * 128:(kc + 1) * 128], QT[pr, a, qc * 512:(qc + 1) * 512],
                    start=True, stop=True), writes=["ps%d" % sb])
                k.op("act", lambda e: e.activation(
                    pT[sb], PS[sb][:, :], AF.Exp, scale=0.125, bias=negmb),
                    reads=["ps%d" % sb], writes=[("pT", sb)])

            def st_pv(kc):
                sb = sbs[kc]
                wr = ["ps%d" % obank] if kc in (0, NKC - 1) else []
                k.op("pe", lambda e: e.matmul(
                    PS[obank][0:65, :], VX[:, kc, g, :], pT[sb], start=(kc == 0), stop=(kc == NKC - 1)),
                    reads=[("pT", sb)], writes=wr)
            st_s(0)
            for kc in range(1, NKC):
                st_s(kc)
                st_pv(kc - 1)
            st_pv(NKC - 1)
            k.op("act", lambda e, ob=ob, obank=obank: e.copy(osb[ob][0:65, :], PS[obank][0:65, :]),
                 reads=["ps%d" % obank], writes=[("osb", ob)])
            k.op("pe", lambda e, ob=ob: e.matmul(PS[6][0:64, :], onesf[64:65, 0:64], osb[ob][64:65, :], start=True, stop=True),
                 reads=[("osb", ob), "onesf"], writes=["ps6"])
            k.op("dve", lambda e: e.reciprocal(rec[0:64, :], PS[6][0:64, :]), reads=["ps6"], writes=["rec"])
            k.op("dve", lambda e, ob=ob: e.tensor_tensor(aT[ob][0:64, :], osb[ob][0:64, :], rec[0:64, :], ALU.mult),
                 reads=["rec", ("osb", ob)], writes=[("aT", ob)])
            k.dma("pool", attnT_d[h, :, qc * 512:(qc + 1) * 512], aT[ob][0:64, :], reads=[("aT", ob)], writes=["attnT_d"])
    if "attn" in debug:
        af = A.alloc([128, 512], BF16)
        for h in range(8):
            k.dma("sp", af[0:64, :], attnT_d[h, :, 0:512], reads=["attnT_d"], writes=["af"])
            k.dma("sp", dbg_out("attnT%d" % h, [64, 512], BF16), af[0:64, :], reads=["af"])
    k.barrier()
    A.reset(P3)

    if stop == 'attn':
        return nc, IN, DBG, k
    A.reset(PERSIST)
    hy_sb = A.alloc([128, NT, HYC], BF16)
    PERSIST2 = A.off
    NF = 8192.0
    TWO_PI = 6.283185307179586

    def load_tab(dram, shape, dt, pieces=1):
        stg_ = A.alloc(shape, F32)
        tab = A.alloc(shape, dt) if dt != F32 else stg_
        flat = (lambda ap: ap if len(shape) == 2 else ap.rearrange("p a b -> p (a b)") if len(shape) == 3 else ap.rearrange("p a b c -> p (a b c)"))
        n = int(np.prod(shape[1:]))
        step = n // pieces
        for q in range(pieces):
            k.dma("sp", flat(stg_)[0:shape[0], q * step:(q + 1) * step], dram[:, q * step:(q + 1) * step], writes=[("tabstg", id(stg_), q)])
        if dt != F32:
            k.op("dve", lambda e: e.tensor_copy(flat(tab), flat(stg_)), reads=[("tabstg", id(stg_), q) for q in range(pieces)], writes=[("tab", id(tab))])
        return tab
    W1 = load_tab(W1_d, [128, 512], BF16)
    KC = load_tab(KC_d, [128, 128], BF16)
    KS = load_tab(KS_d, [128, 128], BF16)
    NKS = load_tab(NKS_d, [128, 128], BF16)
    R1 = load_tab(R1_d, [128, 256], BF16)
    R2 = load_tab(R2_d, [128, 256], BF16)
    C2 = load_tab(C2_d, [128, 2, 128], BF16)
    NS2 = load_tab(NS2_d, [128, 2, 128], BF16)
    TW1 = load_tab(TW1_d, [128, 768], F32)
    TW2 = load_tab(TW2_d, [128, 2, 3, 128], F32)
    skipN = load_tab(skipT_d, [128, 2, 128], F32)
    k.op("dve", lambda e: e.tensor_scalar(skipN, skipN, 1.0 / NF, None, ALU.mult),
         reads=[("tabstg", id(skipN), 0)], writes=["skipN"])
    k.barrier()
    PERSIST_T = A.off
    def fft_A(u_flat, rkeys, sl):
        k.op("pe", lambda e: e.matmul(PS[sl][:, :], u_flat, W1, start=True, stop=True), reads=rkeys, writes=["ps%d" % sl])
        t1 = ft1[sl]; t2 = ft2[sl]; ap_ = fap[sl]
        k.op("dve", lambda e: e.tensor_tensor(t1.rearrange("p (r f) -> p r f", r=2), PS[sl][:, :].rearrange("p (r f) -> p r f", r=2),
                                              bcast(TW1[:, 0:256].unsqueeze(1), [128, 2, 256]), ALU.mult),
             reads=["ps%d" % sl], writes=[("ft1", sl)])
        k.op("dve", lambda e: e.tensor_tensor(t2[:, 0:256], PS[sl][:, 256:512], TW1[:, 256:512], ALU.mult),
             reads=["ps%d" % sl], writes=[("ft2a", sl)])
        k.op("dve", lambda e: e.tensor_tensor(t2[:, 256:512], PS[sl][:, 0:256], TW1[:, 512:768], ALU.mult),
             reads=["ps%d" % sl], writes=[("ft2b", sl)])
        k.op("pool", lambda e: e.tensor_tensor(ap_, t1, t2, ALU.add),
             reads=[("ft1", sl), ("ft2a", sl), ("ft2b", sl)], writes=[("fap", sl)])

    def fft_B(sl, bb):
        ap_ = fap[sl]

        def f2(e):
            e.matmul(PS[bb][:, 0:256], KC, ap_[:, 0:256], start=True, stop=False)
            e.matmul(PS[bb][:, 0:256], KS, ap_[:, 256:512], start=False, stop=True)
            e.matmul(PS[bb][:, 256:512], KC, ap_[:, 256:512], start=True, stop=False)
            return e.matmul(PS[bb][:, 256:512], NKS, ap_[:, 0:256], start=False, stop=True)
        k.op("pe", f2, reads=[("fap", sl)], writes=["ps%d" % bb])
        return bb

    ft1 = [A.alloc([128, 512], F32) for _ in range(4)]
    ft2 = [A.alloc([128, 512], F32) for _ in range(4)]
    fap = [A.alloc([128, 512], BF16) for _ in range(4)]
    FWORK = A.off
    hA = A.alloc([128, L], F32)
    zT = A.alloc([128, L], F32)
    hB = A.alloc([128, L], F32)
    w123 = A.alloc([128, 3, 64], F32)
    bfr = A.alloc([128, 8], F32)
    for q in range(8):
        k.dma("sp", zT[0:33, q * 512:(q + 1) * 512], zT_d[:, q * 512:(q + 1) * 512], writes=[("zT", q)])
    k.dma("sp", w123[0:33, 0, :], hfw1_d, writes=["w1"])
    k.dma("sp", w123[0:64, 1, :], hfw2_d, writes=["w2"])
    k.dma("sp", w123[0:64, 2, :], hfw3_d, writes=["w3"])
    k.dma("sp", bfr[0:64, 0:4], hfb_d, writes=["bfr"])
    k.op("dve", lambda e: e.tensor_scalar(bfr[0:64, 4:5], bfr[0:64, 3:4], 1.0 / TWO_PI, None, ALU.mult), reads=["bfr"], writes=["bfr2"])
    ri_ = A.alloc([128, 512], I32)
    rr_ = A.alloc([128, 512], F32)
    srcs = [zT, hA, hB, hA]
    Ks_ = [33, 64, 64]
    for layer in range(3):
        src_, dst_ = srcs[layer], srcs[layer + 1]
        K_ = Ks_[layer]
        for q in range(8):
            bk = q % 2
            k.op("pe", lambda e: e.matmul(PS[bk][0:64, :], w123[0:K_, layer, :], src_[0:K_, q * 512:(q + 1) * 512], start=True, stop=True),
                 reads=[("zT", q), "w1", "w2", "w3", ("hm", layer, q)], writes=["ps%d" % bk])
            k.op("dve", lambda e: e.tensor_scalar(rr_[0:64, :], PS[bk][0:64, :], bfr[0:64, layer:layer + 1], bfr[0:64, 4:5], ALU.add, ALU.mult),
                 reads=["ps%d" % bk, "bfr", "bfr2"], writes=["rr"])
            k.op("dve", lambda e: e.tensor_copy(ri_[0:64, :], rr_[0:64, :]), reads=["rr"], writes=["ri"])
            k.op("dve", lambda e: e.tensor_tensor(rr_[0:64, :], rr_[0:64, :], ri_[0:64, :], ALU.subtract), reads=["rr", "ri"], writes=["rr"])
            k.op("act", lambda e: e.activation(dst_[0:64, q * 512:(q + 1) * 512], rr_[0:64, :], AF.Sin, scale=TWO_PI * 0.999999),
                 reads=["rr"], writes=[("hm", layer + 1, q)])
    k.barrier()
    A.reset(FWORK)
    hm3 = A.alloc([128, L], F32)
    w4s = A.alloc([128, 2048], F32)
    for q in range(4):
        k.dma("pool", w4s[0:64, q * 512:(q + 1) * 512], hfw4_d[:, q * 512:(q + 1) * 512], writes=[("w4", q)])
    t01bc = A.alloc([128, L], F32)
    for q in range(8):
        k.dma("sp", t01bc[:, q * 512:(q + 1) * 512], bcast(t01_d[:, q * 512:(q + 1) * 512], [128, 512]), writes=[("t01", q)])
    ndel = A.alloc([128, 4], F32)
    k.dma("sp", ndel, ndelT_d, writes=["ndel"])
    decay = A.alloc([128, L], F32)
    hraw = [A.alloc([128, L], F32) for _ in range(2)]
    junkb = A.alloc([128, L // 2], BF16)
    hf = [A.alloc([128, 32, 32, 4], BF16) for _ in range(2)]
    ssum = A.alloc([128, 8], F32)
    ffs = A.alloc([128, 512], F32)
    ktmp = A.alloc([128, 512], F32)
    kf3 = [A.alloc([128, 768], F32) for _ in range(2)]
    kfn = 0
    for cc in range(4):
        for q in range(8):
            k.op("act", lambda e, q=q: e.activation(decay[:, q * 512:(q + 1) * 512], t01bc[:, q * 512:(q + 1) * 512], AF.Exp, scale=ndel[:, cc:cc + 1]),
                 reads=[("t01", q), "ndel"], writes=[("decay", q)])
        for o in range(2):
            for dr in range(2):
                col0 = o * 1024 + dr * 512 + cc * 128
                for q in range(8):
                    bk = 4 + q % 2
                    k.op("pe", lambda e, q=q: e.matmul(PS[bk][:, :], w4s[0:64, col0:col0 + 128], hm3[0:64, q * 512:(q + 1) * 512], start=True, stop=True),
                         reads=[("w4", col0 // 512)], writes=["ps%d" % bk])
                    k.op("dve", lambda e, q=q: e.tensor_tensor(hraw[dr][:, q * 512:(q + 1) * 512], PS[bk][:, :], decay[:, q * 512:(q + 1) * 512], ALU.mult),
                         reads=["ps%d" % bk, ("decay", q)], writes=[("hraw", dr, q)])
                if dr == 1:
                    k.op("dve", lambda e: e.memset(hraw[1][:, 0:1], 0.0), reads=[("hraw", 1, 0)], writes=[("hraw", 1, 0)])
                for hh_ in range(2):
                    k.op("act", lambda e, dr=dr, hh_=hh_: e.activation(junkb, hraw[dr][:, hh_ * 2048:(hh_ + 1) * 2048], AF.Abs,
                                                                    accum_out=ssum[:, 4 + 2 * dr + hh_:5 + 2 * dr + hh_]),
                         reads=[("hraw", dr, q) for q in range(8)], writes=["junkb", ("ssum", dr, hh_)])
            k.op("dve", lambda e: e.tensor_reduce(ssum[:, 0:1], ssum[:, 4:8], AX.X, ALU.add),
                 reads=[("ssum", d_, h_) for d_ in range(2) for h_ in range(2)], writes=["ssum0"])
            k.op("dve", lambda e: e.tensor_scalar(ssum[:, 2:3], ssum[:, 0:1], EPS, None, ALU.add), reads=["ssum0"], writes=["ssum2"])
            k.op("dve", lambda e: e.reciprocal(ssum[:, 3:4], ssum[:, 2:3]), reads=["ssum2"], writes=["ssum3"])
            for dr in range(2):
                for q in range(8):
                    k.op("act", lambda e, dr=dr, q=q: e.activation(hraw[dr][:, q * 512:(q + 1) * 512], hraw[dr][:, q * 512:(q + 1) * 512], AF.Identity, scale=ssum[:, 3:4]),
                         reads=["ssum3", "junkb", ("hraw", dr, q)], writes=[("hraw", dr, q)])
                hv = hraw[dr].rearrange("p (a i) -> p a i", i=32)
                for i4 in range(8):
                    bk = 6 + i4 % 2

                    def trf(e, i4=i4, bk=bk, hv=hv):
                        ins = None
                        for j in range(4):
                            ins = e.transpose(PS[bk][:, j * 128:(j + 1) * 128], hv[:, :, i4 * 4 + j], ident)
                        return ins
                    k.op("pe", trf, reads=[("hraw", dr, q) for q in range(8)] + ["ident"], writes=["ps%d" % bk])
                    k.op("act", lambda e, i4=i4, bk=bk, dr=dr: e.copy(
                        hf[dr][:, :, i4 * 4:(i4 + 1) * 4, :].rearrange("p g i c -> p i g c"),
                        PS[bk][:, :].rearrange("p (i g c) -> p i g c", i=4, g=32)),
                        reads=["ps%d" % bk], writes=[("hf", dr, i4)])
            hkeys0 = [("hf", 0, i4) for i4 in range(8)]
            hkeys1 = [("hf", 1, i4) for i4 in range(8)]

            def stA(g):
                sl = 2 * (g % 2)
                fft_A(hf[0][:, g, :, :].rearrange("p i c -> p (i c)"), hkeys0, sl)
                fft_A(hf[1][:, g, :, :].rearrange("p i c -> p (i c)"), hkeys1, sl + 1)

            def stB(g):
                nonlocal kfn
                G = cc * 32 + g
                sl = 2 * (g % 2)
                bf_ = fft_B(sl, 4 + sl)
                k.op("act", lambda e: e.copy(ffs, PS[bf_][:, :]), reads=["ps%d" % bf_], writes=["ffs"])
                bb_ = fft_B(sl + 1, 5 + sl)
                kk_ = kf3[kfn % 2]; kfn += 1
                k.op("dve", lambda e: e.tensor_tensor(ktmp[:, 0:256], PS[bb_][:, 0:256], ffs[:, 0:256], ALU.add),
                     reads=["ps%d" % bb_, "ffs"], writes=["ktmpa"])
                k.op("dve", lambda e: e.tensor_tensor(ktmp[:, 256:512], ffs[:, 256:512], PS[bb_][:, 256:512], ALU.subtract),
                     reads=["ps%d" % bb_, "ffs"], writes=["ktmpb"])
                k.op("dve", lambda e: e.tensor_scalar(kk_[:, 0:256], ktmp[:, 0:256], 1.0 / NF, skipN[:, o, G:G + 1], ALU.mult, ALU.add),
                     reads=["ktmpa", "skipN"], writes=[("kf3a", kfn % 2)])
                k.op("act", lambda e: e.activation(kk_[:, 256:512], ktmp[:, 256:512], AF.Identity, scale=-1.0 / NF),
                     reads=["ktmpb"], writes=[("kf3b", kfn % 2)])
                k.op("act", lambda e: e.activation(kk_[:, 512:768], ktmp[:, 256:512], AF.Identity, scale=1.0 / NF),
                     reads=["ktmpb"], writes=[("kf3c", kfn % 2)])
                k.dma("sp", kf_d[o, G, :, :], kk_, reads=[("kf3a", kfn % 2), ("kf3b", kfn % 2), ("kf3c", kfn % 2)], writes=[("kfd", o, G)])
            stA(0)
            for g in range(1, 32):
                stA(g)
                stB(g - 1)
            stB(31)
    if "kf" in debug:
        k.barrier()
        for o in range(2):
            for G in (0, 77):
                dk_ = dbg_out("kf_%d_%d" % (o, G), [128, 768])
                k.dma("sp", kf3[0], kf_d[o, G, :, :], writes=["kkdbg"])
                k.dma("sp", dk_, kf3[0], reads=["kkdbg"], writes=["kkdbg2"])
                k.barrier()
    k.barrier()
    if stop == 'filt':
        return nc, IN, DBG, k
    A.reset(PERSIST_T)
    ft1 = [A.alloc([128, 512], F32) for _ in range(4)]
    ft2 = [A.alloc([128, 512], F32) for _ in range(4)]
    fap = [A.alloc([128, 512], BF16) for _ in range(4)]

    cw = A.alloc([128, 3, 3 * HYC], F32)
    cb = A.alloc([128, 3 * HYC], F32)
    for j in range(3):
        for hh in range(3):
            k.dma("sp", cw[:, j, hh * 512:(hh + 1) * 512], bcast(hcw_d[j:j + 1, hh * 512:(hh + 1) * 512], [128, 512]), writes=[("cw", j, hh)])
    for hh in range(3):
        k.dma("sp", cb[:, hh * 512:(hh + 1) * 512], bcast(hcb_d[:, hh * 512:(hh + 1) * 512], [128, 512]), writes=[("cb", hh)])
    k.barrier()
    CH = 64
    Zb = [A.alloc([128, 34, CH], F32) for _ in range(3)]
    zt_ = A.alloc([128, 32, CH], F32)
    zu_ = A.alloc([128, 32, CH], F32)
    xg = [A.alloc([128, 32, CH], F32) for _ in range(2)]
    u1 = A.alloc([128, 16, 32, 4], BF16)
    u2 = A.alloc([128, 16, 32, 4], BF16)
    kfb = [A.alloc([128, 768], F32) for _ in range(3)]
    pt1 = [A.alloc([128, 512], F32) for _ in range(2)]
    pt2 = [A.alloc([128, 512], F32) for _ in range(2)]
    ysb = [A.alloc([128, 512], BF16) for _ in range(2)]
    it1 = [A.alloc([128, 2, 2, 128], F32) for _ in range(2)]
    it2 = [A.alloc([128, 2, 2, 128], F32) for _ in range(2)]
    Bbuf = [A.alloc([128, 2, 2, 4, 128], BF16) for _ in range(2)]
    zmain = zhy_d[0:L, :].rearrange("(p j) c -> p j c", j=32)
    zext = zhy_d[2:L + 2, :].rearrange("(p j) c -> p j c", j=32)
    cnt = [0]

    def conv(u_in, ukey, o, G0, gate, gkey, writer):
        base = cnt[0]
        cnt[0] += 16

        def stA(g):
            n = base + g
            k.dma("pool", kfb[n % 3], kf_d[o, G0 + g, :, :], reads=[("kfd", o, G0 + g)], writes=[("kfb", n % 3)])
            fft_A(u_in[:, g, :, :].rearrange("p i c -> p (i c)"), [ukey], n % 2)

        def stB(g):
            n = base + g
            kfbuf = kfb[n % 3]
            bu = fft_B(n % 2, 2 + n % 2)
            a1 = pt1[n % 2]; a2 = pt2[n % 2]; ys = ysb[n % 2]
            k.op("dve", lambda e: e.tensor_tensor(a1.rearrange("p (r f) -> p r f", r=2), PS[bu][:, :].rearrange("p (r f) -> p r f", r=2),
                                                  bcast(kfbuf[:, 0:256].unsqueeze(1), [128, 2, 256]), ALU.mult),
                 reads=["ps%d" % bu, ("kfb", n % 3)], writes=[("pt1", n % 2)])
            k.op("dve", lambda e: e.tensor_tensor(a2[:, 0:256], PS[bu][:, 256:512], kfbuf[:, 256:512], ALU.mult),
                 reads=["ps%d" % bu, ("kfb", n % 3)], writes=[("pt2a", n % 2)])
            k.op("dve", lambda e: e.tensor_tensor(a2[:, 256:512], PS[bu][:, 0:256], kfbuf[:, 512:768], ALU.mult),
                 reads=["ps%d" % bu, ("kfb", n % 3)], writes=[("pt2b", n % 2)])
            k.op("pool", lambda e: e.tensor_tensor(ys, a1, a2, ALU.add),
                 reads=[("pt1", n % 2), ("pt2a", n % 2), ("pt2b", n % 2)], writes=[("ysb", n % 2)])

        def stC(g):
            n = base + g
            ys = ysb[n % 2]
            bi = 4 + n % 2

            def i1(e):
                ins = None
                for hh in range(2):
                    e.matmul(PS[bi][:, hh * 256:(hh + 1) * 256], ys[:, hh * 128:(hh + 1) * 128], R1, start=True, stop=False)
                    ins = e.matmul(PS[bi][:, hh * 256:(hh + 1) * 256], ys[:, 256 + hh * 128:256 + (hh + 1) * 128], R2, start=False, stop=True)
                return ins
            k.op("pe", i1, reads=[("ysb", n % 2)], writes=["ps%d" % bi])
            b1 = it1[n % 2]; b2 = it2[n % 2]
            Bv = PS[bi][:, :].rearrange("p (h r x) -> p h r x", h=2, r=2)
            k.op("dve", lambda e: e.tensor_tensor(b1, Bv, bcast(TW2[:, :, 0:1, :], [128, 2, 2, 128]), ALU.mult),
                 reads=["ps%d" % bi], writes=[("it1", n % 2)])
            k.op("dve", lambda e: e.tensor_tensor(b2[:, :, 0, :], Bv[:, :, 1, :], TW2[:, :, 1, :], ALU.mult),
                 reads=["ps%d" % bi], writes=[("it2a", n % 2)])
            k.op("dve", lambda e: e.tensor_tensor(b2[:, :, 1, :], Bv[:, :, 0, :], TW2[:, :, 2, :], ALU.mult),
                 reads=["ps%d" % bi], writes=[("it2b", n % 2)])
            q4, slot = (n // 4), n % 4
            Bb = Bbuf[q4 % 2]
            k.op("pool", lambda e: e.tensor_tensor(Bb[:, :, :, slot, :], b1, b2, ALU.add),
                 reads=[("it1", n % 2), ("it2a", n % 2), ("it2b", n % 2)], writes=[("Bbuf", q4 % 2, slot)])
            if slot == 3:
                bo = 6 + q4 % 2

                def i2(e):
                    e.matmul(PS[bo][:, :], C2[:, 0, :], Bb[:, 0, 0, :, :].rearrange("p s x -> p (s x)"), start=True, stop=False)
                    e.matmul(PS[bo][:, :], NS2[:, 0, :], Bb[:, 0, 1, :, :].rearrange("p s x -> p (s x)"), start=False, stop=False)
                    e.matmul(PS[bo][:, :], C2[:, 1, :], Bb[:, 1, 0, :, :].rearrange("p s x -> p (s x)"), start=False, stop=False)
                    return e.matmul(PS[bo][:, :], NS2[:, 1, :], Bb[:, 1, 1, :, :].rearrange("p s x -> p (s x)"), start=False, stop=True)
                k.op("pe", i2, reads=[("Bbuf", q4 % 2, s_) for s_ in range(4)], writes=["ps%d" % bo])
                gq = g // 4
                yv = PS[bo][:, :].rearrange("p (s i c) -> p s i c", s=4, i=32)
                gv = gate[:, :, gq * 16:(gq + 1) * 16].rearrange("p i (s c) -> p s i c", s=4)
                writer(gq, yv, gv, "ps%d" % bo, gkey)
        for st in range(18):
            if st < 16:
                stA(st)
            if 1 <= st <= 16:
                stB(st - 1)
            if 2 <= st <= 17:
                stC(st - 2)

    for chn in range(HYC // CH):
        ch0 = chn * CH
        G0 = chn * 16
        for kind in range(3):
            col0 = kind * HYC + ch0
            k.dma("sp", Zb[kind][:, 0:16, :], zmain[:, 0:16, col0:col0 + CH], writes=[("Zb", kind, 0)])
            k.dma("sp", Zb[kind][:, 16:32, :], zmain[:, 16:32, col0:col0 + CH], writes=[("Zb", kind, 1)])
            k.dma("sp", Zb[kind][:, 32:34, :], zext[:, 30:32, col0:col0 + CH], writes=[("Zb", kind, 2)])
            zk = [("Zb", kind, j) for j in range(3)]
            wv = lambda j: bcast(cw[:, j:j + 1, col0:col0 + CH], [128, 32, CH])
            k.op("dve", lambda e: e.tensor_tensor(zt_, Zb[kind][:, 0:32, :], wv(0), ALU.mult), reads=zk, writes=["zt"])
            k.op("pool", lambda e: e.tensor_tensor(zu_, Zb[kind][:, 1:33, :], wv(1), ALU.mult), reads=zk, writes=["zu"])
            k.op("dve", lambda e: e.tensor_tensor(zt_, zt_, zu_, ALU.add), reads=["zt", "zu"], writes=["zt"])
            k.op("pool", lambda e: e.tensor_tensor(zu_, Zb[kind][:, 2:34, :], wv(2), ALU.mult), reads=zk + ["zt"], writes=["zu"])
            k.op("dve", lambda e: e.tensor_tensor(zt_, zt_, zu_, ALU.add), reads=["zt", "zu"], writes=["zt"])
            bv = bcast(cb[:, col0:col0 + CH].unsqueeze(1), [128, 32, CH])
            if kind == 0:
                k.op("dve", lambda e: e.tensor_tensor(u1.rearrange("p g i c -> p i g c"), zt_.rearrange("p i (g c) -> p i g c", c=4),
                                                      bv.rearrange("p i (g c) -> p i g c", c=4) if False else bcast(cb[:, col0:col0 + CH].rearrange("p (g c) -> p g c", c=4).unsqueeze(1), [128, 32, 16, 4]), ALU.add),
                     reads=["zt"], writes=["u1"])
            else:
                k.op("dve", lambda e: e.tensor_tensor(xg[kind - 1], zt_, bv, ALU.add), reads=["zt"], writes=[("xg", kind - 1)])

        def w1_(gq, yv, gv, pkey, gkey):
            k.op("dve", lambda e: e.tensor_tensor(u2[:, gq * 4:(gq + 1) * 4, :, :], yv, gv, ALU.mult), reads=[pkey, gkey], writes=["u2"])

        def w2_(gq, yv, gv, pkey, gkey):
            ov = hy_sb[:, :, ch0 + gq * 16:ch0 + (gq + 1) * 16].rearrange("p i (s c) -> p s i c", s=4)
            k.op("dve", lambda e: e.tensor_tensor(ov, yv, gv, ALU.mult), reads=[pkey, gkey], writes=[("hy", chn, gq)])
        conv(u1, "u1", 0, G0, xg[0], ("xg", 0), w1_)
        conv(u2, "u2", 1, G0, xg[1], ("xg", 1), w2_)
    if "hy" in debug:
        k.barrier()
        hyf = A.alloc([128, 4, HYC], F32) if False else pt1[0]
        for i_ in range(4):
            k.op("dve", lambda e, i_=i_: e.tensor_copy(hyf, hy_sb[:, i_, :]), writes=["hyf"])
            k.dma("sp", dbg_out("hy%d" % i_, [128, HYC]), hyf, reads=["hyf"], writes=["hyfd"])
            k.barrier()
    k.barrier()
    if stop == 'hy':
        return nc, IN, DBG, k
    A.reset(PERSIST2)
    wao = A.alloc([128, 4, D], BF16)
    wout = A.alloc([128, 8, D], BF16)
    why = A.alloc([128, 4, D], BF16)
    wqb = A.alloc([128, 8, 2048], BF16)
    keysT = A.alloc([128, 16, 128], BF16)
    g1bc = A.alloc([128, D], F32)
    g2bc = A.alloc([128, D], F32)
    A2bc = A.alloc([128, D], BF16)
    B2bc = A.alloc([128, D], BF16)
    gfin = A.alloc([128, D], F32)
    iota16 = A.alloc([128, 16], F32)
    PB = A.off
    rep = A.alloc([128, 128], F32)
    stg = [A.alloc([128, D], F32) for _ in range(2)]
    io_ = A.alloc([128, 16], I32)
    k.op("pool", lambda e: e.iota(io_, pattern=[[1, 16]], base=0, channel_multiplier=0), writes=["io_"])
    k.op("dve", lambda e: e.tensor_copy(iota16, io_), reads=["io_"], writes=["iota16"])
    k.dma("sp", gfin, bcast(gfin_d, [128, D]), writes=["gfin"])

    def load_w(dst_fn, src_fn, n):
        for j in range(n):
            b = j % 2
            k.dma("sp" if b == 0 else "pool", stg[b], src_fn(j), writes=[("stg", b)])
            if b == 0:
                k.op("act", lambda e, j=j: e.copy(dst_fn(j), stg[0]), reads=[("stg", 0)], writes=[("wload", id(dst_fn), j)])
            else:
                k.op("dve", lambda e, j=j: e.tensor_copy(dst_fn(j), stg[1]), reads=[("stg", 1)], writes=[("wload", id(dst_fn), j)])
    load_w(lambda j: wao[:, j, :], lambda j: wao_d[j * 128:(j + 1) * 128, :], 4)
    load_w(lambda j: wout[:, j, :], lambda j: wout_d[j * 128:(j + 1) * 128, :], 8)
    load_w(lambda j: why[:, j, :], lambda j: why_d[j * 128:(j + 1) * 128, :], 4)
    load_w(lambda j: wqb[:, j // 2, (j % 2) * 1024:(j % 2 + 1) * 1024], lambda j: wq_d[(j // 2) * 128:(j // 2 + 1) * 128, (j % 2) * 1024:(j % 2 + 1) * 1024], 16)
    load_w(lambda j: keysT[:, j * 8:(j + 1) * 8, :].rearrange("p a n -> p (a n)"), lambda j: keysT_d[:, j * 1024:(j + 1) * 1024], 2)
    bcl = [(g1bc, lambda j: modT[:, 16 + j, 0:1]), (g2bc, lambda j: modT[:, 40 + j, 0:1]),
           (A2bc, lambda j: A2[:, j:j + 1]), (B2bc, lambda j: modT[:, 24 + j, 0:1])]
    for bi_, (dst, colf) in enumerate(bcl):
        for j in range(8):
            k.op("dve", lambda e, j=j, colf=colf: e.tensor_copy(rep, bcast(colf(j), [128, 128])), reads=["modT", "A2"], writes=["rep"])
            k.op("pe", lambda e, j=j: e.matmul(PS[j // 4][:, (j % 4) * 128:(j % 4 + 1) * 128], rep, ident, start=True, stop=True),
                 reads=["rep", "ident"], writes=["ps%d" % (j // 4)])
        for hf in range(2):
            k.op("act", lambda e, hf=hf, dst=dst: e.copy(dst[:, hf * 512:(hf + 1) * 512], PS[hf][:, :]), reads=["ps%d" % hf], writes=[("bc", bi_, hf)])
    k.barrier()
    A.reset(PB)
    aTt = A.alloc([128, 4, 128], BF16)
    gsb = A.alloc([128, 2 * D], BF16)
    xs = A.alloc([128, D], F32)
    mg = A.alloc([128, D], F32)
    mT = A.alloc([128, 8, 128], BF16)
    x1 = A.alloc([128, D], F32)
    ob = A.alloc([128, D], F32)
    hyT = A.alloc([128, 4, 128], BF16)
    sB = A.alloc([128, 16], F32)
    qTs = A.alloc([128, 16, 128], BF16)
    big8 = A.alloc([128, 2048], F32)
    hyf = big8[:, 0:HYC]
    mx = A.alloc([128, 16, 16], F32)
    mi = A.alloc([128, 16, 16], U32)
    mif = A.alloc([128, 16, 16], F32)
    wk4 = A.alloc([128, 4, 256], F32)
    best = A.alloc([128, 8, 16], F32)
    pos = A.alloc([128, 8, 16], U32)
    pia = A.alloc([128, 8, 16], U32)
    pib = A.alloc([128, 8, 16], U32)
    paf = A.alloc([128, 8, 16], F32)
    pbf = A.alloc([128, 8, 16], F32)
    isel = A.alloc([128, 2, 128], F32)
    eidf = A.alloc([128, 128], F32)
    eidx = A.alloc([128, 128], I32)
    gw = A.alloc([128, 8, 16], F32)
    sm8 = A.alloc([128, 16], F32)
    av = A.alloc([128, 128], F32)
    wgt = A.alloc([128, 128], F32)
    junkp = ob
    acc = xs
    gl = A.alloc([128, 128], F32)
    dg = [A.alloc([128, 128], BF16) for _ in range(4)]
    identb = A.alloc([128, 128], BF16)
    k.op("dve", lambda e: e.tensor_copy(identb, ident), reads=["ident"], writes=["identb"])
    NR = 9
    ring = [A.alloc([128, 2 * D], BF16) for _ in range(NR)]
    out_v = out_d.rearrange("(p i) d -> i p d", i=NT)
    attn_ev = attnT_d.rearrange("(j two) d t -> two d j t", two=2)
    rn = [0]
    sc3 = big8.rearrange("p (a n) -> p a n", a=16)
    cand = big8.rearrange("p (h a b) -> p h a b", h=8, a=16)
    mxv = mx.rearrange("p (h t) k -> p h t k", t=2)
    mifv = mif.rearrange("p (h t) k -> p h t k", t=2)
    npe = NT if "nopeer" not in skip else 0
    GW = 512 if "halfrow" in skip else D
    for it in range(NT):
        k.dma("sp", xs, x_v[it], writes=["xs"])
        for two in range(2):
            k.dma("pool", aTt[64 * two:64 * two + 64, :, :], attn_ev[two][:, :, it * 128:(it + 1) * 128], writes=[("aTt", two)])
        k.dma("sp", gsb, gates_v[it], writes=["gsb"])
        k.op("act", lambda e: e.copy(hyf, hy_sb[:, it, :]), writes=["hyf"])

        def trh(e):
            ins = None
            for j in range(4):
                ins = e.transpose(PS[6][:, j * 128:(j + 1) * 128], hyf[:, j * 128:(j + 1) * 128], ident)
            return ins
        k.op("pe", trh, reads=["hyf", "ident"], writes=["ps6"])
        k.op("act", lambda e: e.copy(hyT, PS[6][:, :].rearrange("p (j t) -> p j t", j=4)), reads=["ps6"], writes=["hyT"])
        for hf in range(2):
            def pa(e, hf=hf):
                ins = None
                for j in range(4):
                    ins = e.matmul(PS[hf][:, :], aTt[:, j, :], wao[:, j, hf * 512:(hf + 1) * 512], start=(j == 0), stop=(j == 3))
                return ins
            k.op("pe", pa, reads=[("aTt", 0), ("aTt", 1)], writes=["ps%d" % hf])
            k.op("dve", lambda e, hf=hf: e.tensor_tensor(mg[:, hf * 512:(hf + 1) * 512], PS[hf][:, :], gsb[:, hf * 512:(hf + 1) * 512], ALU.mult),
                 reads=["ps%d" % hf, "gsb"], writes=[("mg", hf)])

            def ph(e, hf=hf):
                ins = None
                for j in range(4):
                    ins = e.matmul(PS[6 + hf][:, :], hyT[:, j, :], why[:, j, hf * 512:(hf + 1) * 512], start=(j == 0), stop=(j == 3))
                return ins
            k.op("pe", ph, reads=["hyT"], writes=["ps%d" % (6 + hf)])
            k.op("dve", lambda e, hf=hf: e.tensor_tensor(ob[:, hf * 512:(hf + 1) * 512], PS[6 + hf][:, :], gsb[:, D + hf * 512:D + (hf + 1) * 512], ALU.mult),
                 reads=["ps%d" % (6 + hf), "gsb"], writes=[("ob", hf)])
            k.op("pool", lambda e, hf=hf: e.tensor_tensor(mg[:, hf * 512:(hf + 1) * 512], mg[:, hf * 512:(hf + 1) * 512], ob[:, hf * 512:(hf + 1) * 512], ALU.add),
                 reads=[("mg", hf), ("ob", hf)], writes=[("mg", hf)])
        for hf in range(2):
            def trm(e, hf=hf):
                ins = None
                for j in range(4):
                    jj = hf * 4 + j
                    ins = e.transpose(PS[2 + hf][:, j * 128:(j + 1) * 128], mg[:, jj * 128:(jj + 1) * 128], ident)
                return ins
            k.op("pe", trm, reads=[("mg", 0), ("mg", 1)], writes=["ps%d" % (2 + hf)])
            k.op("act", lambda e, hf=hf: e.copy(mT[:, hf * 4:(hf + 1) * 4, :], PS[2 + hf][:, :].rearrange("p (j t) -> p j t", j=4)),
                 reads=["ps%d" % (2 + hf)], writes=[("mT", hf)])
        for hf in range(2):
            def mo(e, hf=hf):
                ins = None
                for kk in range(8):
                    ins = e.matmul(PS[4 + hf][:, :], mT[:, kk, :], wout[:, kk, hf * 512:(hf + 1) * 512], start=(kk == 0), stop=(kk == 7))
                return ins
            k.op("pe", mo, reads=[("mT", 0), ("mT", 1)], writes=["ps%d" % (4 + hf)])
            k.op("dve", lambda e, hf=hf: e.tensor_tensor(x1[:, hf * 512:(hf + 1) * 512], PS[4 + hf][:, :], g1bc[:, hf * 512:(hf + 1) * 512], ALU.mult),
                 reads=["ps%d" % (4 + hf)], writes=[("x1", hf)])
        k.op("pool", lambda e: e.tensor_tensor(x1, x1, xs, ALU.add), reads=[("x1", 0), ("x1", 1), "xs"], writes=["x1s"])
        if "x1" in debug and it < 4:
            k.dma("sp", dbg_out("x1_%d" % it, [128, D]), x1, reads=["x1s"])
        s = sB
        if it < npe:
            k.op("act", lambda e: e.activation(mg, x1, AF.Square, accum_out=s[:, 4:5]), reads=["x1s", ("mg", 0), ("mg", 1)], writes=["mgx", "sB2"])
            k.op("dve", lambda e: e.tensor_scalar(s[:, 5:6], s[:, 4:5], 1.0 / D, EPS, ALU.mult, ALU.add), reads=["sB2"], writes=["sB2"])
            k.op("act", lambda e: e.activation(s[:, 6:7], s[:, 5:6], AF.Sqrt), reads=["sB2"], writes=["sB2"])
            k.op("dve", lambda e: e.reciprocal(s[:, 7:8], s[:, 6:7]), reads=["sB2"], writes=["sB2"])
            k.op("act", lambda e: e.activation(mg, x1, AF.Identity, scale=s[:, 7:8]), reads=["x1s", "sB2", "mgx"], writes=["mgx"])
            for hf in range(2):
                def tr2(e, hf=hf):
                    ins = None
                    for j in range(4):
                        jj = hf * 4 + j
                        ins = e.transpose(PS[hf][:, j * 128:(j + 1) * 128], mg[:, jj * 128:(jj + 1) * 128], ident)
                    return ins
                k.op("pe", tr2, reads=["mgx"], writes=["ps%d" % hf])
            for jj in range(8):
                hf, j = jj // 4, jj % 4
                if jj % 2 == 0:
                    k.op("act", lambda e, jj=jj, hf=hf, j=j: e.activation(mT[:, jj, :], PS[hf][:, j * 128:(j + 1) * 128], AF.Identity,
                                                                       scale=A2[:, jj:jj + 1], bias=modT[:, 24 + jj, 0:1]),
                         reads=["ps%d" % hf, ("mT", 0), ("mT", 1)], writes=[("h2T", jj)])
                else:
                    k.op("dve", lambda e, jj=jj, hf=hf, j=j: e.tensor_scalar(mT[:, jj, :], PS[hf][:, j * 128:(j + 1) * 128],
                                                                         A2[:, jj:jj + 1], modT[:, 24 + jj, 0:1], ALU.mult, ALU.add),
                         reads=["ps%d" % hf, ("mT", 0), ("mT", 1)], writes=[("h2T", jj)])
            k.op("dve", lambda e: e.tensor_tensor(mg, mg, A2bc, ALU.mult), reads=["mgx"], writes=["mgx"])
            k.op("pool", lambda e: e.tensor_tensor(mg, mg, B2bc, ALU.add), reads=["mgx"], writes=["h2"])
            h2keys = [("h2T", jj) for jj in range(8)]
            for qb in range(4):
                def qmm(e, qb=qb):
                    ins = None
                    for q4 in range(4):
                        hh = qb * 4 + q4
                        for kk in range(8):
                            ins = e.matmul(PS[2 + qb][:, q4 * 128:(q4 + 1) * 128], wqb[:, kk, hh * 128:(hh + 1) * 128], mT[:, kk, :],
                                           start=(kk == 0), stop=(kk == 7))
                    return ins
                k.op("pe", qmm, reads=h2keys, writes=["ps%d" % (2 + qb)])
                k.op("act", lambda e, qb=qb: e.copy(qTs[:, qb * 4:(qb + 1) * 4, :], PS[2 + qb][:, :].rearrange("p (a t) -> p a t", a=4)),
                     reads=["ps%d" % (2 + qb)], writes=[("qTs", qb)])
            sbanks = [6, 7, 0, 1]
            for qb in range(4):
                bk = sbanks[qb]

                def smm(e, qb=qb, bk=bk):
                    ins = None
                    for q4 in range(4):
                        hh = qb * 4 + q4
                        ins = e.matmul(PS[bk][:, q4 * 128:(q4 + 1) * 128], qTs[:, hh, :], keysT[:, hh, :], start=True, stop=True)
                    return ins
                k.op("pe", smm, reads=[("qTs", qb)], writes=["ps%d" % bk])
                if qb % 2 == 0:
                    k.op("act", lambda e, qb=qb, bk=bk: e.copy(big8[:, qb * 512:(qb + 1) * 512], PS[bk][:, :]), reads=["ps%d" % bk, "big8"], writes=[("sc", qb)])
                else:
                    k.op("dve", lambda e, qb=qb, bk=bk: e.tensor_copy(big8[:, qb * 512:(qb + 1) * 512], PS[bk][:, :]), reads=["ps%d" % bk, "big8"], writes=[("sc", qb)])
            for q4 in range(4):
                hhs = [4 * q4 + u for u in range(4)]
                sk_ = [("sc", q4)]
                for hh in hhs:
                    k.op("dve", lambda e, hh=hh: e.max(mx[:, hh, 0:8], sc3[:, hh, :]), reads=sk_, writes=[("mx", hh)])
                for hh in hhs:
                    k.op("dve", lambda e, hh=hh: e.max_index(mi[:, hh, 0:8], mx[:, hh, 0:8], sc3[:, hh, :]), reads=sk_ + [("mx", hh)], writes=[("mi", hh)])
                for u, hh in enumerate(hhs):
                    k.op("dve", lambda e, hh=hh, u=u: e.match_replace(wk4[:, u, 0:128], mx[:, hh, 0:8], sc3[:, hh, :], -1e30), reads=sk_ + [("mx", hh)], writes=[("wk", u)])
                for u, hh in enumerate(hhs):
                    k.op("dve", lambda e, hh=hh, u=u: e.max(mx[:, hh, 8:16], wk4[:, u, 0:128]), reads=[("wk", u)], writes=[("mx", hh)])
                for u, hh in enumerate(hhs):
                    k.op("dve", lambda e, hh=hh, u=u: e.max_index(mi[:, hh, 8:16], mx[:, hh, 8:16], wk4[:, u, 0:128]), reads=[("wk", u), ("mx", hh)], writes=[("mi", hh)])
            mxk = [("mx", hh) for hh in range(16)]
            mik = [("mi", hh) for hh in range(16)]
            k.op("dve", lambda e: e.tensor_copy(mif, mi), reads=mik, writes=["mif"])
            k.op("dve", lambda e: e.tensor_tensor(cand, bcast(mxv[:, :, 0, :].unsqueeze(3), [128, 8, 16, 16]),
                                                  bcast(mxv[:, :, 1, :].unsqueeze(2), [128, 8, 16, 16]), ALU.add),
                 reads=mxk + [("sc", q) for q in range(4)], writes=["big8"])
            candf = big8.rearrange("p (h x) -> p h x", h=8)
            for q4 in range(2):
                hs = [4 * q4 + u for u in range(4)]
                for h in hs:
                    k.op("dve", lambda e, h=h: e.max(best[:, h, 0:8], candf[:, h, :]), reads=["big8"], writes=[("best", h)])
                for h in hs:
                    k.op("dve", lambda e, h=h: e.max_index(pos[:, h, 0:8], best[:, h, 0:8], candf[:, h, :]), reads=["big8", ("best", h)], writes=[("pos", h)])
                for u, h in enumerate(hs):
                    k.op("dve", lambda e, h=h, u=u: e.match_replace(wk4[:, u, :], best[:, h, 0:8], candf[:, h, :], -1e30), reads=["big8", ("best", h)], writes=[("wk", u)])
                for u, h in enumerate(hs):
                    k.op("dve", lambda e, h=h, u=u: e.max(best[:, h, 8:16], wk4[:, u, :]), reads=[("wk", u)], writes=[("best", h)])
                for u, h in enumerate(hs):
                    k.op("dve", lambda e, h=h, u=u: e.max_index(pos[:, h, 8:16], best[:, h, 8:16], wk4[:, u, :]), reads=[("wk", u), ("best", h)], writes=[("pos", h)])
            bk_ = [("best", h) for h in range(8)]
            pk_ = [("pos", h) for h in range(8)]
            k.op("dve", lambda e: e.tensor_scalar(pia, pos, 4, None, ALU.arith_shift_right), reads=pk_, writes=["pia"])
            k.op("dve", lambda e: e.tensor_scalar(pib, pos, 15, None, ALU.bitwise_and), reads=pk_, writes=["pib"])
            k.op("dve", lambda e: e.tensor_copy(paf, pia), reads=["pia"], writes=["paf"])
            k.op("dve", lambda e: e.tensor_copy(pbf, pib), reads=["pib"], writes=["pbf"])
            oh = big8.rearrange("p (h a b) -> p h a b", h=8, a=16)
            for t_, pf_ in enumerate((paf, pbf)):
                k.op("dve", lambda e, pf_=pf_: e.tensor_tensor(oh, bcast(pf_.unsqueeze(3), [128, 8, 16, 16]),
                                                               bcast(iota16.unsqueeze(1).unsqueeze(1), [128, 8, 16, 16]), ALU.is_equal),
                     reads=["paf", "pbf", "iota16", "big8"] + bk_ + pk_, writes=["big8"])
                k.op("dve", lambda e, t_=t_: e.tensor_tensor(oh, oh, bcast(mifv[:, :, t_, :].unsqueeze(2), [128, 8, 16, 16]), ALU.mult),
                     reads=["big8", "mif"], writes=["big8"])
                k.op("dve", lambda e, t_=t_: e.tensor_reduce(isel[:, t_, :].rearrange("p (h a) -> p h a", h=8), oh, AX.X, ALU.add),
                     reads=["big8"], writes=[("isel", t_)])
            k.op("dve", lambda e: e.scalar_tensor_tensor(eidf, isel[:, 0, :], 128.0, isel[:, 1, :], ALU.mult, ALU.add),
                 reads=[("isel", 0), ("isel", 1)], writes=["eidf"])
            k.op("dve", lambda e: e.tensor_copy(eidx, eidf), reads=["eidf"], writes=["eidx"])
            k.op("dve", lambda e: e.tensor_tensor(gw, best, bcast(best[:, :, 0:1], [128, 8, 16]), ALU.subtract), reads=bk_, writes=["gw"])
            k.op("act", lambda e: e.activation(gw, gw, AF.Exp), reads=["gw"], writes=["gw"])
            k.op("dve", lambda e: e.tensor_reduce(sm8[:, 0:8], gw, AX.X, ALU.add), reads=["gw"], writes=["sm8"])
            k.op("dve", lambda e: e.reciprocal(sm8[:, 8:16], sm8[:, 0:8]), reads=["sm8"], writes=["sm8"])
            k.op("dve", lambda e: e.tensor_tensor(gw, gw, bcast(sm8[:, 8:16].unsqueeze(2), [128, 8, 16]), ALU.mult), reads=["gw", "sm8"], writes=["gw"])
            k.op("dve", lambda e: e.memset(av, 0.0), writes=["av"])
            gwf = gw.rearrange("p h k -> p (h k)")
            slots = {}
            for j in range(129):
                if j < 128:
                    r_ = rn[0] % NR; rn[0] += 1
                    slots[j] = r_
                    k.dma("pool", None, None, reads=["eidx"], writes=[("ring", r_)],
                          fn=lambda e, j=j, r_=r_: e.indirect_dma_start(out=ring[r_], out_offset=None, in_=uvb_d[:, :],
                                                                        in_offset=bass.IndirectOffsetOnAxis(ap=eidx[:, j:j + 1], axis=0)))
                    k.op("dve", lambda e, j=j, r_=r_: e.scalar_tensor_tensor(junkp, ring[r_][:, 0:D], 1.0, mg, ALU.mult, ALU.mult, accum_out=av[:, j:j + 1]),
                         reads=[("ring", r_), "h2", "av"], writes=[("av", j)])
                    k.op("act", lambda e, j=j: e.activation(gl[:, j:j + 1], av[:, j:j + 1], AF.Gelu), reads=[("av", j)], writes=[("gl", j)])
                if j >= 1:
                    jj = j - 1
                    r_ = slots[jj]
                    db = jj % 4
                    k.op("dve", lambda e, jj=jj, db=db: e.tensor_scalar(dg[db], identb, gl[:, jj:jj + 1], gwf[:, jj:jj + 1], ALU.mult, ALU.mult),
                         reads=[("gl", jj), "gw", "identb"], writes=[("dg", db)])

                    def vmm(e, jj=jj, db=db, r_=r_):
                        e.matmul(PS[2][:, :], dg[db], ring[r_][:, D:D + 512], start=(jj == 0), stop=(jj == 127))
                        return e.matmul(PS[3][:, :], dg[db], ring[r_][:, D + 512:2 * D], start=(jj == 0), stop=(jj == 127))
                    k.op("pe", vmm, reads=[("dg", db), ("ring", r_)], writes=(["ps2", "ps3"] if jj in (0, 127) else []))
            for hf in range(2):
                k.op("act", lambda e, hf=hf: e.copy(acc[:, hf * 512:(hf + 1) * 512], PS[2 + hf][:, :]), reads=["ps%d" % (2 + hf), "x1s"], writes=["xs"])
            if "peer" in debug and it < 2:
                k.dma("sp", dbg_out("peer_%d" % it, [128, D]), acc, reads=["xs"])
                k.dma("sp", dbg_out("h2_%d" % it, [128, D]), mg, reads=["h2"])
                k.dma("sp", dbg_out("eid_%d" % it, [128, 128]), eidf, reads=["eidf"])
            k.op("dve", lambda e: e.tensor_tensor(acc, acc, g2bc, ALU.mult), reads=["xs"], writes=["xs"])
            k.op("pool", lambda e: e.tensor_tensor(x1, x1, acc, ALU.add), reads=["xs", "x1s"], writes=["x1s"])
        k.op("act", lambda e: e.activation(ob, x1, AF.Square, accum_out=s[:, 0:1]), reads=["x1s", ("ob", 0), ("ob", 1)], writes=["obx", "sB"])
        k.op("dve", lambda e: e.tensor_scalar(s[:, 1:2], s[:, 0:1], 1.0 / D, EPS, ALU.mult, ALU.add), reads=["sB"], writes=["sB"])
        k.op("act", lambda e: e.activation(s[:, 2:3], s[:, 1:2], AF.Sqrt), reads=["sB"], writes=["sB"])
        k.op("dve", lambda e: e.reciprocal(s[:, 3:4], s[:, 2:3]), reads=["sB"], writes=["sB"])
        k.op("dve", lambda e: e.scalar_tensor_tensor(ob, x1, s[:, 3:4], gfin, ALU.mult, ALU.mult),
             reads=["x1s", "sB", "gfin", "obx"], writes=["obx"])
        k.dma("sp", out_v[it], ob, reads=["obx"], writes=["out"])
    k.barrier()
    return nc, IN, DBG, k


def rope_tables():
    p = np.arange(128)[:, None]
    i = np.arange(NT)[None, :]
    t = 32 * p + i
    row = (t // 64).astype(np.float32)
    col = (t % 64).astype(np.float32)
    inv = (10000.0 ** (-np.arange(0, 32, 2, dtype=np.float32) / 32)).astype(np.float32)
    ar = row[..., None] * inv
    ac = col[..., None] * inv
    ang = np.concatenate([ar, ar, ac, ac], axis=-1)
    cos = np.cos(ang).astype(np.float32)
    sin = np.sin(ang).astype(np.float32)
    sgn = np.ones(64, np.float32)
    sgn[0:16] = -1; sgn[32:48] = -1
    return cos.reshape(128, NT * 64), (sin * sgn).reshape(128, NT * 64)


def swap_halves(g):
    g = g.reshape(2, 2, 16)
    return np.ascontiguousarray(g[:, ::-1, :]).reshape(1, 64)


def fft_plan_tables():
    p = np.arange(128, dtype=np.float64)[:, None]
    f1 = np.arange(256, dtype=np.float64)[None, :]
    a = 2 * np.pi * p * f1 / 256
    W1 = np.concatenate([np.cos(a), -np.sin(a)], axis=1)
    P = np.arange(128)
    s2 = (P // 4).astype(np.float64)
    c4 = P % 4
    th = 2 * np.pi * s2[:, None] * s2[None, :] / 32
    dl = (c4[:, None] == c4[None, :]).astype(np.float64)
    KC = np.cos(th) * dl
    KS = np.sin(th) * dl
    R1 = np.concatenate([KC, KS], axis=1)
    R2 = np.concatenate([-KS, KC], axis=1)
    hh = np.arange(2, dtype=np.float64)[None, :, None]
    s1 = np.arange(128, dtype=np.float64)[None, None, :]
    a2 = 2 * np.pi * (128 * hh + p[:, :, None]) * s1 / 256
    C2 = np.cos(a2).reshape(128, 256)
    NS2 = (-np.sin(a2)).reshape(128, 256)
    ph = 2 * np.pi * s2[:, None] * f1 / 8192
    TW1 = np.concatenate([np.cos(ph), np.sin(ph), -np.sin(ph)], axis=1)
    f1b = (128 * np.arange(2, dtype=np.float64)[None, :, None] + p[:, :, None])
    ph2 = 2 * np.pi * s2[None, None, :] * f1b / 8192
    TW2 = np.stack([np.cos(ph2), -np.sin(ph2), np.sin(ph2)], axis=2).reshape(128, 768)
    f = lambda x: np.ascontiguousarray(x.astype(np.float32))
    return {"W1": f(W1), "KC": f(KC), "KS": f(KS), "NKS": f(-KS), "R1": f(R1), "R2": f(R2),
            "C2": f(C2), "NS2": f(NS2), "TW1": f(TW1), "TW2": f(TW2)}


def filter_consts():
    t01 = np.linspace(0.0, 1.0, L, dtype=np.float32)[:, None]
    w = (np.float32(2.0 * np.pi) * np.arange(L, dtype=np.float32)[:, None] / np.float32(L)).astype(np.float32)
    fb = np.linspace(1e-4, 15, 16, dtype=np.float32)[None]
    z = np.concatenate([t01, np.cos(fb * w), -np.sin(fb * w)], axis=-1).astype(np.float32)
    max_decay = np.log(1e-2) / 0.3
    min_decay = np.log(1e-2) / 1.5
    deltas = np.abs(np.linspace(min_decay, max_decay, HYC, dtype=np.float32))
    return {"zT": np.ascontiguousarray(z.T), "t01": np.ascontiguousarray(t01.T),
            "ndelT": np.ascontiguousarray((-deltas).reshape(4, 128).T.astype(np.float32))}


def make_in_maps(inputs):
    f = lambda a: np.ascontiguousarray(np.asarray(a, dtype=np.float32))
    cos, sin = rope_tables()
    shared = {
        "cctxT": f(inputs["c_ctx"].reshape(8, 128).T),
        "ada_w": f(inputs["ada_w"][0]),
        "ada_bT": f(inputs["ada_b"][0].reshape(48, 128).T),
        "gmixT": f(inputs["norm_mix_g"][0].reshape(8, 128).T),
        "gffnT": f(inputs["norm_ffn_g"][0].reshape(8, 128).T),
        "w_in": f(inputs["w_in"][0]),
        "rope_cos": cos, "rope_sin": sin,
        "gq": f(inputs["q_norm_g"][0].reshape(1, 64)),
        "gk": f(inputs["k_norm_g"][0].reshape(1, 64)),
        "gqsw": f(swap_halves(np.asarray(inputs["q_norm_g"][0]))),
        "gksw": f(swap_halves(np.asarray(inputs["k_norm_g"][0]))),
        "w_attn_out": f(inputs["w_attn_out"][0]),
        "w_out": f(inputs["w_out"][0]),
        "final_norm_g": f(inputs["final_norm_g"].reshape(1, D)),
        "w_hy_out": f(inputs["w_hy_out"][0]), "peer_wq": f(inputs["peer_wq"][0]),
        "keysT": f(np.stack([np.asarray(inputs["peer_keys1"][0]), np.asarray(inputs["peer_keys2"][0])], axis=1).transpose(3, 0, 1, 2).reshape(128, 2048)),
        "peer_u": f(inputs["peer_u"][0]), "peer_v": f(inputs["peer_v"][0]),
        "hf_w1": f(inputs["hf_w1"][0]), "hf_w2": f(inputs["hf_w2"][0]), "hf_w3": f(inputs["hf_w3"][0]), "hf_w4": f(inputs["hf_w4"][0]),
        "hf_b": f(np.stack([np.asarray(inputs["hf_b1"][0]), np.asarray(inputs["hf_b2"][0]), np.asarray(inputs["hf_b3"][0]), np.asarray(inputs["hf_freq"][0])], axis=1)),
        "hy_conv_w": f(inputs["hy_conv_w"][0]), "hy_conv_b": f(np.asarray(inputs["hy_conv_b"][0]).reshape(1, 3 * HYC)),
        "skipT": f(np.asarray(inputs["hy_skip"][0]).reshape(2, 128, 4)[:, :, np.arange(128) % 4].transpose(2, 0, 1).reshape(128, 256)),
    }
    shared.update(fft_plan_tables())
    shared.update(filter_consts())
    maps = []
    for b in range(8):
        m = dict(shared)
        m["x"] = f(inputs["x"][b])
        m["ctx"] = f(inputs["ctx"][b])
        m["cT"] = f(inputs["c"][b].reshape(8, 128).T)
        maps.append(m)
    return maps


def kernel(**inputs):
    nc, IN, DBG, k = build_program()
    maps = make_in_maps(inputs)
    maps = [{n: m[n] for n in IN} for m in maps]
    res = run_bass_kernel_spmd(nc, maps, core_ids=list(range(8)))
    out = np.stack([np.asarray(r["out"], dtype=np.float32) for r in res.results], axis=0)
    return out
```

```python
import numpy as np
import ml_dtypes
import concourse.bass as bass
import concourse.mybir as mybir
from concourse.bass_utils import run_bass_kernel_spmd

F32 = mybir.dt.float32
BF16 = mybir.dt.bfloat16
I32 = mybir.dt.int32
U32 = mybir.dt.uint32
U8 = mybir.dt.uint8
ALU = mybir.AluOpType
AF = mybir.ActivationFunctionType
AX = mybir.AxisListType

D = 1024
L = 4096
NT = 32
CTX = 256
EPS = 1e-6
NKEY = L + CTX
NKC = NKEY // 128
IN_COLS = 4352
HYC = 512


ATTACH_WAITS = True


class KB:
    def __init__(self, nc, n_dma_slots=32):
        self.nc = nc
        self.eng = {"pe": nc.tensor, "act": nc.scalar, "dve": nc.vector,
                    "pool": nc.gpsimd, "sp": nc.sync}
        self.sem, self.cnt, self.semobj = {}, {}, {}
        for n in self.eng:
            self.sem[n] = nc.alloc_semaphore("s_" + n)
            self.cnt[n] = 0
            self.semobj["s_" + n] = self.sem[n]
        self.slots = []
        for i in range(n_dma_slots):
            s = nc.alloc_semaphore("d_%d" % i)
            self.slots.append([s, 0])
            self.semobj["d_%d" % i] = s
        self.slot_rr = 0
        self.known = {n: {} for n in self.eng}
        self.lastw, self.reads = {}, {}
        self.ninstr = 0

    def _need(self, e, tick, pend=None):
        if tick is None:
            return
        sn, val = tick
        if self.known[e].get(sn, 0) >= val:
            return
        self.known[e][sn] = val
        if pend is not None:
            pend[sn] = max(pend.get(sn, 0), val)
            return
        self.eng[e].wait_ge(self.semobj[sn], val)
        self.ninstr += 1

    def _pre(self, e, reads, writes, pend=None):
        for k in reads:
            self._need(e, self.lastw.get(k), pend)
        for k in writes:
            self._need(e, self.lastw.get(k), pend)
            for t in self.reads.get(k, ()):
                self._need(e, t, pend)

    def _flush(self, e, pend, keep_last):
        items = list(pend.items())
        last = None
        if keep_last and items:
            last = items.pop()
        for sn, val in items:
            self.eng[e].wait_ge(self.semobj[sn], val)
            self.ninstr += 1
        return last

    def _post(self, tick, reads, writes):
        for k in reads:
            self.reads.setdefault(k, []).append(tick)
        for k in writes:
            self.lastw[k] = tick
            self.reads[k] = []

    def op(self, e, fn, reads=(), writes=()):
        psr = [r for r in reads if isinstance(r, str) and r.startswith("ps")]
        if psr:
            reads = [r for r in reads if r not in psr]
            writes = list(writes) + psr
        pend = {}
        self._pre(e, reads, writes, pend)
        single = ATTACH_WAITS and getattr(fn, "__name__", "") == "<lambda>"
        last = self._flush(e, pend, single)
        ins = fn(self.eng[e])
        if last is not None:
            ins._wait_ge(self.semobj[last[0]], last[1])
        self.cnt[e] += 1
        ins.then_inc(self.sem[e], 1)
        self.ninstr += 1
        tick = ("s_" + e, self.cnt[e])
        self._post(tick, reads, writes)
        return tick

    def dma(self, e, out, in_, reads=(), writes=(), fn=None, **kw):
        pend = {}
        self._pre(e, reads, writes, pend)
        si = self.slot_rr
        self.slot_rr = (self.slot_rr + 1) % len(self.slots)
        slot = self.slots[si]
        sn = "d_%d" % si
        self._need(e, (sn, slot[1]) if slot[1] else None, pend)
        last = self._flush(e, pend, ATTACH_WAITS)
        if fn is None:
            ins = self.eng[e].dma_start(out=out, in_=in_, **kw)
        else:
            ins = fn(self.eng[e])
        if last is not None:
            ins._wait_ge(self.semobj[last[0]], last[1])
        slot[1] += 16
        ins.then_inc(slot[0], 16)
        self.ninstr += 1
        tick = (sn, slot[1])
        self._post(tick, reads, writes)
        return tick

    def barrier(self):
        for e in self.eng:
            for i, s in enumerate(self.slots):
                if s[1]:
                    self._need(e, ("d_%d" % i, s[1]))
            for n in self.eng:
                if n != e and self.cnt[n]:
                    self._need(e, ("s_" + n, self.cnt[n]))
        self.lastw, self.reads = {}, {}


class Arena:
    def __init__(self, nc, nbytes):
        self.big = nc.alloc_sbuf_tensor("arena", [128, nbytes], U8)
        self.nbytes = nbytes
        self.off = 0

    def reset(self, off=0):
        self.off = off

    def alloc(self, shape, dtype, parts=128):
        isz = 4 if dtype in (F32, I32, U32) else 2
        n = int(np.prod(shape[1:]))
        size = (n * isz + 63) // 64 * 64
        assert self.off + size <= self.nbytes, ("arena overflow", self.off, size, self.nbytes)
        ap = self.big[0:shape[0], self.off:self.off + n * isz].bitcast(dtype)
        self.off += size
        if len(shape) > 2:
            names = " ".join("d%d" % i for i in range(1, len(shape)))
            kw = {"d%d" % i: shape[i] for i in range(1, len(shape))}
            ap = ap.rearrange("p (%s) -> p %s" % (names, names), **kw)
        return ap


def bcast(ap, shape):
    return ap.to_broadcast(list(shape))


def build_program(debug=(), stop=None, skip=()):
    nc = bass.Bass("TRN2", target_bir_lowering=False)
    IN = {}

    def inp(name, shape, dt=F32):
        IN[name] = nc.dram_tensor(name, list(shape), dt, kind="ExternalInput").ap()
        return IN[name]

    x_d = inp("x", [L, D])
    ctx_d = inp("ctx", [CTX, D])
    cT_d = inp("cT", [128, 8])
    cctxT_d = inp("cctxT", [128, 8])
    adaw_d = inp("ada_w", [D, 6 * D])
    adabT_d = inp("ada_bT", [128, 48])
    gmixT_d = inp("gmixT", [128, 8])
    gffnT_d = inp("gffnT", [128, 8])
    win_d = inp("w_in", [D, IN_COLS])
    cos_d = inp("rope_cos", [128, NT * 64])
    sin_d = inp("rope_sin", [128, NT * 64])
    gq_d = inp("gq", [1, 64])
    gk_d = inp("gk", [1, 64])
    gqsw_d = inp("gqsw", [1, 64])
    gksw_d = inp("gksw", [1, 64])
    wao_d = inp("w_attn_out", [512, D])
    wout_d = inp("w_out", [D, D])
    gfin_d = inp("final_norm_g", [1, D])
    why_d = inp("w_hy_out", [HYC, D]); wq_d = inp("peer_wq", [D, 2048]); keysT_d = inp("keysT", [128, 2048])
    pu_d = inp("peer_u", [16384, D]); pv_d = inp("peer_v", [16384, D])
    W1_d = inp("W1", [128, 512]); KC_d = inp("KC", [128, 128]); KS_d = inp("KS", [128, 128]); NKS_d = inp("NKS", [128, 128])
    R1_d = inp("R1", [128, 256]); R2_d = inp("R2", [128, 256]); C2_d = inp("C2", [128, 256]); NS2_d = inp("NS2", [128, 256])
    TW1_d = inp("TW1", [128, 768]); TW2_d = inp("TW2", [128, 768]); skipT_d = inp("skipT", [128, 256])
    zT_d = inp("zT", [33, L]); t01_d = inp("t01", [1, L]); ndelT_d = inp("ndelT", [128, 4])
    hfw1_d = inp("hf_w1", [33, 64]); hfw2_d = inp("hf_w2", [64, 64]); hfw3_d = inp("hf_w3", [64, 64]); hfw4_d = inp("hf_w4", [64, 2048])
    hfb_d = inp("hf_b", [64, 4]); hcw_d = inp("hy_conv_w", [3, 3 * HYC]); hcb_d = inp("hy_conv_b", [1, 3 * HYC])

    out_d = nc.dram_tensor("out", [L, D], F32, kind="ExternalOutput").ap()
    DBG = {}

    def dbg_out(name, shape, dt=F32):
        DBG[name] = nc.dram_tensor("dbg_" + name, list(shape), dt, kind="ExternalOutput").ap()
        return DBG[name]

    zhy_d = nc.dram_tensor("zhy_s", [L + 2, 3 * HYC], F32).ap()
    gates_d = nc.dram_tensor("gates_s", [L, 2 * D], BF16).ap()
    attnT_d = nc.dram_tensor("attnT_s", [8, 64, L], BF16).ap()
    kf_d = nc.dram_tensor("kf_s", [2, 128, 128, 768], F32).ap()
    uvb_d = nc.dram_tensor("uvb_s", [16384, 2 * D], BF16).ap()

    k = KB(nc)
    A = Arena(nc, 206 * 1024)
    PS = [nc.alloc_psum_tensor("ps%d" % i, [128, 512], F32) for i in range(8)]

    ident = A.alloc([128, 128], F32)
    ti = A.alloc([128, 128], I32)
    k.op("pool", lambda e: e.iota(ti, pattern=[[1, 128]], base=0, channel_multiplier=-1), writes=["ti"])
    k.op("dve", lambda e: e.tensor_scalar(ident, ti, 0, None, ALU.is_equal), reads=["ti"], writes=["ident"])
    modT = A.alloc([128, 48, 2], F32)
    A1 = A.alloc([128, 8], F32)
    Ac1 = A.alloc([128, 8], F32)
    A2 = A.alloc([128, 8], F32)
    gmixT = A.alloc([128, 8], F32)
    gffnT = A.alloc([128, 8], F32)
    negmb = A.alloc([128, 1], F32)
    epsc = A.alloc([128, 1], F32)
    k.op("dve", lambda e: e.memset(epsc, EPS), writes=["epsc"])
    PERSIST = A.off

    craw = A.alloc([128, 2, 8], F32)
    sc2 = A.alloc([128, 8, 2], F32)
    adabT = A.alloc([128, 48], F32)
    k.dma("sp", craw[:, 0, :], cT_d, writes=["craw0"])
    k.dma("sp", craw[:, 1, :], cctxT_d, writes=["craw1"])
    k.dma("sp", adabT, adabT_d, writes=["adabT"])
    k.dma("sp", gmixT, gmixT_d, writes=["gmixT"])
    k.dma("sp", gffnT, gffnT_d, writes=["gffnT"])
    k.op("act", lambda e: e.activation(sc2[:, :, 0], craw[:, 0, :], AF.Silu), reads=["craw0"], writes=["sc2a"])
    k.op("act", lambda e: e.activation(sc2[:, :, 1], craw[:, 1, :], AF.Silu), reads=["craw1"], writes=["sc2b"])
    awt = [A.alloc([128, 8, 1024], F32) for _ in range(2)]
    adaw_v = adaw_d.rearrange("(k p) c -> p k c", p=128)
    for blk in range(6):
        b = blk % 2
        for kk in range(8):
            k.dma("sp" if kk % 2 == 0 else "pool", awt[b][:, kk, :], adaw_d[kk * 128:(kk + 1) * 128, blk * 1024:(blk + 1) * 1024],
                  writes=[("awt", b, kk)])

        def mm(e, blk=blk, b=b):
            ins = None
            for n in range(8):
                cn = blk * 8 + n
                for kk in range(8):
                    ins = e.matmul(PS[0][:, 2 * cn:2 * cn + 2], awt[b][:, kk, n * 128:(n + 1) * 128], sc2[:, kk, :],
                                   start=(kk == 0), stop=(kk == 7))
            return ins
        k.op("pe", mm, reads=[("awt", b, kk) for kk in range(8)] + ["sc2a", "sc2b"], writes=["ps0"])
    k.op("dve", lambda e: e.tensor_tensor(modT, PS[0][:, 0:96].rearrange("p (c t) -> p c t", t=2),
                                          bcast(adabT.unsqueeze(2), [128, 48, 2]), ALU.add),
         reads=["ps0", "adabT"], writes=["modT"])
    k.op("dve", lambda e: e.scalar_tensor_tensor(A1, modT[:, 8:16, 0], 1.0, gmixT, ALU.add, ALU.mult),
         reads=["modT", "gmixT"], writes=["A1"])
    k.op("dve", lambda e: e.scalar_tensor_tensor(Ac1, modT[:, 8:16, 1], 1.0, gmixT, ALU.add, ALU.mult),
         reads=["modT", "gmixT"], writes=["Ac1"])
    k.op("dve", lambda e: e.scalar_tensor_tensor(A2, modT[:, 32:40, 0], 1.0, gffnT, ALU.add, ALU.mult),
         reads=["modT", "gffnT"], writes=["A2"])
    if "modT" in debug:
        k.dma("sp", dbg_out("modT", [128, 96]), modT.rearrange("p c t -> p (c t)"), reads=["modT"])
    k.barrier()
    A.reset(PERSIST)

    if stop == 'p0':
        return nc, IN, DBG, k
    winb = A.alloc([128, 8, IN_COLS], BF16)
    QT = A.alloc([128, 4, L], BF16)
    KT = A.alloc([128, NKEY], BF16)
    VX = A.alloc([128, NKC, 2, 65], BF16)
    cosq = A.alloc([128, NT, 64], F32)
    sinq = A.alloc([128, NT, 64], F32)
    cosk = A.alloc([128, NT, 64], F32)
    sink = A.alloc([128, NT, 64], F32)
    gtab = A.alloc([128, 4, 64], F32)
    P3 = A.off
    for j, gd in enumerate((gq_d, gk_d, gqsw_d, gksw_d)):
        k.dma("sp", gtab[:, j, :], bcast(gd, [128, 64]), writes=[("gtab", j)])
    HW = IN_COLS // 4
    wst = [A.alloc([128, HW], F32) for _ in range(4)]
    for kk in range(8):
        for hh in range(4):
            k.dma("sp" if hh % 2 == 0 else "pool", wst[hh], win_d[kk * 128:(kk + 1) * 128, hh * HW:(hh + 1) * HW], writes=[("wst", hh)])
            if hh % 2 == 0:
                k.op("act", lambda e, kk=kk, hh=hh: e.copy(winb[:, kk, hh * HW:(hh + 1) * HW], wst[hh]), reads=[("wst", hh)], writes=[("winb", kk, hh)])
            else:
                k.op("dve", lambda e, kk=kk, hh=hh: e.tensor_copy(winb[:, kk, hh * HW:(hh + 1) * HW], wst[hh]), reads=[("wst", hh)], writes=[("winb", kk, hh)])
    for q4 in range(4):
        k.dma("sp", cosk[:, q4 * 8:(q4 + 1) * 8, :].rearrange("p a b -> p (a b)"), cos_d[:, q4 * 512:(q4 + 1) * 512], writes=[("cosk", q4)])
        k.dma("pool", sink[:, q4 * 8:(q4 + 1) * 8, :].rearrange("p a b -> p (a b)"), sin_d[:, q4 * 512:(q4 + 1) * 512], writes=[("sink", q4)])
    k.op("dve", lambda e: e.tensor_tensor(cosq, cosk, bcast(gtab[:, 0:1, :], [128, NT, 64]), ALU.mult),
         reads=[("cosk", q) for q in range(4)] + [("gtab", 0)], writes=["cosq"])
    k.op("dve", lambda e: e.tensor_tensor(sinq, sink, bcast(gtab[:, 2:3, :], [128, NT, 64]), ALU.mult),
         reads=[("sink", q) for q in range(4)] + [("gtab", 2)], writes=["sinq"])
    k.op("dve", lambda e: e.tensor_tensor(cosk, cosk, bcast(gtab[:, 1:2, :], [128, NT, 64]), ALU.mult),
         reads=["cosq", ("gtab", 1)], writes=["cosk"])
    k.op("dve", lambda e: e.tensor_tensor(sink, sink, bcast(gtab[:, 3:4, :], [128, NT, 64]), ALU.mult),
         reads=["sinq", ("gtab", 3)], writes=["sink"])
    mqk = A.alloc([128, 2], F32)
    k.op("dve", lambda e: e.tensor_reduce(mqk[:, 0:1], gtab[:, 0, :], AX.X, ALU.max, apply_absolute_value=True),
         reads=[("gtab", 0)], writes=["mqk0"])
    k.op("dve", lambda e: e.tensor_reduce(mqk[:, 1:2], gtab[:, 1, :], AX.X, ALU.max, apply_absolute_value=True),
         reads=[("gtab", 1)], writes=["mqk1"])
    k.op("dve", lambda e: e.scalar_tensor_tensor(negmb, mqk[:, 0:1], -8.0, mqk[:, 1:2], ALU.mult, ALU.mult),
         reads=["mqk0", "mqk1"], writes=["negmb"])
    k.op("pool", lambda e: e.memset(VX[:, :, :, 64:65], 1.0), writes=["vx1"])
    zrow = A.alloc([128, 3 * HYC], F32)
    k.op("pool", lambda e: e.memset(zrow, 0.0), writes=["zrow"])
    k.dma("pool", zhy_d[0:1, :], zrow[0:1, :], reads=["zrow"])
    k.dma("pool", zhy_d[L + 1:L + 2, :], zrow[0:1, :], reads=["zrow"])

    k.barrier()
    if stop == 'p3a':
        return nc, IN, DBG, k
    A.reset(P3)
    xb = [A.alloc([128, D], F32) for _ in range(2)]
    xn = [A.alloc([128, D], F32) for _ in range(2)]
    hT = [A.alloc([128, 8, 128], BF16) for _ in range(2)]
    st = [A.alloc([128, 24], F32) for _ in range(2)]
    sq = A.alloc([128, 640], F32)
    t1 = A.alloc([128, 640], F32)
    t2 = A.alloc([128, 640], F32)
    qrp = A.alloc([128, 4, 2, 64], F32)
    kr = A.alloc([128, 128], F32)
    zh = [A.alloc([128, 3 * HYC], F32)] * 2
    gs = [A.alloc([128, 2 * D], BF16)] * 2
    x_v = x_d.rearrange("(p i) d -> i p d", i=NT)
    zhy_v = zhy_d[1:L + 1, :].rearrange("(p i) c -> i p c", i=NT)
    gates_v = gates_d.rearrange("(p i) c -> i p c", i=NT)
    zb = 0

    for it in range(NT + 2):
        if stop == 'p3b' and it == 1:
            k.barrier()
            return nc, IN, DBG, k
        b = it % 2
        isx = it < NT
        src = x_v[it] if isx else ctx_d[(it - NT) * 128:(it - NT + 1) * 128, :]
        k.dma("sp", xb[b], src, writes=[("xb", b)])
        s = st[b]
        k.op("act", lambda e, b=b, s=s: e.activation(xn[b], xb[b], AF.Square, accum_out=s[:, 0:1]),
             reads=[("xb", b)], writes=[("xn", b), ("st", b)])
        k.op("dve", lambda e, s=s: e.tensor_scalar(s[:, 1:2], s[:, 0:1], 1.0 / D, EPS, ALU.mult, ALU.add),
             reads=[("st", b)], writes=[("st", b)])
        k.op("act", lambda e, s=s: e.activation(s[:, 2:3], s[:, 1:2], AF.Sqrt), reads=[("st", b)], writes=[("st", b)])
        k.op("dve", lambda e, s=s: e.reciprocal(s[:, 3:4], s[:, 2:3]), reads=[("st", b)], writes=[("st", b)])
        k.op("act", lambda e, b=b, s=s: e.activation(xn[b], xb[b], AF.Identity, scale=s[:, 3:4]),
             reads=[("xb", b), ("st", b)], writes=[("xn", b)])
        for half in range(2):
            def tr(e, b=b, half=half):
                ins = None
                for j in range(4):
                    jj = half * 4 + j
                    ins = e.transpose(PS[half][:, j * 128:(j + 1) * 128], xn[b][:, jj * 128:(jj + 1) * 128], ident)
                return ins
            k.op("pe", tr, reads=[("xn", b), "ident"], writes=["ps%d" % half])
        Asc = A1 if isx else Ac1
        bcol = 0 if isx else 1
        for jj in range(8):
            half, j = jj // 4, jj % 4
            if jj % 2 == 0:
                k.op("act", lambda e, b=b, jj=jj, half=half, j=j, Asc=Asc, bcol=bcol: e.activation(
                    hT[b][:, jj, :], PS[half][:, j * 128:(j + 1) * 128], AF.Identity,
                    scale=Asc[:, jj:jj + 1], bias=modT[:, jj, bcol:bcol + 1]),
                    reads=["ps%d" % half, "A1", "Ac1", "modT"], writes=[("hT", b, jj)])
            else:
                k.op("dve", lambda e, b=b, jj=jj, half=half, j=j, Asc=Asc, bcol=bcol: e.tensor_scalar(
                    hT[b][:, jj, :], PS[half][:, j * 128:(j + 1) * 128],
                    Asc[:, jj:jj + 1], modT[:, jj, bcol:bcol + 1], ALU.mult, ALU.add),
                    reads=["ps%d" % half, "A1", "Ac1", "modT"], writes=[("hT", b, jj)])
        hkeys = [("hT", b, jj) for jj in range(8)]
        wkeys = []

        def zmm(c0, n, bank):
            def f(e):
                ins = None
                for kk in range(8):
                    ins = e.matmul(PS[bank][:, 0:n], hT[b][:, kk, :], winb[:, kk, c0:c0 + n], start=(kk == 0), stop=(kk == 7))
                return ins
            k.op("pe", f, reads=hkeys + wkeys, writes=["ps%d" % bank])

        bank = 2 + zb % 4; zb += 1
        zmm(512, 256, bank)
        kp = PS[bank][:, 0:128]
        k.op("act", lambda e, kp=kp: e.activation(sq[:, 512:640], kp, AF.Square), reads=["ps%d" % bank], writes=["sqk"])
        k.op("dve", lambda e, s=s: e.tensor_reduce(s[:, 4:6], sq[:, 512:640].rearrange("p (h d) -> p h d", d=64), AX.X, ALU.add),
             reads=["sqk"], writes=[("st", b)])
        k.op("dve", lambda e, s=s: e.tensor_scalar(s[:, 4:6], s[:, 4:6], 1.0 / 64, EPS, ALU.mult, ALU.add),
             reads=[("st", b)], writes=[("st", b)])
        k.op("act", lambda e, s=s: e.activation(s[:, 4:6], s[:, 4:6], AF.Sqrt), reads=[("st", b)], writes=[("st", b)])
        k.op("dve", lambda e, s=s: e.reciprocal(s[:, 6:8], s[:, 4:6]), reads=[("st", b)], writes=[("st", b)])
        kp3 = kp.rearrange("p (h d) -> p h d", d=64)
        t1k = t1[:, 512:640].rearrange("p (h d) -> p h d", d=64)
        t2k = t2[:, 512:640].rearrange("p (h d) -> p h d", d=64)
        if isx:
            k.op("dve", lambda e: e.tensor_tensor(t1k, kp3, bcast(cosk[:, it:it + 1, :], [128, 2, 64]), ALU.mult),
                 reads=["ps%d" % bank, "cosk"], writes=["t1k"])
            for a in range(2):
                for f in range(2):
                    o0 = a * 32 + f * 16
                    i0 = a * 32 + (1 - f) * 16
                    k.op("dve", lambda e, o0=o0, i0=i0: e.tensor_tensor(
                        t2k[:, :, o0:o0 + 16], kp3[:, :, i0:i0 + 16],
                        bcast(sink[:, it:it + 1, o0:o0 + 16], [128, 2, 16]), ALU.mult),
                        reads=["ps%d" % bank, "sink"], writes=[("t2k", a, f)])
            k.op("dve", lambda e: e.tensor_tensor(t1k, t1k, t2k, ALU.add),
                 reads=["t1k"] + [("t2k", a, f) for a in range(2) for f in range(2)], writes=["t1k"])
        else:
            k.op("dve", lambda e: e.tensor_tensor(t1k, kp3, bcast(gtab[:, 1:2, :], [128, 2, 64]), ALU.mult),
                 reads=["ps%d" % bank, ("gtab", 1)], writes=["t1k"])
        k.op("dve", lambda e, s=s: e.tensor_tensor(kr.rearrange("p (h d) -> p h d", d=64), t1k,
                                                   bcast(s[:, 6:8].unsqueeze(2), [128, 2, 64]), ALU.mult),
             reads=["t1k", ("st", b)], writes=["kr"])
        k.op("act", lambda e, bank=bank: e.copy(VX[:, it, :, 0:64], PS[bank][:, 128:256].rearrange("p (g d) -> p g d", d=64)),
             reads=["ps%d" % bank], writes=[("vx", it)])
        k.op("pe", lambda e: e.transpose(PS[7][:, 0:128], kr, ident), reads=["kr", "ident"], writes=["ps7"])
        k.op("act", lambda e: e.copy(KT[:, it * 128:(it + 1) * 128], PS[7][:, 0:128]), reads=["ps7"], writes=[("kt", it)])
        if not isx:
            continue
        bank = 2 + zb % 4; zb += 1
        zmm(0, 512, bank)
        qp = PS[bank][:, 0:512]
        k.op("act", lambda e, qp=qp: e.activation(sq[:, 0:512], qp, AF.Square), reads=["ps%d" % bank], writes=["sqq"])
        k.op("dve", lambda e, s=s: e.tensor_reduce(s[:, 8:16], sq[:, 0:512].rearrange("p (h d) -> p h d", d=64), AX.X, ALU.add),
             reads=["sqq"], writes=[("st", b)])
        k.op("dve", lambda e, s=s: e.tensor_scalar(s[:, 8:16], s[:, 8:16], 1.0 / 64, EPS, ALU.mult, ALU.add),
             reads=[("st", b)], writes=[("st", b)])
        k.op("act", lambda e, s=s: e.activation(s[:, 8:16], s[:, 8:16], AF.Sqrt), reads=[("st", b)], writes=[("st", b)])
        k.op("dve", lambda e, s=s: e.reciprocal(s[:, 16:24], s[:, 8:16]), reads=[("st", b)], writes=[("st", b)])
        qp3 = qp.rearrange("p (h d) -> p h d", d=64)
        t1q = t1[:, 0:512].rearrange("p (h d) -> p h d", d=64)
        t2q = t2[:, 0:512].rearrange("p (h d) -> p h d", d=64)
        k.op("dve", lambda e: e.tensor_tensor(t1q, qp3, bcast(cosq[:, it:it + 1, :], [128, 8, 64]), ALU.mult),
             reads=["ps%d" % bank, "cosq"], writes=["t1q"])
        for a in range(2):
            for f in range(2):
                o0 = a * 32 + f * 16
                i0 = a * 32 + (1 - f) * 16
                k.op("dve", lambda e, o0=o0, i0=i0: e.tensor_tensor(
                    t2q[:, :, o0:o0 + 16], qp3[:, :, i0:i0 + 16],
                    bcast(sinq[:, it:it + 1, o0:o0 + 16], [128, 8, 16]), ALU.mult),
                    reads=["ps%d" % bank, "sinq"], writes=[("t2q", a, f)])
        k.op("dve", lambda e: e.tensor_tensor(t1q, t1q, t2q, ALU.add),
             reads=["t1q"] + [("t2q", a, f) for a in range(2) for f in range(2)], writes=["t1q"])
        k.op("dve", lambda e, s=s: e.tensor_tensor(
            qrp.rearrange("p a s d -> p s a d"), t1[:, 0:512].rearrange("p (s a d) -> p s a d", s=2, a=4),
            bcast(s[:, 16:24].rearrange("p (s a) -> p s a", s=2).unsqueeze(3), [128, 2, 4, 64]), ALU.mult),
            reads=["t1q", ("st", b)], writes=["qrp"])

        def trq(e):
            ins = None
            for a in range(4):
                ins = e.transpose(PS[6][:, a * 128:(a + 1) * 128], qrp[:, a, :, :].rearrange("p s d -> p (s d)"), ident)
            return ins
        k.op("pe", trq, reads=["qrp", "ident"], writes=["ps6"])
        k.op("act", lambda e: e.copy(QT[:, :, it * 128:(it + 1) * 128], PS[6][:, :].rearrange("p (a t) -> p a t", a=4)),
             reads=["ps6"], writes=[("qt", it)])
        for c in range(3):
            bank = 2 + zb % 4; zb += 1
            zmm(768 + c * 512, 512, bank)
            k.op("act", lambda e, c=c, bank=bank: e.copy(zh[b][:, c * 512:(c + 1) * 512], PS[bank][:, :]),
                 reads=["ps%d" % bank], writes=[("zh", c)])
        for c in range(3):
            if "scr" not in skip:
                k.dma("pool", zhy_v[it][:, c * 512:(c + 1) * 512], zh[b][:, c * 512:(c + 1) * 512], reads=[("zh", c)])
        for c in range(4):
            bank = 2 + zb % 4; zb += 1
            zmm(2304 + c * 512, 512, bank)
            k.op("act", lambda e, c=c, bank=bank: e.activation(gs[b][:, c * 512:(c + 1) * 512], PS[bank][:, :], AF.Sigmoid),
                 reads=["ps%d" % bank], writes=[("gs", c)])
        if "scr" not in skip:
            k.dma("pool", gates_v[it], gs[b], reads=[("gs", c) for c in range(4)])

    if "QT" in debug:
        k.barrier()
        A.reset(P3)
        qf = A.alloc([128, 4, 512], F32)
        k.op("dve", lambda e: e.tensor_copy(qf, QT[:, :, 0:512]), reads=[("qt", i) for i in range(4)], writes=["qf"])
        dq = dbg_out("QT", [128, 2048])
        for a in range(4):
            k.dma("sp", dq[:, a * 512:(a + 1) * 512], qf[:, a, :], reads=["qf"])
        kf_ = A.alloc([128, NKEY], F32)
        k.op("dve", lambda e: e.tensor_copy(kf_, KT), reads=[("kt", i) for i in range(NKC)], writes=["kf_"])
        dk = dbg_out("KT", [128, NKEY])
        for a in range(NKC // 2):
            k.dma("sp", dk[:, a * 256:(a + 1) * 256], kf_[:, a * 256:(a + 1) * 256], reads=["kf_"])
    k.barrier()
    A.reset(P3)

    if stop == 'p3':
        return nc, IN, DBG, k
    pT = [A.alloc([128, 512], BF16) for _ in range(3)]
    osb = [A.alloc([128, 512], F32) for _ in range(2)]
    rec = A.alloc([128, 512], F32)
    aT = [A.alloc([128, 512], BF16) for _ in range(2)]
    ones = A.alloc([128, 64], BF16)
    onesf = A.alloc([128, 64], F32)
    k.op("dve", lambda e: e.memset(onesf, 1.0), writes=["onesf"])
    cst = [A.alloc([128, D], F32) for _ in range(4)]
    cbf = [A.alloc([128, D], BF16) for _ in range(4)]
    cvn = [0]

    def conv_chunk():
        cn = cvn[0]; cvn[0] += 1
        if cn >= 256:
            return
        tb, rch, b4 = cn // 128, cn % 128, cn % 4
        src_d = pu_d if tb == 0 else pv_d
        k.dma("sp", cst[b4], src_d[rch * 128:(rch + 1) * 128, :], writes=[("cst", b4)])
        k.op("dve", lambda e: e.tensor_copy(cbf[b4], cst[b4]), reads=[("cst", b4)], writes=[("cbf", b4)])
        k.dma("sp", uvb_d[rch * 128:(rch + 1) * 128, tb * D:(tb + 1) * D], cbf[b4], reads=[("cbf", b4)], writes=["uvb"])
    step = 0
    for h in range(8):
        g, a = h // 4, h % 4
        pr = slice(64 * g, 64 * g + 64)
        for qc in range(8):
            ob = (h * 8 + qc) % 2
            obank = 4 + ob
            sbs = {}

            def st_s(kc):
                nonlocal step
                if kc % 8 == 0:
                    conv_chunk()
                sb = step % 3
                step += 1
                sbs[kc] = sb
                k.op("pe", lambda e: e.matmul(
                    PS[sb][:, :], KT[pr, kc * 128:(kc + 1) * 128], QT[pr, a, qc * 512:(qc + 1) * 512],
                    start=True, stop=True), writes=["ps%d" % sb])
                k.op("act", lambda e: e.activation(
                    pT[sb], PS[sb][:, :], AF.Exp, scale=0.125, bias=negmb),
                    reads=["ps%d" % sb], writes=[("pT", sb)])

            def st_pv(kc):
                sb = sbs[kc]
                wr = ["ps%d" % obank] if kc in (0, NKC - 1) else []
                k.op("pe", lambda e: e.matmul(
                    PS[obank][0:65, :], VX[:, kc, g, :], pT[sb], start=(kc == 0), stop=(kc == NKC - 1)),
                    reads=[("pT", sb)], writes=wr)
            st_s(0)
            for kc in range(1, NKC):
                st_s(kc)
                st_pv(kc - 1)
            st_pv(NKC - 1)
            k.op("act", lambda e, ob=ob, obank=obank: e.copy(osb[ob][0:65, :], PS[obank][0:65, :]),
                 reads=["ps%d" % obank], writes=[("osb", ob)])
            k.op("pe", lambda e, ob=ob: e.matmul(PS[6][0:64, :], onesf[64:65, 0:64], osb[ob][64:65, :], start=True, stop=True),
                 reads=[("osb", ob), "onesf"], writes=["ps6"])
            k.op("dve", lambda e: e.reciprocal(rec[0:64, :], PS[6][0:64, :]), reads=["ps6"], writes=["rec"])
            k.op("dve", lambda e, ob=ob: e.tensor_tensor(aT[ob][0:64, :], osb[ob][0:64, :], rec[0:64, :], ALU.mult),
                 reads=["rec", ("osb", ob)], writes=[("aT", ob)])
            k.dma("pool", attnT_d[h, :, qc * 512:(qc + 1) * 512], aT[ob][0:64, :], reads=[("aT", ob)], writes=["attnT_d"])
    if "attn" in debug:
        af = A.alloc([128, 512], BF16)
        for h in range(8):
            k.dma("sp", af[0:64, :], attnT_d[h, :, 0:512], reads=["attnT_d"], writes=["af"])
            k.dma("sp", dbg_out("attnT%d" % h, [64, 512], BF16), af[0:64, :], reads=["af"])
    k.barrier()
    A.reset(P3)

    if stop == 'attn':
        return nc, IN, DBG, k
    A.reset(PERSIST)
    hy_sb = A.alloc([128, NT, HYC], BF16)
    PERSIST2 = A.off
    NF = 8192.0
    TWO_PI = 6.283185307179586

    def load_tab(dram, shape, dt, pieces=1):
        stg_ = A.alloc(shape, F32)
        tab = A.alloc(shape, dt) if dt != F32 else stg_
        flat = (lambda ap: ap if len(shape) == 2 else ap.rearrange("p a b -> p (a b)") if len(shape) == 3 else ap.rearrange("p a b c -> p (a b c)"))
        n = int(np.prod(shape[1:]))
        step = n // pieces
        for q in range(pieces):
            k.dma("sp", flat(stg_)[0:shape[0], q * step:(q + 1) * step], dram[:, q * step:(q + 1) * step], writes=[("tabstg", id(stg_), q)])
        if dt != F32:
            k.op("dve", lambda e: e.tensor_copy(flat(tab), flat(stg_)), reads=[("tabstg", id(stg_), q) for q in range(pieces)], writes=[("tab", id(tab))])
        return tab
    W1 = load_tab(W1_d, [128, 512], BF16)
    KC = load_tab(KC_d, [128, 128], BF16)
    KS = load_tab(KS_d, [128, 128], BF16)
    NKS = load_tab(NKS_d, [128, 128], BF16)
    R1 = load_tab(R1_d, [128, 256], BF16)
    R2 = load_tab(R2_d, [128, 256], BF16)
    C2 = load_tab(C2_d, [128, 2, 128], BF16)
    NS2 = load_tab(NS2_d, [128, 2, 128], BF16)
    TW1 = load_tab(TW1_d, [128, 768], F32)
    TW2 = load_tab(TW2_d, [128, 2, 3, 128], F32)
    skipN = load_tab(skipT_d, [128, 2, 128], F32)
    k.op("dve", lambda e: e.tensor_scalar(skipN, skipN, 1.0 / NF, None, ALU.mult),
         reads=[("tabstg", id(skipN), 0)], writes=["skipN"])
    k.barrier()
    PERSIST_T = A.off
    def fft_A(u_flat, rkeys, sl):
        k.op("pe", lambda e: e.matmul(PS[sl][:, :], u_flat, W1, start=True, stop=True), reads=rkeys, writes=["ps%d" % sl])
        t1 = ft1[sl]; t2 = ft2[sl]; ap_ = fap[sl]
        k.op("dve", lambda e: e.tensor_tensor(t1.rearrange("p (r f) -> p r f", r=2), PS[sl][:, :].rearrange("p (r f) -> p r f", r=2),
                                              bcast(TW1[:, 0:256].unsqueeze(1), [128, 2, 256]), ALU.mult),
             reads=["ps%d" % sl], writes=[("ft1", sl)])
        k.op("dve", lambda e: e.tensor_tensor(t2[:, 0:256], PS[sl][:, 256:512], TW1[:, 256:512], ALU.mult),
             reads=["ps%d" % sl], writes=[("ft2a", sl)])
        k.op("dve", lambda e: e.tensor_tensor(t2[:, 256:512], PS[sl][:, 0:256], TW1[:, 512:768], ALU.mult),
             reads=["ps%d" % sl], writes=[("ft2b", sl)])
        k.op("pool", lambda e: e.tensor_tensor(ap_, t1, t2, ALU.add),
             reads=[("ft1", sl), ("ft2a", sl), ("ft2b", sl)], writes=[("fap", sl)])

    def fft_B(sl, bb):
        ap_ = fap[sl]

        def f2(e):
            e.matmul(PS[bb][:, 0:256], KC, ap_[:, 0:256], start=True, stop=False)
            e.matmul(PS[bb][:, 0:256], KS, ap_[:, 256:512], start=False, stop=True)
            e.matmul(PS[bb][:, 256:512], KC, ap_[:, 256:512], start=True, stop=False)
            return e.matmul(PS[bb][:, 256:512], NKS, ap_[:, 0:256], start=False, stop=True)
        k.op("pe", f2, reads=[("fap", sl)], writes=["ps%d" % bb])
        return bb

    ft1 = [A.alloc([128, 512], F32) for _ in range(4)]
    ft2 = [A.alloc([128, 512], F32) for _ in range(4)]
    fap = [A.alloc([128, 512], BF16) for _ in range(4)]
    FWORK = A.off
    hA = A.alloc([128, L], F32)
    zT = A.alloc([128, L], F32)
    hB = A.alloc([128, L], F32)
    w123 = A.alloc([128, 3, 64], F32)
    bfr = A.alloc([128, 8], F32)
    for q in range(8):
        k.dma("sp", zT[0:33, q * 512:(q + 1) * 512], zT_d[:, q * 512:(q + 1) * 512], writes=[("zT", q)])
    k.dma("sp", w123[0:33, 0, :], hfw1_d, writes=["w1"])
    k.dma("sp", w123[0:64, 1, :], hfw2_d, writes=["w2"])
    k.dma("sp", w123[0:64, 2, :], hfw3_d, writes=["w3"])
    k.dma("sp", bfr[0:64, 0:4], hfb_d, writes=["bfr"])
    k.op("dve", lambda e: e.tensor_scalar(bfr[0:64, 4:5], bfr[0:64, 3:4], 1.0 / TWO_PI, None, ALU.mult), reads=["bfr"], writes=["bfr2"])
    ri_ = A.alloc([128, 512], I32)
    rr_ = A.alloc([128, 512], F32)
    srcs = [zT, hA, hB, hA]
    Ks_ = [33, 64, 64]
    for layer in range(3):
        src_, dst_ = srcs[layer], srcs[layer + 1]
        K_ = Ks_[layer]
        for q in range(8):
            bk = q % 2
            k.op("pe", lambda e: e.matmul(PS[bk][0:64, :], w123[0:K_, layer, :], src_[0:K_, q * 512:(q + 1) * 512], start=True, stop=True),
                 reads=[("zT", q), "w1", "w2", "w3", ("hm", layer, q)], writes=["ps%d" % bk])
            k.op("dve", lambda e: e.tensor_scalar(rr_[0:64, :], PS[bk][0:64, :], bfr[0:64, layer:layer + 1], bfr[0:64, 4:5], ALU.add, ALU.mult),
                 reads=["ps%d" % bk, "bfr", "bfr2"], writes=["rr"])
            k.op("dve", lambda e: e.tensor_copy(ri_[0:64, :], rr_[0:64, :]), reads=["rr"], writes=["ri"])
            k.op("dve", lambda e: e.tensor_tensor(rr_[0:64, :], rr_[0:64, :], ri_[0:64, :], ALU.subtract), reads=["rr", "ri"], writes=["rr"])
            k.op("act", lambda e: e.activation(dst_[0:64, q * 512:(q + 1) * 512], rr_[0:64, :], AF.Sin, scale=TWO_PI * 0.999999),
                 reads=["rr"], writes=[("hm", layer + 1, q)])
    k.barrier()
    A.reset(FWORK)
    hm3 = A.alloc([128, L], F32)
    w4s = A.alloc([128, 2048], F32)
    for q in range(4):
        k.dma("pool", w4s[0:64, q * 512:(q + 1) * 512], hfw4_d[:, q * 512:(q + 1) * 512], writes=[("w4", q)])
    t01bc = A.alloc([128, L], F32)
    for q in range(8):
        k.dma("sp", t01bc[:, q * 512:(q + 1) * 512], bcast(t01_d[:, q * 512:(q + 1) * 512], [128, 512]), writes=[("t01", q)])
    ndel = A.alloc([128, 4], F32)
    k.dma("sp", ndel, ndelT_d, writes=["ndel"])
    decay = A.alloc([128, L], F32)
    hraw = [A.alloc([128, L], F32) for _ in range(2)]
    junkb = A.alloc([128, L // 2], BF16)
    hf = [A.alloc([128, 32, 32, 4], BF16) for _ in range(2)]
    ssum = A.alloc([128, 8], F32)
    ffs = A.alloc([128, 512], F32)
    ktmp = A.alloc([128, 512], F32)
    kf3 = [A.alloc([128, 768], F32) for _ in range(2)]
    kfn = 0
    for cc in range(4):
        for q in range(8):
            k.op("act", lambda e, q=q: e.activation(decay[:, q * 512:(q + 1) * 512], t01bc[:, q * 512:(q + 1) * 512], AF.Exp, scale=ndel[:, cc:cc + 1]),
                 reads=[("t01", q), "ndel"], writes=[("decay", q)])
        for o in range(2):
            for dr in range(2):
                col0 = o * 1024 + dr * 512 + cc * 128
                for q in range(8):
                    bk = 4 + q % 2
                    k.op("pe", lambda e, q=q: e.matmul(PS[bk][:, :], w4s[0:64, col0:col0 + 128], hm3[0:64, q * 512:(q + 1) * 512], start=True, stop=True),
                         reads=[("w4", col0 // 512)], writes=["ps%d" % bk])
                    k.op("dve", lambda e, q=q: e.tensor_tensor(hraw[dr][:, q * 512:(q + 1) * 512], PS[bk][:, :], decay[:, q * 512:(q + 1) * 512], ALU.mult),
                         reads=["ps%d" % bk, ("decay", q)], writes=[("hraw", dr, q)])
                if dr == 1:
                    k.op("dve", lambda e: e.memset(hraw[1][:, 0:1], 0.0), reads=[("hraw", 1, 0)], writes=[("hraw", 1, 0)])
                for hh_ in range(2):
                    k.op("act", lambda e, dr=dr, hh_=hh_: e.activation(junkb, hraw[dr][:, hh_ * 2048:(hh_ + 1) * 2048], AF.Abs,
                                                                    accum_out=ssum[:, 4 + 2 * dr + hh_:5 + 2 * dr + hh_]),
                         reads=[("hraw", dr, q) for q in range(8)], writes=["junkb", ("ssum", dr, hh_)])
            k.op("dve", lambda e: e.tensor_reduce(ssum[:, 0:1], ssum[:, 4:8], AX.X, ALU.add),
                 reads=[("ssum", d_, h_) for d_ in range(2) for h_ in range(2)], writes=["ssum0"])
            k.op("dve", lambda e: e.tensor_scalar(ssum[:, 2:3], ssum[:, 0:1], EPS, None, ALU.add), reads=["ssum0"], writes=["ssum2"])
            k.op("dve", lambda e: e.reciprocal(ssum[:, 3:4], ssum[:, 2:3]), reads=["ssum2"], writes=["ssum3"])
            for dr in range(2):
                for q in range(8):
                    k.op("act", lambda e, dr=dr, q=q: e.activation(hraw[dr][:, q * 512:(q + 1) * 512], hraw[dr][:, q * 512:(q + 1) * 512], AF.Identity, scale=ssum[:, 3:4]),
                         reads=["ssum3", "junkb", ("hraw", dr, q)], writes=[("hraw", dr, q)])
                hv = hraw[dr].rearrange("p (a i) -> p a i", i=32)
                for i4 in range(8):
                    bk = 6 + i4 % 2

                    def trf(e, i4=i4, bk=bk, hv=hv):
                        ins = None
                        for j in range(4):
                            ins = e.transpose(PS[bk][:, j * 128:(j + 1) * 128], hv[:, :, i4 * 4 + j], ident)
                        return ins
                    k.op("pe", trf, reads=[("hraw", dr, q) for q in range(8)] + ["ident"], writes=["ps%d" % bk])
                    k.op("act", lambda e, i4=i4, bk=bk, dr=dr: e.copy(
                        hf[dr][:, :, i4 * 4:(i4 + 1) * 4, :].rearrange("p g i c -> p i g c"),
                        PS[bk][:, :].rearrange("p (i g c) -> p i g c", i=4, g=32)),
                        reads=["ps%d" % bk], writes=[("hf", dr, i4)])
            hkeys0 = [("hf", 0, i4) for i4 in range(8)]
            hkeys1 = [("hf", 1, i4) for i4 in range(8)]

            def stA(g):
                sl = 2 * (g % 2)
                fft_A(hf[0][:, g, :, :].rearrange("p i c -> p (i c)"), hkeys0, sl)
                fft_A(hf[1][:, g, :, :].rearrange("p i c -> p (i c)"), hkeys1, sl + 1)

            def stB(g):
                nonlocal kfn
                G = cc * 32 + g
                sl = 2 * (g % 2)
                bf_ = fft_B(sl, 4 + sl)
                k.op("act", lambda e: e.copy(ffs, PS[bf_][:, :]), reads=["ps%d" % bf_], writes=["ffs"])
                bb_ = fft_B(sl + 1, 5 + sl)
                kk_ = kf3[kfn % 2]; kfn += 1
                k.op("dve", lambda e: e.tensor_tensor(ktmp[:, 0:256], PS[bb_][:, 0:256], ffs[:, 0:256], ALU.add),
                     reads=["ps%d" % bb_, "ffs"], writes=["ktmpa"])
                k.op("dve", lambda e: e.tensor_tensor(ktmp[:, 256:512], ffs[:, 256:512], PS[bb_][:, 256:512], ALU.subtract),
                     reads=["ps%d" % bb_, "ffs"], writes=["ktmpb"])
                k.op("dve", lambda e: e.tensor_scalar(kk_[:, 0:256], ktmp[:, 0:256], 1.0 / NF, skipN[:, o, G:G + 1], ALU.mult, ALU.add),
                     reads=["ktmpa", "skipN"], writes=[("kf3a", kfn % 2)])
                k.op("act", lambda e: e.activation(kk_[:, 256:512], ktmp[:, 256:512], AF.Identity, scale=-1.0 / NF),
                     reads=["ktmpb"], writes=[("kf3b", kfn % 2)])
                k.op("act", lambda e: e.activation(kk_[:, 512:768], ktmp[:, 256:512], AF.Identity, scale=1.0 / NF),
                     reads=["ktmpb"], writes=[("kf3c", kfn % 2)])
                k.dma("sp", kf_d[o, G, :, :], kk_, reads=[("kf3a", kfn % 2), ("kf3b", kfn % 2), ("kf3c", kfn % 2)], writes=[("kfd", o, G)])
            stA(0)
            for g in range(1, 32):
                stA(g)
                stB(g - 1)
            stB(31)
    if "kf" in debug:
        k.barrier()
        for o in range(2):
            for G in (0, 77):
                dk_ = dbg_out("kf_%d_%d" % (o, G), [128, 768])
                k.dma("sp", kf3[0], kf_d[o, G, :, :], writes=["kkdbg"])
                k.dma("sp", dk_, kf3[0], reads=["kkdbg"], writes=["kkdbg2"])
                k.barrier()
    k.barrier()
    if stop == 'filt':
        return nc, IN, DBG, k
    A.reset(PERSIST_T)
    ft1 = [A.alloc([128, 512], F32) for _ in range(4)]
    ft2 = [A.alloc([128, 512], F32) for _ in range(4)]
    fap = [A.alloc([128, 512], BF16) for _ in range(4)]

    cw = A.alloc([128, 3, 3 * HYC], F32)
    cb = A.alloc([128, 3 * HYC], F32)
    for j in range(3):
        for hh in range(3):
            k.dma("sp", cw[:, j, hh * 512:(hh + 1) * 512], bcast(hcw_d[j:j + 1, hh * 512:(hh + 1) * 512], [128, 512]), writes=[("cw", j, hh)])
    for hh in range(3):
        k.dma("sp", cb[:, hh * 512:(hh + 1) * 512], bcast(hcb_d[:, hh * 512:(hh + 1) * 512], [128, 512]), writes=[("cb", hh)])
    k.barrier()
    CH = 64
    Zb = [A.alloc([128, 34, CH], F32) for _ in range(3)]
    zt_ = A.alloc([128, 32, CH], F32)
    zu_ = A.alloc([128, 32, CH], F32)
    xg = [A.alloc([128, 32, CH], F32) for _ in range(2)]
    u1 = A.alloc([128, 16, 32, 4], BF16)
    u2 = A.alloc([128, 16, 32, 4], BF16)
    kfb = [A.alloc([128, 768], F32) for _ in range(3)]
    pt1 = [A.alloc([128, 512], F32) for _ in range(2)]
    pt2 = [A.alloc([128, 512], F32) for _ in range(2)]
    ysb = [A.alloc([128, 512], BF16) for _ in range(2)]
    it1 = [A.alloc([128, 2, 2, 128], F32) for _ in range(2)]
    it2 = [A.alloc([128, 2, 2, 128], F32) for _ in range(2)]
    Bbuf = [A.alloc([128, 2, 2, 4, 128], BF16) for _ in range(2)]
    zmain = zhy_d[0:L, :].rearrange("(p j) c -> p j c", j=32)
    zext = zhy_d[2:L + 2, :].rearrange("(p j) c -> p j c", j=32)
    cnt = [0]

    def conv(u_in, ukey, o, G0, gate, gkey, writer):
        base = cnt[0]
        cnt[0] += 16

        def stA(g):
            n = base + g
            k.dma("pool", kfb[n % 3], kf_d[o, G0 + g, :, :], reads=[("kfd", o, G0 + g)], writes=[("kfb", n % 3)])
            fft_A(u_in[:, g, :, :].rearrange("p i c -> p (i c)"), [ukey], n % 2)

        def stB(g):
            n = base + g
            kfbuf = kfb[n % 3]
            bu = fft_B(n % 2, 2 + n % 2)
            a1 = pt1[n % 2]; a2 = pt2[n % 2]; ys = ysb[n % 2]
            k.op("dve", lambda e: e.tensor_tensor(a1.rearrange("p (r f) -> p r f", r=2), PS[bu][:, :].rearrange("p (r f) -> p r f", r=2),
                                                  bcast(kfbuf[:, 0:256].unsqueeze(1), [128, 2, 256]), ALU.mult),
                 reads=["ps%d" % bu, ("kfb", n % 3)], writes=[("pt1", n % 2)])
            k.op("dve", lambda e: e.tensor_tensor(a2[:, 0:256], PS[bu][:, 256:512], kfbuf[:, 256:512], ALU.mult),
                 reads=["ps%d" % bu, ("kfb", n % 3)], writes=[("pt2a", n % 2)])
            k.op("dve", lambda e: e.tensor_tensor(a2[:, 256:512], PS[bu][:, 0:256], kfbuf[:, 512:768], ALU.mult),
                 reads=["ps%d" % bu, ("kfb", n % 3)], writes=[("pt2b", n % 2)])
            k.op("pool", lambda e: e.tensor_tensor(ys, a1, a2, ALU.add),
                 reads=[("pt1", n % 2), ("pt2a", n % 2), ("pt2b", n % 2)], writes=[("ysb", n % 2)])

        def stC(g):
            n = base + g
            ys = ysb[n % 2]
            bi = 4 + n % 2

            def i1(e):
                ins = None
                for hh in range(2):
                    e.matmul(PS[bi][:, hh * 256:(hh + 1) * 256], ys[:, hh * 128:(hh + 1) * 128], R1, start=True, stop=False)
                    ins = e.matmul(PS[bi][:, hh * 256:(hh + 1) * 256], ys[:, 256 + hh * 128:256 + (hh + 1) * 128], R2, start=False, stop=True)
                return ins
            k.op("pe", i1, reads=[("ysb", n % 2)], writes=["ps%d" % bi])
            b1 = it1[n % 2]; b2 = it2[n % 2]
            Bv = PS[bi][:, :].rearrange("p (h r x) -> p h r x", h=2, r=2)
            k.op("dve", lambda e: e.tensor_tensor(b1, Bv, bcast(TW2[:, :, 0:1, :], [128, 2, 2, 128]), ALU.mult),
                 reads=["ps%d" % bi], writes=[("it1", n % 2)])
            k.op("dve", lambda e: e.tensor_tensor(b2[:, :, 0, :], Bv[:, :, 1, :], TW2[:, :, 1, :], ALU.mult),
                 reads=["ps%d" % bi], writes=[("it2a", n % 2)])
            k.op("dve", lambda e: e.tensor_tensor(b2[:, :, 1, :], Bv[:, :, 0, :], TW2[:, :, 2, :], ALU.mult),
                 reads=["ps%d" % bi], writes=[("it2b", n % 2)])
            q4, slot = (n // 4), n % 4
            Bb = Bbuf[q4 % 2]
            k.op("pool", lambda e: e.tensor_tensor(Bb[:, :, :, slot, :], b1, b2, ALU.add),
                 reads=[("it1", n % 2), ("it2a", n % 2), ("it2b", n % 2)], writes=[("Bbuf", q4 % 2, slot)])
            if slot == 3:
                bo = 6 + q4 % 2

                def i2(e):
                    e.matmul(PS[bo][:, :], C2[:, 0, :], Bb[:, 0, 0, :, :].rearrange("p s x -> p (s x)"), start=True, stop=False)
                    e.matmul(PS[bo][:, :], NS2[:, 0, :], Bb[:, 0, 1, :, :].rearrange("p s x -> p (s x)"), start=False, stop=False)
                    e.matmul(PS[bo][:, :], C2[:, 1, :], Bb[:, 1, 0, :, :].rearrange("p s x -> p (s x)"), start=False, stop=False)
                    return e.matmul(PS[bo][:, :], NS2[:, 1, :], Bb[:, 1, 1, :, :].rearrange("p s x -> p (s x)"), start=False, stop=True)
                k.op("pe", i2, reads=[("Bbuf", q4 % 2, s_) for s_ in range(4)], writes=["ps%d" % bo])
                gq = g // 4
                yv = PS[bo][:, :].rearrange("p (s i c) -> p s i c", s=4, i=32)
                gv = gate[:, :, gq * 16:(gq + 1) * 16].rearrange("p i (s c) -> p s i c", s=4)
                writer(gq, yv, gv, "ps%d" % bo, gkey)
        for st in range(18):
            if st < 16:
                stA(st)
            if 1 <= st <= 16:
                stB(st - 1)
            if 2 <= st <= 17:
                stC(st - 2)

    for chn in range(HYC // CH):
        ch0 = chn * CH
        G0 = chn * 16
        for kind in range(3):
            col0 = kind * HYC + ch0
            k.dma("sp", Zb[kind][:, 0:16, :], zmain[:, 0:16, col0:col0 + CH], writes=[("Zb", kind, 0)])
            k.dma("sp", Zb[kind][:, 16:32, :], zmain[:, 16:32, col0:col0 + CH], writes=[("Zb", kind, 1)])
            k.dma("sp", Zb[kind][:, 32:34, :], zext[:, 30:32, col0:col0 + CH], writes=[("Zb", kind, 2)])
            zk = [("Zb", kind, j) for j in range(3)]
            wv = lambda j: bcast(cw[:, j:j + 1, col0:col0 + CH], [128, 32, CH])
            k.op("dve", lambda e: e.tensor_tensor(zt_, Zb[kind][:, 0:32, :], wv(0), ALU.mult), reads=zk, writes=["zt"])
            k.op("pool", lambda e: e.tensor_tensor(zu_, Zb[kind][:, 1:33, :], wv(1), ALU.mult), reads=zk, writes=["zu"])
            k.op("dve", lambda e: e.tensor_tensor(zt_, zt_, zu_, ALU.add), reads=["zt", "zu"], writes=["zt"])
            k.op("pool", lambda e: e.tensor_tensor(zu_, Zb[kind][:, 2:34, :], wv(2), ALU.mult), reads=zk + ["zt"], writes=["zu"])
            k.op("dve", lambda e: e.tensor_tensor(zt_, zt_, zu_, ALU.add), reads=["zt", "zu"], writes=["zt"])
            bv = bcast(cb[:, col0:col0 + CH].unsqueeze(1), [128, 32, CH])
            if kind == 0:
                k.op("dve", lambda e: e.tensor_tensor(u1.rearrange("p g i c -> p i g c"), zt_.rearrange("p i (g c) -> p i g c", c=4),
                                                      bv.rearrange("p i (g c) -> p i g c", c=4) if False else bcast(cb[:, col0:col0 + CH].rearrange("p (g c) -> p g c", c=4).unsqueeze(1), [128, 32, 16, 4]), ALU.add),
                     reads=["zt"], writes=["u1"])
            else:
                k.op("dve", lambda e: e.tensor_tensor(xg[kind - 1], zt_, bv, ALU.add), reads=["zt"], writes=[("xg", kind - 1)])

        def w1_(gq, yv, gv, pkey, gkey):
            k.op("dve", lambda e: e.tensor_tensor(u2[:, gq * 4:(gq + 1) * 4, :, :], yv, gv, ALU.mult), reads=[pkey, gkey], writes=["u2"])

        def w2_(gq, yv, gv, pkey, gkey):
            ov = hy_sb[:, :, ch0 + gq * 16:ch0 + (gq + 1) * 16].rearrange("p i (s c) -> p s i c", s=4)
            k.op("dve", lambda e: e.tensor_tensor(ov, yv, gv, ALU.mult), reads=[pkey, gkey], writes=[("hy", chn, gq)])
        conv(u1, "u1", 0, G0, xg[0], ("xg", 0), w1_)
        conv(u2, "u2", 1, G0, xg[1], ("xg", 1), w2_)
    if "hy" in debug:
        k.barrier()
        hyf = A.alloc([128, 4, HYC], F32) if False else pt1[0]
        for i_ in range(4):
            k.op("dve", lambda e, i_=i_: e.tensor_copy(hyf, hy_sb[:, i_, :]), writes=["hyf"])
            k.dma("sp", dbg_out("hy%d" % i_, [128, HYC]), hyf, reads=["hyf"], writes=["hyfd"])
            k.barrier()
    k.barrier()
    if stop == 'hy':
        return nc, IN, DBG, k
    A.reset(PERSIST2)
    wao = A.alloc([128, 4, D], BF16)
    wout = A.alloc([128, 8, D], BF16)
    why = A.alloc([128, 4, D], BF16)
    wqb = A.alloc([128, 8, 2048], BF16)
    keysT = A.alloc([128, 16, 128], BF16)
    g1bc = A.alloc([128, D], F32)
    g2bc = A.alloc([128, D], F32)
    A2bc = A.alloc([128, D], BF16)
    B2bc = A.alloc([128, D], BF16)
    gfin = A.alloc([128, D], F32)
    iota16 = A.alloc([128, 16], F32)
    PB = A.off
    rep = A.alloc([128, 128], F32)
    stg = [A.alloc([128, D], F32) for _ in range(2)]
    io_ = A.alloc([128, 16], I32)
    k.op("pool", lambda e: e.iota(io_, pattern=[[1, 16]], base=0, channel_multiplier=0), writes=["io_"])
    k.op("dve", lambda e: e.tensor_copy(iota16, io_), reads=["io_"], writes=["iota16"])
    k.dma("sp", gfin, bcast(gfin_d, [128, D]), writes=["gfin"])

    def load_w(dst_fn, src_fn, n):
        for j in range(n):
            b = j % 2
            k.dma("sp" if b == 0 else "pool", stg[b], src_fn(j), writes=[("stg", b)])
            if b == 0:
                k.op("act", lambda e, j=j: e.copy(dst_fn(j), stg[0]), reads=[("stg", 0)], writes=[("wload", id(dst_fn), j)])
            else:
                k.op("dve", lambda e, j=j: e.tensor_copy(dst_fn(j), stg[1]), reads=[("stg", 1)], writes=[("wload", id(dst_fn), j)])
    load_w(lambda j: wao[:, j, :], lambda j: wao_d[j * 128:(j + 1) * 128, :], 4)
    load_w(lambda j: wout[:, j, :], lambda j: wout_d[j * 128:(j + 1) * 128, :], 8)
    load_w(lambda j: why[:, j, :], lambda j: why_d[j * 128:(j + 1) * 128, :], 4)
    load_w(lambda j: wqb[:, j // 2, (j % 2) * 1024:(j % 2 + 1) * 1024], lambda j: wq_d[(j // 2) * 128:(j // 2 + 1) * 128, (j % 2) * 1024:(j % 2 + 1) * 1024], 16)
    load_w(lambda j: keysT[:, j * 8:(j + 1) * 8, :].rearrange("p a n -> p (a n)"), lambda j: keysT_d[:, j * 1024:(j + 1) * 1024], 2)
    bcl = [(g1bc, lambda j: modT[:, 16 + j, 0:1]), (g2bc, lambda j: modT[:, 40 + j, 0:1]),
           (A2bc, lambda j: A2[:, j:j + 1]), (B2bc, lambda j: modT[:, 24 + j, 0:1])]
    for bi_, (dst, colf) in enumerate(bcl):
        for j in range(8):
            k.op("dve", lambda e, j=j, colf=colf: e.tensor_copy(rep, bcast(colf(j), [128, 128])), reads=["modT", "A2"], writes=["rep"])
            k.op("pe", lambda e, j=j: e.matmul(PS[j // 4][:, (j % 4) * 128:(j % 4 + 1) * 128], rep, ident, start=True, stop=True),
                 reads=["rep", "ident"], writes=["ps%d" % (j // 4)])
        for hf in range(2):
            k.op("act", lambda e, hf=hf, dst=dst: e.copy(dst[:, hf * 512:(hf + 1) * 512], PS[hf][:, :]), reads=["ps%d" % hf], writes=[("bc", bi_, hf)])
    k.barrier()
    A.reset(PB)
    aTt = A.alloc([128, 4, 128], BF16)
    xs = A.alloc([128, D], F32)
    mg = A.alloc([128, D], F32)
    mT = A.alloc([128, 8, 128], BF16)
    x1 = A.alloc([128, D], F32)
    ob = A.alloc([128, D], F32)
    hyT = A.alloc([128, 4, 128], BF16)
    sB = A.alloc([128, 16], F32)
    qTs = A.alloc([128, 16, 128], BF16)
    big8 = A.alloc([128, 2048], F32)
    hyf = big8[:, 0:HYC]
    mx = A.alloc([128, 16, 16], F32)
    mi = A.alloc([128, 16, 16], U32)
    mif = A.alloc([128, 16, 16], F32)
    wk4 = A.alloc([128, 4, 256], F32)
    best = A.alloc([128, 8, 16], F32)
    pos = A.alloc([128, 8, 16], U32)
    pia = A.alloc([128, 8, 16], U32)
    pib = A.alloc([128, 8, 16], U32)
    paf = A.alloc([128, 8, 16], F32)
    pbf = A.alloc([128, 8, 16], F32)
    isel = A.alloc([128, 2, 128], F32)
    eidf = A.alloc([128, 128], F32)
    eidx = A.alloc([128, 128], I32)
    gw = A.alloc([128, 8, 16], F32)
    sm8 = A.alloc([128, 16], F32)
    av = A.alloc([128, 128], F32)
    wgt = A.alloc([128, 128], F32)
    junkp = ob
    acc = xs
    gl = A.alloc([128, 128], F32)
    dg = [A.alloc([128, 128], BF16) for _ in range(4)]
    identb = A.alloc([128, 128], BF16)
    k.op("dve", lambda e: e.tensor_copy(identb, ident), reads=["ident"], writes=["identb"])
    NR = 10
    ring = [A.alloc([128, 2 * D], BF16) for _ in range(NR)]
    gsb = ring[NR - 1]
    GSK = ("ring", NR - 1)
    out_v = out_d.rearrange("(p i) d -> i p d", i=NT)
    attn_ev = attnT_d.rearrange("(j two) d t -> two d j t", two=2)
    rn = [0]
    sc3 = big8.rearrange("p (a n) -> p a n", a=16)
    cand = big8.rearrange("p (h a b) -> p h a b", h=8, a=16)
    mxv = mx.rearrange("p (h t) k -> p h t k", t=2)
    mifv = mif.rearrange("p (h t) k -> p h t k", t=2)
    npe = NT if "nopeer" not in skip else 0
    GW = 512 if "halfrow" in skip else D
    for it in range(NT):
        k.dma("sp", xs, x_v[it], writes=["xs"])
        for two in range(2):
            k.dma("pool", aTt[64 * two:64 * two + 64, :, :], attn_ev[two][:, :, it * 128:(it + 1) * 128], writes=[("aTt", two)])
        k.dma("sp", gsb, gates_v[it], writes=[GSK])
        k.op("act", lambda e: e.copy(hyf, hy_sb[:, it, :]), writes=["hyf"])

        def trh(e):
            ins = None
            for j in range(4):
                ins = e.transpose(PS[6][:, j * 128:(j + 1) * 128], hyf[:, j * 128:(j + 1) * 128], ident)
            return ins
        k.op("pe", trh, reads=["hyf", "ident"], writes=["ps6"])
        k.op("act", lambda e: e.copy(hyT, PS[6][:, :].rearrange("p (j t) -> p j t", j=4)), reads=["ps6"], writes=["hyT"])
        for hf in range(2):
            def pa(e, hf=hf):
                ins = None
                for j in range(4):
                    ins = e.matmul(PS[hf][:, :], aTt[:, j, :], wao[:, j, hf * 512:(hf + 1) * 512], start=(j == 0), stop=(j == 3))
                return ins
            k.op("pe", pa, reads=[("aTt", 0), ("aTt", 1)], writes=["ps%d" % hf])
            k.op("dve", lambda e, hf=hf: e.tensor_tensor(mg[:, hf * 512:(hf + 1) * 512], PS[hf][:, :], gsb[:, hf * 512:(hf + 1) * 512], ALU.mult),
                 reads=["ps%d" % hf, GSK], writes=[("mg", hf)])

            def ph(e, hf=hf):
                ins = None
                for j in range(4):
                    ins = e.matmul(PS[6 + hf][:, :], hyT[:, j, :], why[:, j, hf * 512:(hf + 1) * 512], start=(j == 0), stop=(j == 3))
                return ins
            k.op("pe", ph, reads=["hyT"], writes=["ps%d" % (6 + hf)])
            k.op("dve", lambda e, hf=hf: e.tensor_tensor(ob[:, hf * 512:(hf + 1) * 512], PS[6 + hf][:, :], gsb[:, D + hf * 512:D + (hf + 1) * 512], ALU.mult),
                 reads=["ps%d" % (6 + hf), GSK], writes=[("ob", hf)])
            k.op("pool", lambda e, hf=hf: e.tensor_tensor(mg[:, hf * 512:(hf + 1) * 512], mg[:, hf * 512:(hf + 1) * 512], ob[:, hf * 512:(hf + 1) * 512], ALU.add),
                 reads=[("mg", hf), ("ob", hf)], writes=[("mg", hf)])
        for hf in range(2):
            def trm(e, hf=hf):
                ins = None
                for j in range(4):
                    jj = hf * 4 + j
                    ins = e.transpose(PS[2 + hf][:, j * 128:(j + 1) * 128], mg[:, jj * 128:(jj + 1) * 128], ident)
                return ins
            k.op("pe", trm, reads=[("mg", 0), ("mg", 1)], writes=["ps%d" % (2 + hf)])
            k.op("act", lambda e, hf=hf: e.copy(mT[:, hf * 4:(hf + 1) * 4, :], PS[2 + hf][:, :].rearrange("p (j t) -> p j t", j=4)),
                 reads=["ps%d" % (2 + hf)], writes=[("mT", hf)])
        for hf in range(2):
            def mo(e, hf=hf):
                ins = None
                for kk in range(8):
                    ins = e.matmul(PS[4 + hf][:, :], mT[:, kk, :], wout[:, kk, hf * 512:(hf + 1) * 512], start=(kk == 0), stop=(kk == 7))
                return ins
            k.op("pe", mo, reads=[("mT", 0), ("mT", 1)], writes=["ps%d" % (4 + hf)])
            k.op("dve", lambda e, hf=hf: e.tensor_tensor(x1[:, hf * 512:(hf + 1) * 512], PS[4 + hf][:, :], g1bc[:, hf * 512:(hf + 1) * 512], ALU.mult),
                 reads=["ps%d" % (4 + hf)], writes=[("x1", hf)])
        k.op("pool", lambda e: e.tensor_tensor(x1, x1, xs, ALU.add), reads=[("x1", 0), ("x1", 1), "xs"], writes=["x1s"])
        if "x1" in debug and it < 4:
            k.dma("sp", dbg_out("x1_%d" % it, [128, D]), x1, reads=["x1s"])
        s = sB
        if it < npe:
            k.op("act", lambda e: e.activation(mg, x1, AF.Square, accum_out=s[:, 4:5]), reads=["x1s", ("mg", 0), ("mg", 1)], writes=["mgx", "sB2"])
            k.op("dve", lambda e: e.tensor_scalar(s[:, 5:6], s[:, 4:5], 1.0 / D, EPS, ALU.mult, ALU.add), reads=["sB2"], writes=["sB2"])
            k.op("act", lambda e: e.activation(s[:, 6:7], s[:, 5:6], AF.Sqrt), reads=["sB2"], writes=["sB2"])
            k.op("dve", lambda e: e.reciprocal(s[:, 7:8], s[:, 6:7]), reads=["sB2"], writes=["sB2"])
            k.op("act", lambda e: e.activation(mg, x1, AF.Identity, scale=s[:, 7:8]), reads=["x1s", "sB2", "mgx"], writes=["mgx"])
            for hf in range(2):
                def tr2(e, hf=hf):
                    ins = None
                    for j in range(4):
                        jj = hf * 4 + j
                        ins = e.transpose(PS[hf][:, j * 128:(j + 1) * 128], mg[:, jj * 128:(jj + 1) * 128], ident)
                    return ins
                k.op("pe", tr2, reads=["mgx"], writes=["ps%d" % hf])
            for jj in range(8):
                hf, j = jj // 4, jj % 4
                if jj % 2 == 0:
                    k.op("act", lambda e, jj=jj, hf=hf, j=j: e.activation(mT[:, jj, :], PS[hf][:, j * 128:(j + 1) * 128], AF.Identity,
                                                                       scale=A2[:, jj:jj + 1], bias=modT[:, 24 + jj, 0:1]),
                         reads=["ps%d" % hf, ("mT", 0), ("mT", 1)], writes=[("h2T", jj)])
                else:
                    k.op("dve", lambda e, jj=jj, hf=hf, j=j: e.tensor_scalar(mT[:, jj, :], PS[hf][:, j * 128:(j + 1) * 128],
                                                                         A2[:, jj:jj + 1], modT[:, 24 + jj, 0:1], ALU.mult, ALU.add),
                         reads=["ps%d" % hf, ("mT", 0), ("mT", 1)], writes=[("h2T", jj)])
            k.op("dve", lambda e: e.tensor_tensor(mg, mg, A2bc, ALU.mult), reads=["mgx"], writes=["mgx"])
            k.op("pool", lambda e: e.tensor_tensor(mg, mg, B2bc, ALU.add), reads=["mgx"], writes=["h2"])
            h2keys = [("h2T", jj) for jj in range(8)]
            for qb in range(4):
                def qmm(e, qb=qb):
                    ins = None
                    for q4 in range(4):
                        hh = qb * 4 + q4
                        for kk in range(8):
                            ins = e.matmul(PS[2 + qb][:, q4 * 128:(q4 + 1) * 128], wqb[:, kk, hh * 128:(hh + 1) * 128], mT[:, kk, :],
                                           start=(kk == 0), stop=(kk == 7))
                    return ins
                k.op("pe", qmm, reads=h2keys, writes=["ps%d" % (2 + qb)])
                k.op("act", lambda e, qb=qb: e.copy(qTs[:, qb * 4:(qb + 1) * 4, :], PS[2 + qb][:, :].rearrange("p (a t) -> p a t", a=4)),
                     reads=["ps%d" % (2 + qb)], writes=[("qTs", qb)])
            sbanks = [6, 7, 0, 1]
            for qb in range(4):
                bk = sbanks[qb]

                def smm(e, qb=qb, bk=bk):
                    ins = None
                    for q4 in range(4):
                        hh = qb * 4 + q4
                        ins = e.matmul(PS[bk][:, q4 * 128:(q4 + 1) * 128], qTs[:, hh, :], keysT[:, hh, :], start=True, stop=True)
                    return ins
                k.op("pe", smm, reads=[("qTs", qb)], writes=["ps%d" % bk])
                if qb % 2 == 0:
                    k.op("act", lambda e, qb=qb, bk=bk: e.copy(big8[:, qb * 512:(qb + 1) * 512], PS[bk][:, :]), reads=["ps%d" % bk, "big8"], writes=[("sc", qb)])
                else:
                    k.op("dve", lambda e, qb=qb, bk=bk: e.tensor_copy(big8[:, qb * 512:(qb + 1) * 512], PS[bk][:, :]), reads=["ps%d" % bk, "big8"], writes=[("sc", qb)])
            for q4 in range(4):
                hhs = [4 * q4 + u for u in range(4)]
                sk_ = [("sc", q4)]
                for hh in hhs:
                    k.op("dve", lambda e, hh=hh: e.max(mx[:, hh, 0:8], sc3[:, hh, :]), reads=sk_, writes=[("mx", hh)])
                for hh in hhs:
                    k.op("dve", lambda e, hh=hh: e.max_index(mi[:, hh, 0:8], mx[:, hh, 0:8], sc3[:, hh, :]), reads=sk_ + [("mx", hh)], writes=[("mi", hh)])
                for u, hh in enumerate(hhs):
                    k.op("dve", lambda e, hh=hh, u=u: e.match_replace(wk4[:, u, 0:128], mx[:, hh, 0:8], sc3[:, hh, :], -1e30), reads=sk_ + [("mx", hh)], writes=[("wk", u)])
                for u, hh in enumerate(hhs):
                    k.op("dve", lambda e, hh=hh, u=u: e.max(mx[:, hh, 8:16], wk4[:, u, 0:128]), reads=[("wk", u)], writes=[("mx", hh)])
                for u, hh in enumerate(hhs):
                    k.op("dve", lambda e, hh=hh, u=u: e.max_index(mi[:, hh, 8:16], mx[:, hh, 8:16], wk4[:, u, 0:128]), reads=[("wk", u), ("mx", hh)], writes=[("mi", hh)])
            mxk = [("mx", hh) for hh in range(16)]
            mik = [("mi", hh) for hh in range(16)]
            k.op("dve", lambda e: e.tensor_copy(mif, mi), reads=mik, writes=["mif"])
            k.op("dve", lambda e: e.tensor_tensor(cand, bcast(mxv[:, :, 0, :].unsqueeze(3), [128, 8, 16, 16]),
                                                  bcast(mxv[:, :, 1, :].unsqueeze(2), [128, 8, 16, 16]), ALU.add),
                 reads=mxk + [("sc", q) for q in range(4)], writes=["big8"])
            candf = big8.rearrange("p (h x) -> p h x", h=8)
            for q4 in range(2):
                hs = [4 * q4 + u for u in range(4)]
                for h in hs:
                    k.op("dve", lambda e, h=h: e.max(best[:, h, 0:8], candf[:, h, :]), reads=["big8"], writes=[("best", h)])
                for h in hs:
                    k.op("dve", lambda e, h=h: e.max_index(pos[:, h, 0:8], best[:, h, 0:8], candf[:, h, :]), reads=["big8", ("best", h)], writes=[("pos", h)])
                for u, h in enumerate(hs):
                    k.op("dve", lambda e, h=h, u=u: e.match_replace(wk4[:, u, :], best[:, h, 0:8], candf[:, h, :], -1e30), reads=["big8", ("best", h)], writes=[("wk", u)])
                for u, h in enumerate(hs):
                    k.op("dve", lambda e, h=h, u=u: e.max(best[:, h, 8:16], wk4[:, u, :]), reads=[("wk", u)], writes=[("best", h)])
                for u, h in enumerate(hs):
                    k.op("dve", lambda e, h=h, u=u: e.max_index(pos[:, h, 8:16], best[:, h, 8:16], wk4[:, u, :]), reads=[("wk", u), ("best", h)], writes=[("pos", h)])
            bk_ = [("best", h) for h in range(8)]
            pk_ = [("pos", h) for h in range(8)]
            k.op("dve", lambda e: e.tensor_scalar(pia, pos, 4, None, ALU.arith_shift_right), reads=pk_, writes=["pia"])
            k.op("dve", lambda e: e.tensor_scalar(pib, pos, 15, None, ALU.bitwise_and), reads=pk_, writes=["pib"])
            k.op("dve", lambda e: e.tensor_copy(paf, pia), reads=["pia"], writes=["paf"])
            k.op("dve", lambda e: e.tensor_copy(pbf, pib), reads=["pib"], writes=["pbf"])
            oh = big8.rearrange("p (h a b) -> p h a b", h=8, a=16)
            for t_, pf_ in enumerate((paf, pbf)):
                k.op("dve", lambda e, pf_=pf_: e.tensor_tensor(oh, bcast(pf_.unsqueeze(3), [128, 8, 16, 16]),
                                                               bcast(iota16.unsqueeze(1).unsqueeze(1), [128, 8, 16, 16]), ALU.is_equal),
                     reads=["paf", "pbf", "iota16", "big8"] + bk_ + pk_, writes=["big8"])
                k.op("dve", lambda e, t_=t_: e.tensor_tensor(oh, oh, bcast(mifv[:, :, t_, :].unsqueeze(2), [128, 8, 16, 16]), ALU.mult),
                     reads=["big8", "mif"], writes=["big8"])
                k.op("dve", lambda e, t_=t_: e.tensor_reduce(isel[:, t_, :].rearrange("p (h a) -> p h a", h=8), oh, AX.X, ALU.add),
                     reads=["big8"], writes=[("isel", t_)])
            k.op("dve", lambda e: e.scalar_tensor_tensor(eidf, isel[:, 0, :], 128.0, isel[:, 1, :], ALU.mult, ALU.add),
                 reads=[("isel", 0), ("isel", 1)], writes=["eidf"])
            k.op("dve", lambda e: e.tensor_copy(eidx, eidf), reads=["eidf"], writes=["eidx"])
            k.op("dve", lambda e: e.tensor_tensor(gw, best, bcast(best[:, :, 0:1], [128, 8, 16]), ALU.subtract), reads=bk_, writes=["gw"])
            k.op("act", lambda e: e.activation(gw, gw, AF.Exp), reads=["gw"], writes=["gw"])
            k.op("dve", lambda e: e.tensor_reduce(sm8[:, 0:8], gw, AX.X, ALU.add), reads=["gw"], writes=["sm8"])
            k.op("dve", lambda e: e.reciprocal(sm8[:, 8:16], sm8[:, 0:8]), reads=["sm8"], writes=["sm8"])
            k.op("dve", lambda e: e.tensor_tensor(gw, gw, bcast(sm8[:, 8:16].unsqueeze(2), [128, 8, 16]), ALU.mult), reads=["gw", "sm8"], writes=["gw"])
            k.op("dve", lambda e: e.memset(av, 0.0), writes=["av"])
            gwf = gw.rearrange("p h k -> p (h k)")
            slots = {}
            for j in range(129):
                if j < 128:
                    r_ = rn[0] % NR; rn[0] += 1
                    slots[j] = r_
                    k.dma("pool", None, None, reads=["eidx"], writes=[("ring", r_)],
                          fn=lambda e, j=j, r_=r_: e.indirect_dma_start(out=ring[r_], out_offset=None, in_=uvb_d[:, :],
                                                                        in_offset=bass.IndirectOffsetOnAxis(ap=eidx[:, j:j + 1], axis=0)))
                    k.op("dve", lambda e, j=j, r_=r_: e.scalar_tensor_tensor(junkp, ring[r_][:, 0:D], 1.0, mg, ALU.mult, ALU.mult, accum_out=av[:, j:j + 1]),
                         reads=[("ring", r_), "h2", "av"], writes=[("av", j)])
                    k.op("act", lambda e, j=j: e.activation(gl[:, j:j + 1], av[:, j:j + 1], AF.Gelu), reads=[("av", j)], writes=[("gl", j)])
                if j >= 1:
                    jj = j - 1
                    r_ = slots[jj]
                    db = jj % 4
                    k.op("dve", lambda e, jj=jj, db=db: e.tensor_scalar(dg[db], identb, gl[:, jj:jj + 1], gwf[:, jj:jj + 1], ALU.mult, ALU.mult),
                         reads=[("gl", jj), "gw", "identb"], writes=[("dg", db)])

                    def vmm(e, jj=jj, db=db, r_=r_):
                        e.matmul(PS[2][:, :], dg[db], ring[r_][:, D:D + 512], start=(jj == 0), stop=(jj == 127))
                        return e.matmul(PS[3][:, :], dg[db], ring[r_][:, D + 512:2 * D], start=(jj == 0), stop=(jj == 127))
                    k.op("pe", vmm, reads=[("dg", db), ("ring", r_)], writes=(["ps2", "ps3"] if jj in (0, 127) else []))
            for hf in range(2):
                k.op("act", lambda e, hf=hf: e.copy(acc[:, hf * 512:(hf + 1) * 512], PS[2 + hf][:, :]), reads=["ps%d" % (2 + hf), "x1s"], writes=["xs"])
            if "peer" in debug and it < 2:
                k.dma("sp", dbg_out("peer_%d" % it, [128, D]), acc, reads=["xs"])
                k.dma("sp", dbg_out("h2_%d" % it, [128, D]), mg, reads=["h2"])
                k.dma("sp", dbg_out("eid_%d" % it, [128, 128]), eidf, reads=["eidf"])
            k.op("dve", lambda e: e.tensor_tensor(acc, acc, g2bc, ALU.mult), reads=["xs"], writes=["xs"])
            k.op("pool", lambda e: e.tensor_tensor(x1, x1, acc, ALU.add), reads=["xs", "x1s"], writes=["x1s"])
        k.op("act", lambda e: e.activation(ob, x1, AF.Square, accum_out=s[:, 0:1]), reads=["x1s", ("ob", 0), ("ob", 1)], writes=["obx", "sB"])
        k.op("dve", lambda e: e.tensor_scalar(s[:, 1:2], s[:, 0:1], 1.0 / D, EPS, ALU.mult, ALU.add), reads=["sB"], writes=["sB"])
        k.op("act", lambda e: e.activation(s[:, 2:3], s[:, 1:2], AF.Sqrt), reads=["sB"], writes=["sB"])
        k.op("dve", lambda e: e.reciprocal(s[:, 3:4], s[:, 2:3]), reads=["sB"], writes=["sB"])
        k.op("dve", lambda e: e.scalar_tensor_tensor(ob, x1, s[:, 3:4], gfin, ALU.mult, ALU.mult),
             reads=["x1s", "sB", "gfin", "obx"], writes=["obx"])
        k.dma("sp", out_v[it], ob, reads=["obx"], writes=["out"])
    k.barrier()
    return nc, IN, DBG, k


def rope_tables():
    p = np.arange(128)[:, None]
    i = np.arange(NT)[None, :]
    t = 32 * p + i
    row = (t // 64).astype(np.float32)
    col = (t % 64).astype(np.float32)
    inv = (10000.0 ** (-np.arange(0, 32, 2, dtype=np.float32) / 32)).astype(np.float32)
    ar = row[..., None] * inv
    ac = col[..., None] * inv
    ang = np.concatenate([ar, ar, ac, ac], axis=-1)
    cos = np.cos(ang).astype(np.float32)
    sin = np.sin(ang).astype(np.float32)
    sgn = np.ones(64, np.float32)
    sgn[0:16] = -1; sgn[32:48] = -1
    return cos.reshape(128, NT * 64), (sin * sgn).reshape(128, NT * 64)


def swap_halves(g):
    g = g.reshape(2, 2, 16)
    return np.ascontiguousarray(g[:, ::-1, :]).reshape(1, 64)


def fft_plan_tables():
    p = np.arange(128, dtype=np.float64)[:, None]
    f1 = np.arange(256, dtype=np.float64)[None, :]
    a = 2 * np.pi * p * f1 / 256
    W1 = np.concatenate([np.cos(a), -np.sin(a)], axis=1)
    P = np.arange(128)
    s2 = (P // 4).astype(np.float64)
    c4 = P % 4
    th = 2 * np.pi * s2[:, None] * s2[None, :] / 32
    dl = (c4[:, None] == c4[None, :]).astype(np.float64)
    KC = np.cos(th) * dl
    KS = np.sin(th) * dl
    R1 = np.concatenate([KC, KS], axis=1)
    R2 = np.concatenate([-KS, KC], axis=1)
    hh = np.arange(2, dtype=np.float64)[None, :, None]
    s1 = np.arange(128, dtype=np.float64)[None, None, :]
    a2 = 2 * np.pi * (128 * hh + p[:, :, None]) * s1 / 256
    C2 = np.cos(a2).reshape(128, 256)
    NS2 = (-np.sin(a2)).reshape(128, 256)
    ph = 2 * np.pi * s2[:, None] * f1 / 8192
    TW1 = np.concatenate([np.cos(ph), np.sin(ph), -np.sin(ph)], axis=1)
    f1b = (128 * np.arange(2, dtype=np.float64)[None, :, None] + p[:, :, None])
    ph2 = 2 * np.pi * s2[None, None, :] * f1b / 8192
    TW2 = np.stack([np.cos(ph2), -np.sin(ph2), np.sin(ph2)], axis=2).reshape(128, 768)
    f = lambda x: np.ascontiguousarray(x.astype(np.float32))
    return {"W1": f(W1), "KC": f(KC), "KS": f(KS), "NKS": f(-KS), "R1": f(R1), "R2": f(R2),
            "C2": f(C2), "NS2": f(NS2), "TW1": f(TW1), "TW2": f(TW2)}


def filter_consts():
    t01 = np.linspace(0.0, 1.0, L, dtype=np.float32)[:, None]
    w = (np.float32(2.0 * np.pi) * np.arange(L, dtype=np.float32)[:, None] / np.float32(L)).astype(np.float32)
    fb = np.linspace(1e-4, 15, 16, dtype=np.float32)[None]
    z = np.concatenate([t01, np.cos(fb * w), -np.sin(fb * w)], axis=-1).astype(np.float32)
    max_decay = np.log(1e-2) / 0.3
    min_decay = np.log(1e-2) / 1.5
    deltas = np.abs(np.linspace(min_decay, max_decay, HYC, dtype=np.float32))
    return {"zT": np.ascontiguousarray(z.T), "t01": np.ascontiguousarray(t01.T),
            "ndelT": np.ascontiguousarray((-deltas).reshape(4, 128).T.astype(np.float32))}


def make_in_maps(inputs):
    f = lambda a: np.ascontiguousarray(np.asarray(a, dtype=np.float32))
    cos, sin = rope_tables()
    shared = {
        "cctxT": f(inputs["c_ctx"].reshape(8, 128).T),
        "ada_w": f(inputs["ada_w"][0]),
        "ada_bT": f(inputs["ada_b"][0].reshape(48, 128).T),
        "gmixT": f(inputs["norm_mix_g"][0].reshape(8, 128).T),
        "gffnT": f(inputs["norm_ffn_g"][0].reshape(8, 128).T),
        "w_in": f(inputs["w_in"][0]),
        "rope_cos": cos, "rope_sin": sin,
        "gq": f(inputs["q_norm_g"][0].reshape(1, 64)),
        "gk": f(inputs["k_norm_g"][0].reshape(1, 64)),
        "gqsw": f(swap_halves(np.asarray(inputs["q_norm_g"][0]))),
        "gksw": f(swap_halves(np.asarray(inputs["k_norm_g"][0]))),
        "w_attn_out": f(inputs["w_attn_out"][0]),
        "w_out": f(inputs["w_out"][0]),
        "final_norm_g": f(inputs["final_norm_g"].reshape(1, D)),
        "w_hy_out": f(inputs["w_hy_out"][0]), "peer_wq": f(inputs["peer_wq"][0]),
        "keysT": f(np.stack([np.asarray(inputs["peer_keys1"][0]), np.asarray(inputs["peer_keys2"][0])], axis=1).transpose(3, 0, 1, 2).reshape(128, 2048)),
        "peer_u": f(inputs["peer_u"][0]), "peer_v": f(inputs["peer_v"][0]),
        "hf_w1": f(inputs["hf_w1"][0]), "hf_w2": f(inputs["hf_w2"][0]), "hf_w3": f(inputs["hf_w3"][0]), "hf_w4": f(inputs["hf_w4"][0]),
        "hf_b": f(np.stack([np.asarray(inputs["hf_b1"][0]), np.asarray(inputs["hf_b2"][0]), np.asarray(inputs["hf_b3"][0]), np.asarray(inputs["hf_freq"][0])], axis=1)),
        "hy_conv_w": f(inputs["hy_conv_w"][0]), "hy_conv_b": f(np.asarray(inputs["hy_conv_b"][0]).reshape(1, 3 * HYC)),
        "skipT": f(np.asarray(inputs["hy_skip"][0]).reshape(2, 128, 4)[:, :, np.arange(128) % 4].transpose(2, 0, 1).reshape(128, 256)),
    }
    shared.update(fft_plan_tables())
    shared.update(filter_consts())
    maps = []
    for b in range(8):
        m = dict(shared)
        m["x"] = f(inputs["x"][b])
        m["ctx"] = f(inputs["ctx"][b])
        m["cT"] = f(inputs["c"][b].reshape(8, 128).T)
        maps.append(m)
    return maps


def kernel(**inputs):
    nc, IN, DBG, k = build_program()
    maps = make_in_maps(inputs)
    maps = [{n: m[n] for n in IN} for m in maps]
    res = run_bass_kernel_spmd(nc, maps, core_ids=list(range(8)))
    out = np.stack([np.asarray(r["out"], dtype=np.float32) for r in res.results], axis=0)
    return out
```
